# Optimizing a Trainium2 kernel written in Bass

```python
import math
import jax, jax.numpy as jnp
from jax import lax
import numpy as np


D_MODEL = 4096
BATCH = 4
SEQ = 2048
DEPTH = 2

HEAD_DIM = 128
N_MIXERS = 4
MIX_WIDTH = D_MODEL
GROUP_WIDTH = MIX_WIDTH // N_MIXERS
N_HEADS = GROUP_WIDTH // HEAD_DIM
MOBA_BLOCK = 256
MOBA_TOPK = 3
MOBA_Q_CHUNK = 64
SB_Q_BLOCK = 128
POOL_WINDOWS = (2, 4, 8, 16)
N_POOL_GROUPS = len(POOL_WINDOWS)
POOL_GROUP_DIM = GROUP_WIDTH // N_POOL_GROUPS
DILATED_PATTERNS = ((128, 1), (512, 4), (2048, 16))
DIL_Q_CHUNK = 64
D_FF = ((8 * D_MODEL + 3 * 256 - 1) // (3 * 256)) * 256
IN_WIDTH = 3 * 3 * GROUP_WIDTH + GROUP_WIDTH
RMS_EPS = 1e-6
NEG_INF = -1e30

kernel_name = "hybrid_moba_stickbreak_pool_dilated_block"


def rms_norm(x, g):
    xf = x.astype(jnp.float32)
    y = xf * lax.rsqrt(jnp.mean(xf * xf, axis=-1, keepdims=True) + RMS_EPS)
    return (y * g.astype(jnp.float32)).astype(x.dtype)


def moba_attention(q, k, v):
    B, H, S, hd = q.shape
    nb = -(-S // MOBA_BLOCK)
    pad = nb * MOBA_BLOCK - S
    kp = jnp.pad(k, ((0, 0), (0, 0), (0, pad), (0, 0))).reshape(B, H, nb, MOBA_BLOCK, hd)
    vp = jnp.pad(v, ((0, 0), (0, 0), (0, pad), (0, 0))).reshape(B, H, nb, MOBA_BLOCK, hd)
    kmean = jnp.mean(kp.astype(jnp.float32), axis=3)
    n_sel = min(MOBA_TOPK, nb - 1)
    scale = hd ** -0.5
    b_idx = jnp.arange(B)[:, None, None, None]
    h_idx = jnp.arange(H)[None, :, None, None]

    def chunk(ci):
        c0 = ci * MOBA_Q_CHUNK
        blk = c0 // MOBA_BLOCK
        qc = lax.dynamic_slice_in_dim(q, c0, MOBA_Q_CHUNK, axis=2).astype(jnp.float32)
        tq = c0 + jnp.arange(MOBA_Q_CHUNK)
        k_own = lax.dynamic_index_in_dim(kp, blk, axis=2, keepdims=False).astype(jnp.float32)
        v_own = lax.dynamic_index_in_dim(vp, blk, axis=2, keepdims=False).astype(jnp.float32)
        kpos = blk * MOBA_BLOCK + jnp.arange(MOBA_BLOCK)
        l_own = jnp.einsum('bhqd,bhld->bhql', qc, k_own) * scale
        l_own = jnp.where(kpos[None, :] <= tq[:, None], l_own, NEG_INF)
        if n_sel == 0:
            p_own = jax.nn.softmax(l_own, axis=-1)
            out = jnp.einsum('bhql,bhld->bhqd', p_own, v_own)
            return out.astype(q.dtype)
        gate = jnp.einsum('bhqd,bhnd->bhqn', qc, kmean)
        gate = jnp.where(jnp.arange(nb) < blk, gate, NEG_INF)
        _, idx = lax.top_k(gate, n_sel)
        sel_ok = idx < blk
        k_sel = kp[b_idx, h_idx, idx].astype(jnp.float32)
        v_sel = vp[b_idx, h_idx, idx].astype(jnp.float32)
        l_sel = jnp.einsum('bhqd,bhqnld->bhqnl', qc, k_sel) * scale
        l_sel = jnp.where(sel_ok[..., None], l_sel, NEG_INF)
        l_sel = l_sel.reshape(B, H, MOBA_Q_CHUNK, n_sel * MOBA_BLOCK)
        p = jax.nn.softmax(jnp.concatenate([l_sel, l_own], axis=-1), axis=-1)
        p_sel = p[..., : n_sel * MOBA_BLOCK].reshape(B, H, MOBA_Q_CHUNK, n_sel, MOBA_BLOCK)
        p_own = p[..., n_sel * MOBA_BLOCK:]
        out = (jnp.einsum('bhqnl,bhqnld->bhqd', p_sel, v_sel)
               + jnp.einsum('bhql,bhld->bhqd', p_own, v_own))
        return out.astype(q.dtype)

    outs = lax.map(chunk, jnp.arange(S // MOBA_Q_CHUNK))
    return outs.transpose(1, 2, 0, 3, 4).reshape(B, H, S, hd)


def stick_breaking_attention(q, k, v):
    B, H, S, hd = q.shape
    kf = k.astype(jnp.float32)
    vf = v.astype(jnp.float32)
    s_pos = jnp.arange(S)
    scale = hd ** -0.5

    def block(bi):
        t0 = bi * SB_Q_BLOCK
        qb = lax.dynamic_slice_in_dim(q, t0, SB_Q_BLOCK, axis=2).astype(jnp.float32)
        tq = t0 + jnp.arange(SB_Q_BLOCK)
        z = jnp.einsum('bhqd,bhsd->bhqs', qb, kf) * scale
        strict = s_pos[None, :] < tq[:, None]
        log_keep = jnp.where(strict, jax.nn.log_sigmoid(-z), 0.0)
        after = lax.cumsum(log_keep, axis=3, reverse=True) - log_keep
        a = jnp.where(strict, jnp.exp(jax.nn.log_sigmoid(z) + after), 0.0)
        return jnp.einsum('bhqs,bhsd->bhqd', a, vf).astype(q.dtype)

    outs = lax.map(block, jnp.arange(S // SB_Q_BLOCK))
    return outs.transpose(1, 2, 0, 3, 4).reshape(B, H, S, hd)


def multiscale_pool(u, w_pool, pool_scale):
    B, S, _ = u.shape
    uf = u.astype(jnp.float32).reshape(B, S, N_POOL_GROUPS, POOL_GROUP_DIM)
    cs = jnp.concatenate([jnp.zeros((B, 1, N_POOL_GROUPS, POOL_GROUP_DIM), jnp.float32),
                          jnp.cumsum(uf, axis=1)], axis=1)
    t = jnp.arange(S)[:, None]
    win = jnp.array(POOL_WINDOWS, dtype=jnp.int32)[None, :]
    lo = jnp.maximum(t + 1 - win, 0)
    g_idx = jnp.arange(N_POOL_GROUPS)[None, :]
    win_sum = cs[:, 1:] - cs[:, lo, g_idx]
    count = (t + 1 - lo).astype(jnp.float32)
    d = win_sum / count[None, :, :, None] - uf
    y = jnp.einsum('bsgc,gcd->bsgd', d, w_pool.astype(jnp.float32))
    y = y.reshape(B, S, GROUP_WIDTH) * pool_scale.astype(jnp.float32)
    return y.astype(u.dtype)


def dilated_attention(q, k, v):
    B, H, S, hd = q.shape
    scale = hd ** -0.5

    def chunk(ci):
        t0 = ci * DIL_Q_CHUNK
        qc = lax.dynamic_slice_in_dim(q, t0, DIL_Q_CHUNK, axis=2).astype(jnp.float32)
        tq = t0 + jnp.arange(DIL_Q_CHUNK)
        lses = []
        outs = []
        for window, dil in DILATED_PATTERNS:
            m = jnp.arange(window // dil + 1)
            pos = tq[:, None] - dil * m[None, :]
            ok = pos >= 0
            posc = jnp.maximum(pos, 0)
            kg = k[:, :, posc].astype(jnp.float32)
            vg = v[:, :, posc].astype(jnp.float32)
            logits = jnp.einsum('bhqd,bhqkd->bhqk', qc, kg) * scale
            logits = jnp.where(ok, logits, NEG_INF)
            lse = jax.nn.logsumexp(logits, axis=-1)
            p = jnp.exp(logits - lse[..., None])
            outs.append(jnp.einsum('bhqk,bhqkd->bhqd', p, vg))
            lses.append(lse)
        alpha = jax.nn.softmax(jnp.stack(lses, axis=0), axis=0)
        out = jnp.einsum('pbhq,pbhqd->bhqd', alpha, jnp.stack(outs, axis=0))
        return out.astype(q.dtype)

    outs = lax.map(chunk, jnp.arange(S // DIL_Q_CHUNK))
    return outs.transpose(1, 2, 0, 3, 4).reshape(B, H, S, hd)


def hybrid_mixer(h, w_in, w_pool, pool_scale, mix_out_norm, w_out):
    B, S, _ = h.shape
    proj = h @ w_in
    parts = jnp.split(proj, [GROUP_WIDTH * i for i in range(1, 10)], axis=-1)
    qa, ka, va, qb, kb, vb, qd, kd, vd, u = parts

    def to_heads(t):
        return t.reshape(B, S, N_HEADS, HEAD_DIM).transpose(0, 2, 1, 3)

    def from_heads(t):
        return t.transpose(0, 2, 1, 3).reshape(B, S, GROUP_WIDTH)

    y_a = from_heads(moba_attention(to_heads(qa), to_heads(ka), to_heads(va)))
    y_b = from_heads(stick_breaking_attention(to_heads(qb), to_heads(kb), to_heads(vb)))
    y_c = multiscale_pool(u, w_pool, pool_scale)
    y_d = from_heads(dilated_attention(to_heads(qd), to_heads(kd), to_heads(vd)))
    y = jnp.stack([y_a, y_b, y_c, y_d], axis=2)
    y = rms_norm(y, mix_out_norm.reshape(N_MIXERS, GROUP_WIDTH)).reshape(B, S, MIX_WIDTH)
    return y @ w_out


def swiglu(h, w_gate, w_up, w_down):
    return (jax.nn.silu(h @ w_gate) * (h @ w_up)) @ w_down


def setup_inputs(seed: int = 0) -> dict:
    key = jax.random.key(seed)
    ks = jax.random.split(key, 13)
    f32 = jnp.float32

    def gain(k, n):
        return 1.0 + 0.05 * jax.random.normal(k, (DEPTH, n), f32)

    x = jax.random.normal(ks[0], (BATCH, SEQ, D_MODEL), f32)
    ln_mix_pre = gain(ks[1], D_MODEL)
    w_in = jax.random.normal(ks[2], (DEPTH, D_MODEL, IN_WIDTH), f32) * D_MODEL ** -0.5
    w_pool = jax.random.normal(ks[3], (DEPTH, N_POOL_GROUPS, POOL_GROUP_DIM, POOL_GROUP_DIM), f32) * POOL_GROUP_DIM ** -0.5
    pool_scale = 1.0 + 0.1 * jax.random.normal(ks[4], (DEPTH, GROUP_WIDTH), f32)
    mix_out_norm = gain(ks[5], MIX_WIDTH)
    w_out = jax.random.normal(ks[6], (DEPTH, MIX_WIDTH, D_MODEL), f32) * MIX_WIDTH ** -0.5
    ln_mix_post = gain(ks[7], D_MODEL)
    ln_ffn_pre = gain(ks[8], D_MODEL)
    w_gate = jax.random.normal(ks[9], (DEPTH, D_MODEL, D_FF), f32) * D_MODEL ** -0.5
    w_up = jax.random.normal(ks[10], (DEPTH, D_MODEL, D_FF), f32) * D_MODEL ** -0.5
    w_down = jax.random.normal(ks[11], (DEPTH, D_FF, D_MODEL), f32) * D_FF ** -0.5
    ln_ffn_post = gain(ks[12], D_MODEL)
    return {"x": x, "ln_mix_pre": ln_mix_pre, "w_in": w_in, "w_pool": w_pool,
            "pool_scale": pool_scale, "mix_out_norm": mix_out_norm, "w_out": w_out,
            "ln_mix_post": ln_mix_post, "ln_ffn_pre": ln_ffn_pre, "w_gate": w_gate,
            "w_up": w_up, "w_down": w_down, "ln_ffn_post": ln_ffn_post}


def reference(x, ln_mix_pre, w_in, w_pool, pool_scale, mix_out_norm, w_out,
              ln_mix_post, ln_ffn_pre, w_gate, w_up, w_down, ln_ffn_post):
    for l in range(DEPTH):
        h = rms_norm(x, ln_mix_pre[l])
        m = hybrid_mixer(h, w_in[l], w_pool[l], pool_scale[l], mix_out_norm[l], w_out[l])
        x = x + rms_norm(m, ln_mix_post[l])
        h = rms_norm(x, ln_ffn_pre[l])
        f = swiglu(h, w_gate[l], w_up[l], w_down[l])
        x = x + rms_norm(f, ln_ffn_post[l])
    return x
```

```python
import contextlib
import numpy as np
import concourse.bass as bass
import concourse.mybir as mybir
from concourse.bass_utils import run_bass_kernel_spmd

F32 = mybir.dt.float32
BF16 = mybir.dt.bfloat16
ALU = mybir.AluOpType
AF = mybir.ActivationFunctionType
AX = mybir.AxisListType

SEM_CAP = 30000
RMS_EPS = 1e-6
MOBA_BLOCK = 256
POOL_WINDOWS = (2, 4, 8, 16)
DILATED_PATTERNS = ((128, 1), (512, 4), (2048, 16))


class Sched:
    ENG = ("pe", "act", "dve", "pool", "sp")

    def __init__(self, nc, stack, n_lanes=8):
        self.nc = nc
        self.stack = stack
        self.engs = {"pe": nc.tensor, "act": nc.scalar, "dve": nc.vector, "pool": nc.gpsimd, "sp": nc.sync}
        self.prog = {}
        self.seen = {e: {} for e in self.ENG}
        self.nsem = 0
        self.pe_sems = set()
        for e in self.ENG:
            self.prog[e] = [self._newsem("p_" + e), 0]
        self.pe_sems.add(self.prog["pe"][0].num)
        self.lanes = {}
        for e in ("sp", "pool"):
            self.lanes[e] = [[self._newsem("l_%s%d" % (e, i)), 0] for i in range(n_lanes)]
        self.lane_rr = {e: 0 for e in self.lanes}
        self.all_sems = {}
        self.last_w = {}
        self.readers = {}
        self.n_inst = {e: 0 for e in self.ENG}

    def _newsem(self, name):
        self.nsem += 1
        return self.stack.enter_context(self.nc.semaphore("%s_%d" % (name, self.nsem)))

    def _emit(self, e, fn):
        self.n_inst[e] += 1
        return fn(self.engs[e])

    def _wait(self, e, tok):
        if tok is None:
            return
        sem, val = tok
        if e == "pe" and sem.num in self.pe_sems:
            return
        if self.seen[e].get(sem.num, 0) >= val:
            return
        self.seen[e][sem.num] = val
        self._emit(e, lambda eng: eng.wait_ge(sem, val))

    def _deps(self, e, reads, writes):
        for k in reads:
            self._wait(e, self.last_w.get(k))
        for k in writes:
            self._wait(e, self.last_w.get(k))
            for t in self.readers.get(k, ()):
                self._wait(e, t)

    def _commit(self, tok, reads, writes):
        self.all_sems[tok[0].num] = tok
        for k in writes:
            self.last_w[k] = tok
            self.readers[k] = []
        for k in reads:
            self.readers.setdefault(k, []).append(tok)

    def _next_tok(self, e):
        p = self.prog[e]
        if p[1] >= SEM_CAP:
            p[0] = self._newsem("p_" + e)
            p[1] = 0
            if e == "pe":
                self.pe_sems.add(p[0].num)
        p[1] += 1
        return (p[0], p[1])

    def op(self, e, fn, reads=(), writes=()):
        self._deps(e, reads, writes)
        tok = self._next_tok(e)
        self._emit(e, lambda eng: fn(eng).then_inc(tok[0], 1))
        self._commit(tok, reads, writes)
        return tok

    def group(self, e, fns, reads=(), writes=()):
        self._deps(e, reads, writes)
        tok = self._next_tok(e)
        for fn in fns[:-1]:
            self._emit(e, fn)
        self._emit(e, lambda eng: fns[-1](eng).then_inc(tok[0], 1))
        self._commit(tok, reads, writes)
        return tok

    def dma(self, e, out, in_, reads=(), writes=()):
        lanes = self.lanes[e]
        i = self.lane_rr[e]
        self.lane_rr[e] = (i + 1) % len(lanes)
        ln = lanes[i]
        if ln[1] >= SEM_CAP // 16:
            ln[0] = self._newsem("l_" + e)
            ln[1] = 0
        if ln[1] > 0:
            self._wait(e, (ln[0], 16 * ln[1]))
        self._deps(e, reads, writes)
        ln[1] += 1
        tok = (ln[0], 16 * ln[1])
        self._emit(e, lambda eng: eng.dma_start(out=out, in_=in_).then_inc(tok[0], 16))
        self._commit(tok, reads, writes)
        return tok

    def barrier(self, engines=None):
        toks = list(self.all_sems.values())
        for e in (engines or self.ENG):
            for tok in toks:
                self._wait(e, tok)

    def finish(self):
        self.barrier(engines=("sp",))


ARENA_BYTES = 200 * 1024


def build(cfg):
    S, D, NH, DFF, TO, L = cfg["S"], cfg["D"], cfg["NH"], cfg["DFF"], cfg["TO"], cfg["L"]
    PGn, PGD, NGW = cfg["PGn"], cfg["PGD"], cfg["NGW"]
    windows = cfg["windows"]
    groups = cfg["groups"]
    debug = cfg.get("debug", False)
    KC = D // 128
    GW = NH * 128
    INW = 10 * GW
    MW = 4 * GW
    KC2 = MW // 128
    TB = S // 128
    FC = DFF // 128
    PC = PGD // 128
    NBM = S // MOBA_BLOCK
    NQC = S // 512
    scale = 128 ** -0.5
    TG = 512
    NTG = TO // TG

    nc = bass.Bass("TRN2", target_bir_lowering=False)

    def din(name, shape, dt=F32):
        return nc.dram_tensor(name, list(shape), dt, kind="ExternalInput").ap()

    skind = "ExternalOutput" if debug else "Internal"

    def dscr(name, shape, dt):
        return nc.dram_tensor(name, list(shape), dt, kind=skind).ap()

    x_seq = din("x_seq", [S, D])
    w_in = din("w_in", [L, D, INW])
    w_out = din("w_out", [L, MW, D])
    w_gate = din("w_gate", [L, D, DFF])
    w_up = din("w_up", [L, D, DFF])
    w_down = din("w_down", [L, DFF, D])
    w_pool = din("w_pool", [L, PGn, PGD, PGD])
    gb_pre = din("gb_pre", [L, 128, D])
    gb_post = din("gb_post", [L, 128, D])
    gb_fpre = din("gb_fpre", [L, 128, D])
    gb_fpost = din("gb_fpost", [L, 128, D])
    gb_mo = din("gb_mo", [L, 128, MW])
    psb = din("psb", [L, 128, GW])
    c_ident = din("c_ident", [128, 128])
    c_tri = din("c_tri", [128, 128])
    c_msd = din("c_msd", [128, S])
    c_msc = din("c_msc", [128, S])
    c_mss = din("c_mss", [128, S])
    c_invc = din("c_invc", [PGn, 128, S])
    c_nm = din("c_nm", [128, NBM, 8])
    out = nc.dram_tensor("out", [TO, D], F32, kind="ExternalOutput").ap()

    qkT_d = dscr("qkT_d", [6, NH, 128, S], BF16)
    v_d = dscr("v_d", [3, S, GW], BF16)
    u_d = dscr("u_d", [GW // 128, 128, S], F32)
    y_d = dscr("y_d", [S, MW], BF16)
    xres_d = dscr("xres_d", [TO, D], F32)

    with contextlib.ExitStack() as st:
        s = Sched(nc, st)

        def SB(name, shape, dt):
            return st.enter_context(nc.sbuf_tensor(name, list(shape), dt))

        ps = [st.enter_context(nc.psum_tensor("ps%d" % i, [128, 512], F32)) for i in range(8)]
        psb16 = [p[:].bitcast(BF16) for p in ps]
        rr = [0]

        def PS():
            i = rr[0]
            rr[0] = (i + 1) % 8
            return i

        evt = [0]

        def EV():
            evt[0] ^= 1
            return "act" if evt[0] else "dve"

        def copy_op(e, out_ap, in_ap, reads, writes):
            if e == "act":
                s.op("act", lambda g: g.copy(out=out_ap, in_=in_ap), reads=reads, writes=writes)
            else:
                s.op(e, lambda g: g.tensor_copy(out=out_ap, in_=in_ap), reads=reads, writes=writes)

        idb = SB("idb", [128, 128], BF16)
        trib = SB("trib", [128, 128], BF16)
        onesb = SB("onesb", [128, 128], BF16)
        epsb = SB("epsb", [128, 1], F32)
        stt = SB("stt", [128, 32], F32)
        arena = SB("arena", [128, ARENA_BYTES // 2], BF16)
        s.dma("pool", idb[:], c_ident, writes=["idb"])
        s.dma("pool", trib[:], c_tri, writes=["trib"])
        s.op("dve", lambda g: g.memset(onesb[:], 1.0), writes=["onesb"])
        s.op("dve", lambda g: g.memset(epsb[:], RMS_EPS), writes=["epsb"])

        class Arena:
            def __init__(self):
                self.off = 0

            def at(self, off, shape, dt):
                n = int(np.prod(shape))
                nb = n * (2 if dt == BF16 else 4)
                assert off % 4 == 0 and off + nb <= ARENA_BYTES, (off, nb)
                if dt == BF16:
                    ap = arena[:, off // 2: off // 2 + n]
                else:
                    ap = arena[:, off // 2: off // 2 + 2 * n].bitcast(F32)
                if len(shape) == 2:
                    ap = ap.rearrange("p (a b) -> p a b", a=shape[0])
                return ap, off + ((nb + 31) // 32) * 32

            def reset(self, off=0):
                self.off = off

            def alloc(self, shape, dt):
                ap, self.off = self.at(self.off, shape, dt)
                return ap

        A = Arena()

        def rstd(src_ap, junk_ap, col, denom, rkeys, jkeys):
            s.op("act", lambda g: g.activation(out=junk_ap, in_=src_ap, func=AF.Square, accum_out=stt[:, col:col + 1]),
                 reads=rkeys, writes=list(jkeys) + [("stt", col)])
            s.op("act", lambda g: g.activation(out=stt[:, col + 1:col + 2], in_=stt[:, col:col + 1], func=AF.Sqrt,
                                               scale=1.0 / denom, bias=epsb[:]),
                 reads=[("stt", col), "epsb"], writes=[("stt", col + 1)])
            s.op("dve", lambda g: g.reciprocal(out=stt[:, col + 2:col + 3], in_=stt[:, col + 1:col + 2]),
                 reads=[("stt", col + 1)], writes=[("stt", col + 2)])
            return stt[:, col + 2:col + 3], ("stt", col + 2)

        def transposeT(src, skeys, nchunk, dst, dkey, t_off):
            for g0 in range(0, nchunk, 8):
                n = min(8, nchunk - g0)
                b = PS()
                fns = [(lambda e, j=j: e.transpose(out=psb16[b][:, j * 128:(j + 1) * 128],
                                                   in_=src[:, (g0 + j) * 128:(g0 + j + 1) * 128], identity=idb[:]))
                       for j in range(n)]
                s.group("pe", fns, reads=list(skeys) + ["idb"], writes=[("ps", b)])
                copy_op(EV(), dst[:, g0:g0 + n, t_off:t_off + 128],
                        psb16[b][:, 0:n * 128].rearrange("p (k t) -> p k t", k=n), [("ps", b)], [dkey])

        def mm_group(b, ncols, pairs, reads):
            n = len(pairs)
            fns = [(lambda e, i=i: e.matmul(ps[b][:, 0:ncols], lhsT=pairs[i][0], rhs=pairs[i][1],
                                            start=(i == 0), stop=(i == n - 1))) for i in range(n)]
            s.group("pe", fns, reads=reads, writes=[("ps", b)])

        for l in range(L):
            xsrc = x_seq if l == 0 else xres_d
            A.reset()
            hT = A.alloc([KC, S], BF16)
            p_mark = A.off
            gb = A.alloc([D], F32)
            xt = A.alloc([D], F32)
            xb = A.alloc([D], BF16)
            s.dma("sp", gb, gb_pre[l], writes=["gb"])
            for tb in range(TB):
                s.dma("sp", xt, xsrc[tb * 128:(tb + 1) * 128, :], writes=["xt"])
                r_ap, rk = rstd(xt, xb, 0, D, ["xt"], ["xb"])
                s.op("dve", lambda g: g.scalar_tensor_tensor(out=xb, in0=xt, scalar=r_ap, in1=gb,
                                                             op0=ALU.mult, op1=ALU.mult),
                     reads=["xt", rk, "gb"], writes=["xb"])
                transposeT(xb, ["xb"], KC, hT, ("hT", tb), tb * 128)
            s.barrier()
            A.reset(p_mark)
            CT = 256
            wbufs = [A.alloc([KC, CT], BF16) for _ in range(3)]
            stg = [A.alloc([512], BF16) for _ in range(2)]
            stgf = [A.alloc([512], F32) for _ in range(2)]
            si = 0
            for ct in range(INW // CT):
                wb = wbufs[ct % 3]
                wk = ("w2", ct % 3)
                s.dma("pool", wb, w_in[l, :, ct * CT:(ct + 1) * CT].rearrange("(k p) n -> p k n", p=128), writes=[wk])
                seg = (ct * CT) // GW
                off = ct * CT - seg * GW
                if seg in (2, 5, 8):
                    m = {2: 0, 5: 1, 8: 2}[seg]
                    for tb in range(TB):
                        b = PS()
                        mm_group(b, CT, [(hT[:, k, tb * 128:(tb + 1) * 128], wb[:, k, :]) for k in range(KC)],
                                 [("hT", tb), wk])
                        si ^= 1
                        copy_op(EV(), stg[si][:, 0:CT], ps[b][:, 0:CT], [("ps", b)], [("stg", si)])
                        s.dma("sp", v_d[m, tb * 128:(tb + 1) * 128, off:off + CT], stg[si][:, 0:CT], reads=[("stg", si)])
                else:
                    for sub in range(CT // 128):
                        h = (off + sub * 128) // 128
                        for tc in range(NQC):
                            b = PS()
                            mm_group(b, 512, [(wb[:, k, sub * 128:(sub + 1) * 128], hT[:, k, tc * 512:(tc + 1) * 512])
                                              for k in range(KC)],
                                     [("hT", tc * 4 + i) for i in range(4)] + [wk])
                            si ^= 1
                            if seg == 9:
                                copy_op(EV(), stgf[si], ps[b][:], [("ps", b)], [("stgf", si)])
                                s.dma("sp", u_d[h, :, tc * 512:(tc + 1) * 512], stgf[si], reads=[("stgf", si)])
                            else:
                                idx = {0: 0, 1: 1, 3: 2, 4: 3, 6: 4, 7: 5}[seg]
                                copy_op(EV(), stg[si], ps[b][:], [("ps", b)], [("stg", si)])
                                s.dma("sp", qkT_d[idx, h, :, tc * 512:(tc + 1) * 512], stg[si], reads=[("stg", si)])
            s.barrier()

            A.reset()
            ystage = A.alloc([TB, GW], BF16)
            msd = A.alloc([S], BF16)
            msc = A.alloc([S], BF16)
            mss = A.alloc([S], BF16)
            s.dma("pool", msd, c_msd, writes=["msd"])
            s.dma("pool", msc, c_msc, writes=["msc"])
            s.dma("pool", mss, c_mss, writes=["mss"])
            qT = [A.alloc([S], BF16) for _ in range(2)]
            kT = [A.alloc([S], BF16) for _ in range(2)]
            vx = [A.alloc([TB, 130], BF16) for _ in range(2)]
            for i in range(2):
                s.op("dve", lambda g, i=i: g.memset(vx[i][:, :, 128:129], 1.0), writes=[("vx1", i)])
            Eall = A.alloc([TB, 512], BF16)
            acc = A.alloc([4, 129], F32)
            nm = A.alloc([NBM, 8], F32)
            s.dma("sp", nm, c_nm, writes=["nm"])
            km = A.alloc([NBM], F32)
            kmh = A.alloc([NBM], BF16)
            kml = A.alloc([NBM], BF16)
            g8 = A.alloc([8], F32)
            top8 = A.alloc([8], F32)
            sel = A.alloc([TB, 8], F32)
            s.op("dve", lambda g: g.memset(g8, -1e30), writes=["g8"])
            t_e = A.alloc([512], F32)
            t_sp = A.alloc([512], F32)
            t_1 = A.alloc([512], F32)
            t_3 = A.alloc([512], F32)
            spm = A.alloc([512], BF16)
            Rb = A.alloc([512], F32)
            p3_mark = A.off
            hcount = [0]

            def load_head(m, h):
                i = hcount[0] % 2
                hcount[0] += 1
                s.dma("sp", qT[i], qkT_d[2 * m, h], writes=[("qT", i)])
                s.dma("sp", kT[i], qkT_d[2 * m + 1, h], writes=[("kT", i)])
                s.dma("sp", vx[i][:, :, 0:128],
                      v_d[m, :, h * 128:(h + 1) * 128].rearrange("(tb p) d -> p tb d", p=128), writes=[("vx", i)])
                return i

            def finalize(src_ap, den_ap, rkeys, qb, h):
                s.op("dve", lambda g: g.reciprocal(out=stt[:, 8:9], in_=den_ap), reads=rkeys, writes=[("stt", 8)])
                s.op("dve", lambda g: g.tensor_scalar(out=ystage[:, qb, h * 128:(h + 1) * 128], in0=src_ap,
                                                      scalar1=stt[:, 8:9], scalar2=None, op0=ALU.mult),
                     reads=list(rkeys) + [("stt", 8)], writes=[("ys", qb)])

            def store_mixer(mi):
                s.dma("sp", y_d[:, mi * GW:(mi + 1) * GW].rearrange("(tb p) c -> p tb c", p=128), ystage,
                      reads=[("ys", qb) for qb in range(TB)])

            mtog = [0]

            def mask_eng():
                mtog[0] ^= 1
                return "pool" if mtog[0] else "dve"

            for h in range(NH):
                i = load_head(0, h)
                hk = [("qT", i), ("kT", i), ("vx", i), ("vx1", i)]
                s.op("dve", lambda g: g.tensor_reduce(out=km, in_=kT[i].rearrange("p (n k) -> p n k", k=MOBA_BLOCK),
                                                      axis=AX.X, op=ALU.add), reads=[("kT", i)], writes=["km"])
                s.op("dve", lambda g: g.tensor_scalar(out=kmh, in0=km, scalar1=1.0 / MOBA_BLOCK, scalar2=None,
                                                      op0=ALU.mult), reads=["km"], writes=["kmh"])
                s.op("dve", lambda g: g.scalar_tensor_tensor(out=kml, in0=km, scalar=1.0 / MOBA_BLOCK, in1=kmh,
                                                             op0=ALU.mult, op1=ALU.subtract),
                     reads=["km", "kmh"], writes=["kml"])
                for tb in range(TB):
                    b = PS()
                    mm_group(b, NBM, [(qT[i][:, tb * 128:(tb + 1) * 128], kmh),
                                      (qT[i][:, tb * 128:(tb + 1) * 128], kml)], [("qT", i), "kmh", "kml"])
                    blk = tb // 2
                    s.op("dve", lambda g: g.tensor_tensor(out=g8[:, 0:NBM], in0=ps[b][:, 0:NBM], in1=nm[:, blk, 0:NBM],
                                                          op=ALU.add), reads=[("ps", b), "nm"], writes=["g8"])
                    s.op("dve", lambda g: g.max(out=top8, in_=g8), reads=["g8"], writes=["top8"])
                    s.op("dve", lambda g: g.tensor_scalar(out=sel[:, tb, :], in0=g8, scalar1=top8[:, 2:3], scalar2=None,
                                                          op0=ALU.is_ge), reads=["g8", "top8"], writes=[("sel", tb)])
                for qc in range(NQC):
                    for n in range(2 * qc + 2):
                        q0 = max(qc * 512, n * 256)
                        W = (qc + 1) * 512 - q0
                        c0 = q0 - qc * 512
                        own = (n >= 2 * qc)
                        for j in range(2):
                            kb = 2 * n + j
                            b = PS()
                            mm_group(b, W, [(kT[i][:, kb * 128:(kb + 1) * 128], qT[i][:, q0:q0 + W])], hk)
                            s.op("act", lambda g: g.activation(out=Eall[:, j, c0:512], in_=ps[b][:, 0:W], func=AF.Exp,
                                                               scale=scale), reads=[("ps", b)], writes=[("E", j)])
                            if own and j == 0:
                                s.op(mask_eng(), lambda g: g.tensor_tensor(out=Eall[:, 0, c0:c0 + 256], in0=Eall[:, 0, c0:c0 + 256],
                                                                           in1=msc[:, 0:256], op=ALU.mult),
                                     reads=[("E", 0), "msc"], writes=[("E", 0)])
                            elif own:
                                s.op("pool", lambda g: g.memset(Eall[:, 1, c0:c0 + 128], 0.0), reads=[("E", 1)], writes=[("E", 1)])
                                s.op(mask_eng(), lambda g: g.tensor_tensor(out=Eall[:, 1, c0 + 128:c0 + 256],
                                                                           in0=Eall[:, 1, c0 + 128:c0 + 256],
                                                                           in1=msc[:, 0:128], op=ALU.mult),
                                     reads=[("E", 1), "msc"], writes=[("E", 1)])
                        for sub in range(c0 // 128, 4):
                            qb = qc * 4 + sub
                            b = PS()
                            mm_group(b, 129, [(Eall[:, 0, sub * 128:(sub + 1) * 128], vx[i][:, 2 * n, 0:129]),
                                              (Eall[:, 1, sub * 128:(sub + 1) * 128], vx[i][:, 2 * n + 1, 0:129])],
                                     [("E", 0), ("E", 1)] + hk)
                            ak = ("acc", sub)
                            if n == 0:
                                if qb // 2 == 0:
                                    s.op("dve", lambda g: g.tensor_copy(out=acc[:, sub, :], in_=ps[b][:, 0:129]),
                                         reads=[("ps", b)], writes=[ak])
                                else:
                                    s.op("dve", lambda g: g.tensor_scalar(out=acc[:, sub, :], in0=ps[b][:, 0:129],
                                                                          scalar1=sel[:, qb, 0:1], scalar2=None, op0=ALU.mult),
                                         reads=[("ps", b), ("sel", qb)], writes=[ak])
                            elif n == qb // 2:
                                s.op("dve", lambda g: g.tensor_tensor(out=acc[:, sub, :], in0=ps[b][:, 0:129],
                                                                      in1=acc[:, sub, :], op=ALU.add),
                                     reads=[("ps", b), ak], writes=[ak])
                            else:
                                s.op("dve", lambda g: g.scalar_tensor_tensor(out=acc[:, sub, :], in0=ps[b][:, 0:129],
                                                                             scalar=sel[:, qb, n:n + 1], in1=acc[:, sub, :],
                                                                             op0=ALU.mult, op1=ALU.add),
                                     reads=[("ps", b), ak, ("sel", qb)], writes=[ak])
                    for sub in range(4):
                        finalize(acc[:, sub, 0:128], acc[:, sub, 128:129], [("acc", sub)], qc * 4 + sub, h)
            store_mixer(0)

            for h in range(NH):
                i = load_head(1, h)
                hk = [("qT", i), ("kT", i), ("vx", i)]
                for qc in range(NQC):
                    s.op("dve", lambda g: g.memset(Rb, 0.0), writes=["Rb"])
                    nkb = (qc + 1) * 4
                    for kb in reversed(range(nkb)):
                        q0 = max(qc * 512, kb * 128)
                        W = (qc + 1) * 512 - q0
                        c0 = q0 - qc * 512
                        diag = (kb * 128 >= qc * 512)
                        bz = PS()
                        mm_group(bz, W, [(kT[i][:, kb * 128:(kb + 1) * 128], qT[i][:, q0:q0 + W])], hk)
                        s.op("act", lambda g: g.activation(out=t_e[:, 0:W], in_=ps[bz][:, 0:W], func=AF.Exp, scale=scale),
                             reads=[("ps", bz)], writes=["t_e"])
                        s.op("act", lambda g: g.activation(out=t_sp[:, 0:W], in_=t_e[:, 0:W], func=AF.Ln, bias=1.0),
                             reads=["t_e"], writes=["t_sp"])
                        if diag:
                            s.op("dve", lambda g: g.tensor_tensor(out=spm[:, 0:128], in0=t_sp[:, 0:128], in1=mss[:, 0:128],
                                                                  op=ALU.mult), reads=["t_sp", "mss"], writes=["spm"])
                            if W > 128:
                                s.op("pool", lambda g: g.tensor_copy(out=spm[:, 128:W], in_=t_sp[:, 128:W]),
                                     reads=["t_sp"], writes=["spm2"])
                        else:
                            s.op("pool", lambda g: g.tensor_copy(out=spm[:, 0:W], in_=t_sp[:, 0:W]),
                                 reads=["t_sp"], writes=["spm", "spm2"])
                        bc = PS()
                        mm_group(bc, W, [(trib[:], spm[:, 0:W])], ["trib", "spm", "spm2"])
                        br = PS()
                        mm_group(br, W, [(onesb[:], spm[:, 0:W])], ["onesb", "spm", "spm2"])
                        s.op("dve", lambda g: g.tensor_tensor(out=t_1[:, 0:W], in0=ps[bc][:, 0:W], in1=Rb[:, c0:512],
                                                              op=ALU.add), reads=[("ps", bc), "Rb"], writes=["t_1"])
                        s.op("pool", lambda g: g.tensor_tensor(out=t_1[:, 0:W], in0=t_1[:, 0:W], in1=t_sp[:, 0:W],
                                                               op=ALU.add), reads=["t_1", "t_sp"], writes=["t_1"])
                        s.op("dve", lambda g: g.scalar_tensor_tensor(out=t_3[:, 0:W], in0=ps[bz][:, 0:W], scalar=scale,
                                                                     in1=t_1[:, 0:W], op0=ALU.mult, op1=ALU.subtract),
                             reads=[("ps", bz), "t_1"], writes=["t_3"])
                        s.op("act", lambda g: g.activation(out=Eall[:, kb, c0:512], in_=t_3[:, 0:W], func=AF.Exp),
                             reads=["t_3"], writes=[("E", kb)])
                        if diag:
                            s.op("pool", lambda g: g.tensor_tensor(out=Eall[:, kb, c0:c0 + 128], in0=Eall[:, kb, c0:c0 + 128],
                                                                   in1=mss[:, 0:128], op=ALU.mult),
                                 reads=[("E", kb), "mss"], writes=[("E", kb)])
                        s.op("dve", lambda g: g.tensor_tensor(out=Rb[:, c0:512], in0=ps[br][:, 0:W], in1=Rb[:, c0:512],
                                                              op=ALU.add), reads=[("ps", br), "Rb"], writes=["Rb"])
                    for sub in range(4):
                        qb = qc * 4 + sub
                        b = PS()
                        mm_group(b, 128, [(Eall[:, kb, sub * 128:(sub + 1) * 128], vx[i][:, kb, 0:128]) for kb in range(qb + 1)],
                                 [("E", kb) for kb in range(qb + 1)] + hk)
                        copy_op(EV(), ystage[:, qb, h * 128:(h + 1) * 128], ps[b][:, 0:128], [("ps", b)], [("ys", qb)])
            store_mixer(1)

            for h in range(NH):
                i = load_head(2, h)
                hk = [("qT", i), ("kT", i), ("vx", i), ("vx1", i)]
                for qc in range(NQC):
                    nkb = (qc + 1) * 4
                    for kb in range(nkb):
                        q0 = max(qc * 512, kb * 128)
                        W = (qc + 1) * 512 - q0
                        c0 = q0 - qc * 512
                        b = PS()
                        mm_group(b, W, [(kT[i][:, kb * 128:(kb + 1) * 128], qT[i][:, q0:q0 + W])], hk)
                        s.op("act", lambda g: g.activation(out=Eall[:, kb, c0:512], in_=ps[b][:, 0:W], func=AF.Exp, scale=scale),
                             reads=[("ps", b)], writes=[("E", kb)])
                        mo = q0 - 128 * kb
                        s.op(mask_eng(), lambda g: g.tensor_tensor(out=Eall[:, kb, c0:512], in0=Eall[:, kb, c0:512],
                                                                   in1=msd[:, mo:mo + W], op=ALU.mult),
                             reads=[("E", kb), "msd"], writes=[("E", kb)])
                    for sub in range(4):
                        qb = qc * 4 + sub
                        b = PS()
                        mm_group(b, 129, [(Eall[:, kb, sub * 128:(sub + 1) * 128], vx[i][:, kb, 0:129]) for kb in range(qb + 1)],
                                 [("E", kb) for kb in range(qb + 1)] + hk)
                        finalize(ps[b][:, 0:128], ps[b][:, 128:129], [("ps", b)], qb, h)
            store_mixer(3)

            A.reset(p3_mark)
            up = A.alloc([16 + S], F32)
            pa = A.alloc([16 + S], F32)
            pb = A.alloc([16 + S], F32)
            invt = A.alloc([S], F32)
            tmpf = A.alloc([S], F32)
            dT = A.alloc([PC, S], BF16)
            wp = A.alloc([PC, PGD], BF16)
            pst = A.alloc([GW], F32)
            s.dma("sp", pst, psb[l], writes=["pst"])
            for t_, k_ in ((up, "up"), (pa, "pa"), (pb, "pb")):
                s.op("dve", lambda g, t_=t_: g.memset(t_[:, 0:16], 0.0), writes=[k_ + "z"])
            for gi in range(PGn):
                w = windows[gi]
                s.dma("pool", wp, w_pool[l, gi].rearrange("(c p) d -> p c d", p=128), writes=["wp"])
                s.dma("sp", invt, c_invc[gi], writes=["invt"])
                for c in range(PC):
                    ch = gi * PC + c
                    s.dma("sp", up[:, 16:16 + S], u_d[ch], writes=["up"])
                    cur, ck = up, "up"
                    sh = 1
                    while sh < w:
                        nxt, nk = (pa, "pa") if cur is not pa else (pb, "pb")
                        s.op("dve", lambda g, cur=cur, nxt=nxt, sh=sh: g.tensor_tensor(
                            out=nxt[:, 16:16 + S], in0=cur[:, 16:16 + S], in1=cur[:, 16 - sh:16 - sh + S], op=ALU.add),
                             reads=[ck, ck + "z"], writes=[nk])
                        cur, ck = nxt, nk
                        sh *= 2
                    s.op("pool", lambda g, cur=cur: g.tensor_tensor(out=tmpf, in0=cur[:, 16:16 + S], in1=invt,
                                                                    op=ALU.mult), reads=[ck, "invt"], writes=["tmpf"])
                    s.op("dve", lambda g: g.tensor_tensor(out=dT[:, c, :], in0=tmpf, in1=up[:, 16:16 + S],
                                                          op=ALU.subtract), reads=["tmpf", "up"], writes=[("dT", c)])
                for tb in range(TB):
                    b = PS()
                    mm_group(b, PGD, [(dT[:, c, tb * 128:(tb + 1) * 128], wp[:, c, :]) for c in range(PC)],
                             [("dT", c) for c in range(PC)] + ["wp"])
                    s.op("dve", lambda g: g.tensor_tensor(out=ystage[:, tb, gi * PGD:(gi + 1) * PGD], in0=ps[b][:, 0:PGD],
                                                          in1=pst[:, gi * PGD:(gi + 1) * PGD], op=ALU.mult),
                         reads=[("ps", b), "pst"], writes=[("ys", tb)])
            store_mixer(2)
            s.barrier()

            dst = out if l == L - 1 else xres_d
            LOW = 0
            MID1 = 32 * 1024
            MID2 = 64 * 1024
            MSB_OFF = ARENA_BYTES - 4 * D * 4
            ACT_OFF = ARENA_BYTES - ((FC * TG * 2 + 31) // 32) * 32
            assert KC2 * TG * 2 <= MID1 and KC * TG * 2 <= MID1 and 4 * D * 4 <= MID2
            for tg in range(NTG):
                yT, _ = A.at(LOW, [KC2, TG], BF16)
                A.reset(MID1)
                gbm = A.alloc([MW], F32)
                yt = A.alloc([MW], BF16)
                yb = A.alloc([MW], BF16)
                assert A.off <= MSB_OFF
                s.dma("sp", gbm, gb_mo[l], writes=["gbm"])
                for tb in range(4):
                    T0 = tg * TG + tb * 128
                    s.dma("sp", yt, y_d[T0:T0 + 128, :], writes=["yt"])
                    for gi, ranges in enumerate(groups):
                        for ri, (c0, cl) in enumerate(ranges):
                            col = 12 + ri
                            s.op("act", lambda g: g.activation(out=yb[:, c0:c0 + cl], in_=yt[:, c0:c0 + cl],
                                                               func=AF.Square, accum_out=stt[:, col:col + 1]),
                                 reads=["yt"], writes=[("yb", gi), ("stt", col)])
                        if len(ranges) == 2:
                            s.op("dve", lambda g: g.tensor_tensor(out=stt[:, 12:13], in0=stt[:, 12:13], in1=stt[:, 13:14],
                                                                  op=ALU.add),
                                 reads=[("stt", 12), ("stt", 13)], writes=[("stt", 12)])
                        s.op("act", lambda g: g.activation(out=stt[:, 14:15], in_=stt[:, 12:13], func=AF.Sqrt,
                                                           scale=1.0 / NGW, bias=epsb[:]),
                             reads=[("stt", 12), "epsb"], writes=[("stt", 14)])
                        s.op("dve", lambda g: g.reciprocal(out=stt[:, 16 + gi:17 + gi], in_=stt[:, 14:15]),
                             reads=[("stt", 14)], writes=[("stt", 16 + gi)])
                        for (c0, cl) in ranges:
                            s.op("dve", lambda g: g.scalar_tensor_tensor(out=yb[:, c0:c0 + cl], in0=yt[:, c0:c0 + cl],
                                                                         scalar=stt[:, 16 + gi:17 + gi],
                                                                         in1=gbm[:, c0:c0 + cl], op0=ALU.mult, op1=ALU.mult),
                                 reads=["yt", ("stt", 16 + gi), "gbm"], writes=[("yb", gi)])
                    transposeT(yb, [("yb", gi) for gi in range(len(groups))], KC2, yT, ("yT", tb), tb * 128)
                s.barrier()
                msb, _ = A.at(MSB_OFF, [4, D], F32)
                A.reset(MID1)
                wobuf = [A.alloc([KC2, 512], BF16) for _ in range(2)]
                assert A.off <= MSB_OFF
                for nt in range(D // 512):
                    wb = wobuf[nt % 2]
                    wk = ("wo", nt % 2)
                    s.dma("pool", wb, w_out[l, :, nt * 512:(nt + 1) * 512].rearrange("(k p) n -> p k n", p=128), writes=[wk])
                    for tb in range(4):
                        b = PS()
                        mm_group(b, 512, [(yT[:, k, tb * 128:(tb + 1) * 128], wb[:, k, :]) for k in range(KC2)],
                                 [("yT", tb), wk])
                        copy_op(EV(), msb[:, tb, nt * 512:(nt + 1) * 512], ps[b][:], [("ps", b)], [("msb", tb)])
                s.barrier()
                h2T, _ = A.at(LOW, [KC, TG], BF16)
                A.reset(MID1)
                gb1 = A.alloc([D], F32)
                gb2 = A.alloc([D], F32)
                xt = A.alloc([D], F32)
                xb = A.alloc([D], BF16)
                assert A.off <= MSB_OFF
                s.dma("sp", gb1, gb_post[l], writes=["gb1"])
                s.dma("sp", gb2, gb_fpre[l], writes=["gb2"])
                for tb in range(4):
                    T0 = tg * TG + tb * 128
                    s.dma("sp", xt, xsrc[T0:T0 + 128, :], writes=["xt"])
                    r_ap, rk = rstd(msb[:, tb, :], xb, 0, D, [("msb", tb)], ["xb"])
                    s.op("dve", lambda g: g.scalar_tensor_tensor(out=msb[:, tb, :], in0=msb[:, tb, :], scalar=r_ap,
                                                                 in1=gb1, op0=ALU.mult, op1=ALU.mult),
                         reads=[("msb", tb), rk, "gb1"], writes=[("msb", tb)])
                    s.op("pool", lambda g: g.tensor_tensor(out=xt, in0=xt, in1=msb[:, tb, :], op=ALU.add),
                         reads=["xt", ("msb", tb)], writes=["xt"])
                    s.dma("sp", xres_d[T0:T0 + 128, :], xt, reads=["xt"])
                    r_ap, rk = rstd(xt, xb, 4, D, ["xt"], ["xb"])
                    s.op("dve", lambda g: g.scalar_tensor_tensor(out=xb, in0=xt, scalar=r_ap, in1=gb2,
                                                                 op0=ALU.mult, op1=ALU.mult),
                         reads=["xt", rk, "gb2"], writes=["xb"])
                    transposeT(xb, ["xb"], KC, h2T, ("h2T", tb), tb * 128)
                s.barrier()
                actT, _ = A.at(ACT_OFF, [FC, TG], BF16)
                A.reset(MID1)
                CT2 = 256
                wgu = [A.alloc([KC, CT2], BF16) for _ in range(3)]
                sg = A.alloc([2, TG], F32)
                assert A.off <= ACT_OFF
                wi = 0
                h2keys = [("h2T", tb) for tb in range(4)]
                for j in range(DFF // CT2):
                    wg, wgk = wgu[wi % 3], ("wgu", wi % 3)
                    wi += 1
                    s.dma("pool", wg, w_gate[l, :, j * CT2:(j + 1) * CT2].rearrange("(k p) n -> p k n", p=128), writes=[wgk])
                    wu, wuk = wgu[wi % 3], ("wgu", wi % 3)
                    wi += 1
                    s.dma("pool", wu, w_up[l, :, j * CT2:(j + 1) * CT2].rearrange("(k p) n -> p k n", p=128), writes=[wuk])
                    for c in range(2):
                        b = PS()
                        mm_group(b, TG, [(wg[:, k, c * 128:(c + 1) * 128], h2T[:, k, :]) for k in range(KC)], h2keys + [wgk])
                        s.op("act", lambda g: g.activation(out=sg[:, c, :], in_=ps[b][:], func=AF.Silu),
                             reads=[("ps", b)], writes=[("sg", c)])
                    for c in range(2):
                        b = PS()
                        mm_group(b, TG, [(wu[:, k, c * 128:(c + 1) * 128], h2T[:, k, :]) for k in range(KC)], h2keys + [wuk])
                        s.op("dve", lambda g: g.tensor_tensor(out=actT[:, 2 * j + c, :], in0=ps[b][:], in1=sg[:, c, :],
                                                              op=ALU.mult),
                             reads=[("ps", b), ("sg", c)], writes=[("actT", 2 * j + c)])
                s.barrier()
                fsb, _ = A.at(LOW, [4, D], F32)
                A.reset(MID2)
                FG = 8
                wdb = [A.alloc([FG, 512], BF16) for _ in range(3)]
                assert A.off <= ACT_OFF
                nfg = (FC + FG - 1) // FG
                wdi = 0
                for nt in range(D // 512):
                    banks = [PS() for _ in range(4)]
                    for fg in range(nfg):
                        f0 = fg * FG
                        nf = min(FG, FC - f0)
                        wd, wdk = wdb[wdi % 3], ("wd", wdi % 3)
                        wdi += 1
                        s.dma("pool", wd[:, 0:nf, :],
                              w_down[l, f0 * 128:(f0 + nf) * 128, nt * 512:(nt + 1) * 512].rearrange("(c p) n -> p c n", p=128),
                              writes=[wdk])
                        for tb in range(4):
                            fns = [(lambda e, fc=fc, tb=tb, wd=wd: e.matmul(ps[banks[tb]][:], lhsT=actT[:, fc, tb * 128:(tb + 1) * 128],
                                                                            rhs=wd[:, fc - f0, :], start=(fc == 0), stop=(fc == FC - 1)))
                                   for fc in range(f0, f0 + nf)]
                            s.group("pe", fns, reads=[("actT", fc) for fc in range(f0, f0 + nf)] + [wdk],
                                    writes=[("ps", banks[tb])])
                    for tb in range(4):
                        copy_op(EV(), fsb[:, tb, nt * 512:(nt + 1) * 512], ps[banks[tb]][:], [("ps", banks[tb])], [("fsb", tb)])
                s.barrier()
                A.reset(MID2)
                gb1 = A.alloc([D], F32)
                xt = A.alloc([D], F32)
                xb = A.alloc([D], BF16)
                s.dma("sp", gb1, gb_fpost[l], writes=["gb1"])
                for tb in range(4):
                    T0 = tg * TG + tb * 128
                    s.dma("sp", xt, xres_d[T0:T0 + 128, :], writes=["xt"])
                    r_ap, rk = rstd(fsb[:, tb, :], xb, 0, D, [("fsb", tb)], ["xb"])
                    s.op("dve", lambda g: g.scalar_tensor_tensor(out=fsb[:, tb, :], in0=fsb[:, tb, :], scalar=r_ap,
                                                                 in1=gb1, op0=ALU.mult, op1=ALU.mult),
                         reads=[("fsb", tb), rk, "gb1"], writes=[("fsb", tb)])
                    s.op("pool", lambda g: g.tensor_tensor(out=xt, in0=xt, in1=fsb[:, tb, :], op=ALU.add),
                         reads=["xt", ("fsb", tb)], writes=["xt"])
                    s.dma("sp", dst[T0:T0 + 128, :], xt, reads=["xt"])
                s.barrier()
        s.finish()
    print("instructions:", s.n_inst, "sems:", s.nsem)
    return nc


def make_consts(S, windows):
    kin = np.arange(128)[:, None]
    j = np.arange(S)[None, :]
    delta = j - kin
    msd = np.zeros((128, S), np.float32)
    for window, dil in DILATED_PATTERNS:
        msd += ((delta >= 0) & (delta <= window) & (delta % dil == 0)).astype(np.float32)
    msc = (delta >= 0).astype(np.float32)
    mss = (delta > 0).astype(np.float32)
    t = np.arange(S)
    invc = np.stack([np.broadcast_to(1.0 / np.minimum(w, t + 1).astype(np.float32), (128, S)) for w in windows]).astype(np.float32)
    nbm = S // MOBA_BLOCK
    nm = np.zeros((128, nbm, 8), np.float32)
    for blk in range(nbm):
        for n in range(8):
            if n >= blk:
                nm[:, blk, n] = -1e30
    jj = np.arange(128)[:, None]
    ss = np.arange(128)[None, :]
    tri = (jj > ss).astype(np.float32)
    return {"c_ident": np.eye(128, dtype=np.float32), "c_tri": tri, "c_msd": msd, "c_msc": msc, "c_mss": mss,
            "c_invc": np.ascontiguousarray(invc), "c_nm": nm}


def bcast128(v):
    return np.ascontiguousarray(np.broadcast_to(v[:, None, :], (v.shape[0], 128, v.shape[1]))).astype(np.float32)


_NC_CACHE = {}


def kernel(x, ln_mix_pre, w_in, w_pool, pool_scale, mix_out_norm, w_out, ln_mix_post, ln_ffn_pre,
           w_gate, w_up, w_down, ln_ffn_post):
    x = np.asarray(x, np.float32)
    B, S, D = x.shape
    L = w_in.shape[0]
    DFF = w_gate.shape[2]
    GW = D // 4
    NH = GW // 128
    cfg = dict(S=S, D=D, NH=NH, DFF=DFF, TO=S, L=L, PGn=4, PGD=GW // 4, NGW=GW, windows=list(POOL_WINDOWS),
               groups=[[(i * GW, GW)] for i in range(4)])
    key = (S, D, NH, DFF, L)
    if key not in _NC_CACHE:
        _NC_CACHE[key] = build(cfg)
    nc = _NC_CACHE[key]
    common = {
        "w_in": np.ascontiguousarray(w_in, np.float32), "w_out": np.ascontiguousarray(w_out, np.float32),
        "w_gate": np.ascontiguousarray(w_gate, np.float32), "w_up": np.ascontiguousarray(w_up, np.float32),
        "w_down": np.ascontiguousarray(w_down, np.float32), "w_pool": np.ascontiguousarray(w_pool, np.float32),
        "gb_pre": bcast128(np.asarray(ln_mix_pre)), "gb_post": bcast128(np.asarray(ln_mix_post)),
        "gb_fpre": bcast128(np.asarray(ln_ffn_pre)), "gb_fpost": bcast128(np.asarray(ln_ffn_post)),
        "gb_mo": bcast128(np.asarray(mix_out_norm)), "psb": bcast128(np.asarray(pool_scale)),
    }
    common.update(make_consts(S, cfg["windows"]))
    n_cores = B
    in_maps = []
    for c in range(n_cores):
        m = dict(common)
        m["x_seq"] = np.ascontiguousarray(x[c])
        in_maps.append(m)
    res = run_bass_kernel_spmd(nc, in_maps, core_ids=list(range(n_cores)))
    return np.stack([res.results[c]["out"] for c in range(n_cores)], axis=0).astype(np.float32)
```

```python
import contextlib
import numpy as np
import concourse.bass as bass
import concourse.mybir as mybir
from concourse.bass_utils import run_bass_kernel_spmd

F32 = mybir.dt.float32
BF16 = mybir.dt.bfloat16
ALU = mybir.AluOpType
AF = mybir.ActivationFunctionType
AX = mybir.AxisListType

SEM_CAP = 30000
RMS_EPS = 1e-6
MOBA_BLOCK = 256
POOL_WINDOWS = (2, 4, 8, 16)
DILATED_PATTERNS = ((128, 1), (512, 4), (2048, 16))


class Sched:
    ENG = ("pe", "act", "dve", "pool", "sp")

    def __init__(self, nc, stack, n_lanes=8):
        self.nc = nc
        self.stack = stack
        self.engs = {"pe": nc.tensor, "act": nc.scalar, "dve": nc.vector, "pool": nc.gpsimd, "sp": nc.sync}
        self.prog = {}
        self.seen = {e: {} for e in self.ENG}
        self.nsem = 0
        self.pe_sems = set()
        for e in self.ENG:
            self.prog[e] = [self._newsem("p_" + e), 0]
        self.pe_sems.add(self.prog["pe"][0].num)
        self.lanes = {}
        for e in ("sp", "pool"):
            self.lanes[e] = [[self._newsem("l_%s%d" % (e, i)), 0] for i in range(n_lanes)]
        self.lane_rr = {e: 0 for e in self.lanes}
        self.all_sems = {}
        self.last_w = {}
        self.readers = {}
        self.n_inst = {e: 0 for e in self.ENG}

    def _newsem(self, name):
        self.nsem += 1
        return self.stack.enter_context(self.nc.semaphore("%s_%d" % (name, self.nsem)))

    def _emit(self, e, fn):
        self.n_inst[e] += 1
        return fn(self.engs[e])

    def _wait(self, e, tok):
        if tok is None:
            return
        sem, val = tok
        if e == "pe" and sem.num in self.pe_sems:
            return
        if self.seen[e].get(sem.num, 0) >= val:
            return
        self.seen[e][sem.num] = val
        self._emit(e, lambda eng: eng.wait_ge(sem, val))

    def _deps(self, e, reads, writes):
        for k in reads:
            self._wait(e, self.last_w.get(k))
        for k in writes:
            self._wait(e, self.last_w.get(k))
            for t in self.readers.get(k, ()):
                self._wait(e, t)

    def _commit(self, tok, reads, writes):
        self.all_sems[tok[0].num] = tok
        for k in writes:
            self.last_w[k] = tok
            self.readers[k] = []
        for k in reads:
            self.readers.setdefault(k, []).append(tok)

    def _next_tok(self, e):
        p = self.prog[e]
        if p[1] >= SEM_CAP:
            p[0] = self._newsem("p_" + e)
            p[1] = 0
            if e == "pe":
                self.pe_sems.add(p[0].num)
        p[1] += 1
        return (p[0], p[1])

    def op(self, e, fn, reads=(), writes=()):
        self._deps(e, reads, writes)
        tok = self._next_tok(e)
        self._emit(e, lambda eng: fn(eng).then_inc(tok[0], 1))
        self._commit(tok, reads, writes)
        return tok

    def group(self, e, fns, reads=(), writes=()):
        self._deps(e, reads, writes)
        tok = self._next_tok(e)
        for fn in fns[:-1]:
            self._emit(e, fn)
        self._emit(e, lambda eng: fns[-1](eng).then_inc(tok[0], 1))
        self._commit(tok, reads, writes)
        return tok

    def dma(self, e, out, in_, reads=(), writes=()):
        lanes = self.lanes[e]
        i = self.lane_rr[e]
        self.lane_rr[e] = (i + 1) % len(lanes)
        ln = lanes[i]
        if ln[1] >= SEM_CAP // 16:
            ln[0] = self._newsem("l_" + e)
            ln[1] = 0
        if ln[1] > 0:
            self._wait(e, (ln[0], 16 * ln[1]))
        self._deps(e, reads, writes)
        ln[1] += 1
        tok = (ln[0], 16 * ln[1])
        self._emit(e, lambda eng: eng.dma_start(out=out, in_=in_).then_inc(tok[0], 16))
        self._commit(tok, reads, writes)
        return tok

    def barrier(self, engines=None):
        toks = list(self.all_sems.values())
        for e in (engines or self.ENG):
            for tok in toks:
                self._wait(e, tok)

    def finish(self):
        self.barrier(engines=("sp",))


ARENA_BYTES = 200 * 1024
CC_MAX_BYTES = 2 * 1024 * 1024


def build(cfg):
    S, D, NH, DFF, TO, L = cfg["S"], cfg["D"], cfg["NH"], cfg["DFF"], cfg["TO"], cfg["L"]
    PGn, PGD, NGW = cfg["PGn"], cfg["PGD"], cfg["NGW"]
    windows = cfg["windows"]
    groups = cfg["groups"]
    debug = cfg.get("debug", False)
    NR = cfg.get("NR", 1)
    RG = cfg.get("RG", None)
    KC = D // 128
    GW = NH * 128
    INW = 10 * GW
    MWL = 4 * GW
    MW = NR * MWL
    KC2 = MW // 128
    TB = S // 128
    FC = DFF // 128
    PC = PGD // 128
    NBM = S // MOBA_BLOCK
    NQC = S // 512
    scale = 128 ** -0.5
    TG = 512
    NTG = TO // TG

    nc = bass.Bass("TRN2", target_bir_lowering=False)

    def din(name, shape, dt=F32):
        return nc.dram_tensor(name, list(shape), dt, kind="ExternalInput").ap()

    skind = "ExternalOutput" if debug else "Internal"

    def dscr(name, shape, dt):
        return nc.dram_tensor(name, list(shape), dt, kind=skind).ap()

    x_seq = din("x_seq", [TO, D])
    c_ab = din("c_ab", [128, 2])
    w_in = din("w_in", [L, D, INW])
    w_out = din("w_out", [L, MW, D])
    w_gate = din("w_gate", [L, D, DFF])
    w_up = din("w_up", [L, D, DFF])
    w_down = din("w_down", [L, DFF, D])
    w_pool = din("w_pool", [L, PGn, PGD, PGD])
    gb_pre = din("gb_pre", [L, 128, D])
    gb_post = din("gb_post", [L, 128, D])
    gb_fpre = din("gb_fpre", [L, 128, D])
    gb_fpost = din("gb_fpost", [L, 128, D])
    gb_mo = din("gb_mo", [L, 128, MW])
    psb = din("psb", [L, 128, GW])
    c_ident = din("c_ident", [128, 128])
    c_tri = din("c_tri", [128, 128])
    c_msd = din("c_msd", [128, S])
    c_msc = din("c_msc", [128, S])
    c_mss = din("c_mss", [128, S])
    c_pcoef = din("c_pcoef", [PGn, 4, 128, S])
    c_nm = din("c_nm", [128, NBM, 8])
    out = nc.dram_tensor("out", [TO, D], F32, kind="ExternalOutput").ap()

    qkT_d = dscr("qkT_d", [6, NH, 128, S], BF16)
    v_d = dscr("v_d", [3, S, GW], BF16)
    u_d = dscr("u_d", [GW // 128, 128, S], F32)
    y_d = dscr("y_d", [S, MWL], BF16)
    yg_d = dscr("yg_d", [NR * S, MWL], BF16) if NR > 1 else y_d
    hTo_d = dscr("hTo_d", [128 * KC, TO], BF16)
    hTg_d = dscr("hTg_d", [NR * 128 * KC, TO], BF16) if NR > 1 else hTo_d
    xres_d = dscr("xres_d", [TO, D], F32)

    with contextlib.ExitStack() as st:
        s = Sched(nc, st)

        def SB(name, shape, dt):
            return st.enter_context(nc.sbuf_tensor(name, list(shape), dt))

        ps = [st.enter_context(nc.psum_tensor("ps%d" % i, [128, 512], F32)) for i in range(8)]
        psb16 = [p[:].bitcast(BF16) for p in ps]
        rr = [0]

        def PS():
            i = rr[0]
            rr[0] = (i + 1) % 8
            return i

        evt = [0]

        def EV():
            evt[0] ^= 1
            return "act" if evt[0] else "dve"

        def copy_op(e, out_ap, in_ap, reads, writes):
            if e == "act":
                s.op("act", lambda g: g.copy(out=out_ap, in_=in_ap), reads=reads, writes=writes)
            else:
                s.op(e, lambda g: g.tensor_copy(out=out_ap, in_=in_ap), reads=reads, writes=writes)

        idb = SB("idb", [128, 128], BF16)
        trib = SB("trib", [128, 128], BF16)
        onesb = SB("onesb", [128, 128], BF16)
        epsb = SB("epsb", [128, 1], F32)
        stt = SB("stt", [128, 32], F32)
        arena = SB("arena", [128, ARENA_BYTES // 2], BF16)
        s.dma("pool", idb[:], c_ident, writes=["idb"])
        s.dma("pool", trib[:], c_tri, writes=["trib"])
        s.op("dve", lambda g: g.memset(onesb[:], 1.0), writes=["onesb"])
        s.op("dve", lambda g: g.memset(epsb[:], RMS_EPS), writes=["epsb"])

        class Arena:
            def __init__(self):
                self.off = 0

            def at(self, off, shape, dt):
                n = int(np.prod(shape))
                nb = n * (2 if dt == BF16 else 4)
                assert off % 4 == 0 and off + nb <= ARENA_BYTES, (off, nb)
                if dt == BF16:
                    ap = arena[:, off // 2: off // 2 + n]
                else:
                    ap = arena[:, off // 2: off // 2 + 2 * n].bitcast(F32)
                if len(shape) == 2:
                    ap = ap.rearrange("p (a b) -> p a b", a=shape[0])
                return ap, off + ((nb + 31) // 32) * 32

            def reset(self, off=0):
                self.off = off

            def alloc(self, shape, dt):
                ap, self.off = self.at(self.off, shape, dt)
                return ap

        A = Arena()
        ccsem = [s._newsem("cc"), 0]
        abt = SB("abt", [128, 2], F32)
        s.dma("sp", abt[:], c_ab, writes=["abt"])

        def cc_chunks(rows, cols):
            rc = rows
            while rc * cols * 2 > CC_MAX_BYTES and rc % 2 == 0:
                rc //= 2
            return rc

        def all_gather(src, dst, rows, cols):
            rc = cc_chunks(rows, cols)
            s.barrier()
            for c in range(rows // rc):
                ccsem[1] += 1
                tok = (ccsem[0], ccsem[1])
                s._emit("pool", lambda eng: eng.collective_compute(
                    "AllGather", ALU.bypass, replica_groups=RG, ins=[src[c * rc:(c + 1) * rc, :].opt()],
                    outs=[dst[c * NR * rc:(c + 1) * NR * rc, :].opt()]).then_inc(ccsem[0]))
                s._commit(tok, [], [])
            s.barrier()
            return rc

        def rstd(src_ap, junk_ap, col, denom, rkeys, jkeys):
            s.op("act", lambda g: g.activation(out=junk_ap, in_=src_ap, func=AF.Square, accum_out=stt[:, col:col + 1]),
                 reads=rkeys, writes=list(jkeys) + [("stt", col)])
            s.op("act", lambda g: g.activation(out=stt[:, col + 1:col + 2], in_=stt[:, col:col + 1], func=AF.Sqrt,
                                               scale=1.0 / denom, bias=epsb[:]),
                 reads=[("stt", col), "epsb"], writes=[("stt", col + 1)])
            s.op("dve", lambda g: g.reciprocal(out=stt[:, col + 2:col + 3], in_=stt[:, col + 1:col + 2]),
                 reads=[("stt", col + 1)], writes=[("stt", col + 2)])
            return stt[:, col + 2:col + 3], ("stt", col + 2)

        def transposeT(src, skeys, nchunk, dst, dkey, t_off):
            for g0 in range(0, nchunk, 8):
                n = min(8, nchunk - g0)
                b = PS()
                fns = [(lambda e, j=j: e.transpose(out=psb16[b][:, j * 128:(j + 1) * 128],
                                                   in_=src[:, (g0 + j) * 128:(g0 + j + 1) * 128], identity=idb[:]))
                       for j in range(n)]
                s.group("pe", fns, reads=list(skeys) + ["idb"], writes=[("ps", b)])
                copy_op(EV(), dst[:, g0:g0 + n, t_off:t_off + 128],
                        psb16[b][:, 0:n * 128].rearrange("p (k t) -> p k t", k=n), [("ps", b)], [dkey])

        def mm_group(b, ncols, pairs, reads):
            n = len(pairs)
            fns = [(lambda e, i=i: e.matmul(ps[b][:, 0:ncols], lhsT=pairs[i][0], rhs=pairs[i][1],
                                            start=(i == 0), stop=(i == n - 1))) for i in range(n)]
            s.group("pe", fns, reads=reads, writes=[("ps", b)])

        for l in range(L):
            xsrc = x_seq if l == 0 else xres_d
            A.reset()
            hTo = A.alloc([KC, TO], BF16)
            gb = A.alloc([D], F32)
            xt = A.alloc([D], F32)
            xb = A.alloc([D], BF16)
            s.dma("sp", gb, gb_pre[l], writes=["gb"])
            for tb in range(TO // 128):
                s.dma("sp", xt, xsrc[tb * 128:(tb + 1) * 128, :], writes=["xt"])
                r_ap, rk = rstd(xt, xb, 0, D, ["xt"], ["xb"])
                s.op("dve", lambda g: g.scalar_tensor_tensor(out=xb, in0=xt, scalar=r_ap, in1=gb,
                                                             op0=ALU.mult, op1=ALU.mult),
                     reads=["xt", rk, "gb"], writes=["xb"])
                transposeT(xb, ["xb"], KC, hTo, ("hTo", tb), tb * 128)
            s.dma("sp", hTo_d.rearrange("(p k) t -> p k t", k=KC), hTo, reads=[("hTo", tb) for tb in range(TO // 128)])
            if NR > 1:
                rc = all_gather(hTo_d, hTg_d, 128 * KC, TO)
            else:
                rc = 128 * KC
                s.barrier()
            A.reset()
            hT = A.alloc([KC, S], BF16)
            p_mark = A.off
            ppc = rc // KC
            assert rc % KC == 0 and (ppc % 32 == 0 or ppc == 128)
            for c in range(128 * KC // rc):
                for r in range(NR):
                    s.dma("sp", hT[c * ppc:(c + 1) * ppc, :, r * TO:(r + 1) * TO],
                          hTg_d[(c * NR + r) * rc:(c * NR + r + 1) * rc, :].rearrange("(p k) t -> p k t", k=KC),
                          writes=[("hT", tb, c) for tb in range(r * TO // 128, (r + 1) * TO // 128)])
            NHC = 128 * KC // rc
            A.reset(p_mark)
            CT = 256
            wbufs = [A.alloc([KC, CT], BF16) for _ in range(3)]
            stg = [A.alloc([512], BF16) for _ in range(2)]
            stgf = [A.alloc([512], F32) for _ in range(2)]
            si = 0
            for ct in range(INW // CT):
                wb = wbufs[ct % 3]
                wk = ("w2", ct % 3)
                s.dma("pool", wb, w_in[l, :, ct * CT:(ct + 1) * CT].rearrange("(k p) n -> p k n", p=128), writes=[wk])
                seg = (ct * CT) // GW
                off = ct * CT - seg * GW
                if seg in (2, 5, 8):
                    m = {2: 0, 5: 1, 8: 2}[seg]
                    for tb in range(TB):
                        b = PS()
                        mm_group(b, CT, [(hT[:, k, tb * 128:(tb + 1) * 128], wb[:, k, :]) for k in range(KC)],
                                 [("hT", tb, c_) for c_ in range(NHC)] + [wk])
                        si ^= 1
                        copy_op(EV(), stg[si][:, 0:CT], ps[b][:, 0:CT], [("ps", b)], [("stg", si)])
                        s.dma("sp", v_d[m, tb * 128:(tb + 1) * 128, off:off + CT], stg[si][:, 0:CT], reads=[("stg", si)])
                else:
                    for sub in range(CT // 128):
                        h = (off + sub * 128) // 128
                        for tc in range(NQC):
                            b = PS()
                            mm_group(b, 512, [(wb[:, k, sub * 128:(sub + 1) * 128], hT[:, k, tc * 512:(tc + 1) * 512])
                                              for k in range(KC)],
                                     [("hT", tc * 4 + i, c_) for i in range(4) for c_ in range(NHC)] + [wk])
                            si ^= 1
                            if seg == 9:
                                copy_op(EV(), stgf[si], ps[b][:], [("ps", b)], [("stgf", si)])
                                s.dma("sp", u_d[h, :, tc * 512:(tc + 1) * 512], stgf[si], reads=[("stgf", si)])
                            else:
                                idx = {0: 0, 1: 1, 3: 2, 4: 3, 6: 4, 7: 5}[seg]
                                copy_op(EV(), stg[si], ps[b][:], [("ps", b)], [("stg", si)])
                                s.dma("sp", qkT_d[idx, h, :, tc * 512:(tc + 1) * 512], stg[si], reads=[("stg", si)])
            s.barrier()

            A.reset()
            ystage = A.alloc([TB, GW], BF16)
            msd = A.alloc([S], BF16)
            msc = A.alloc([S], BF16)
            mss = A.alloc([S], BF16)
            s.dma("pool", msd, c_msd, writes=["msd"])
            s.dma("pool", msc, c_msc, writes=["msc"])
            s.dma("pool", mss, c_mss, writes=["mss"])
            qT = [A.alloc([S], BF16) for _ in range(2)]
            kT = [A.alloc([S], BF16) for _ in range(2)]
            vx = [A.alloc([TB, 130], BF16) for _ in range(2)]
            for i in range(2):
                s.op("dve", lambda g, i=i: g.memset(vx[i][:, :, 128:129], 1.0), writes=[("vx1", i)])
            Eall = A.alloc([TB, 512], BF16)
            acc = A.alloc([4, 129], F32)
            nm = A.alloc([NBM, 8], F32)
            s.dma("sp", nm, c_nm, writes=["nm"])
            km = A.alloc([NBM], F32)
            kmh = A.alloc([NBM], BF16)
            kml = A.alloc([NBM], BF16)
            g8 = A.alloc([8], F32)
            top8 = A.alloc([8], F32)
            sel = A.alloc([TB, 8], F32)
            s.op("dve", lambda g: g.memset(g8, -1e30), writes=["g8"])
            t_e = A.alloc([512], F32)
            t_sp = A.alloc([512], F32)
            t_1 = A.alloc([512], F32)
            t_3 = A.alloc([512], F32)
            spm = A.alloc([512], BF16)
            Rb = A.alloc([512], F32)
            p3_mark = A.off
            hcount = [0]

            def load_head(m, h):
                i = hcount[0] % 2
                hcount[0] += 1
                s.dma("sp", qT[i], qkT_d[2 * m, h], writes=[("qT", i)])
                s.dma("sp", kT[i], qkT_d[2 * m + 1, h], writes=[("kT", i)])
                s.dma("sp", vx[i][:, :, 0:128],
                      v_d[m, :, h * 128:(h + 1) * 128].rearrange("(tb p) d -> p tb d", p=128), writes=[("vx", i)])
                return i

            def finalize(src_ap, den_ap, rkeys, qb, h):
                s.op("dve", lambda g: g.reciprocal(out=stt[:, 8:9], in_=den_ap), reads=rkeys, writes=[("stt", 8)])
                s.op("dve", lambda g: g.tensor_scalar(out=ystage[:, qb, h * 128:(h + 1) * 128], in0=src_ap,
                                                      scalar1=stt[:, 8:9], scalar2=None, op0=ALU.mult),
                     reads=list(rkeys) + [("stt", 8)], writes=[("ys", qb)])

            def store_mixer(mi):
                s.dma("sp", y_d[:, mi * GW:(mi + 1) * GW].rearrange("(tb p) c -> p tb c", p=128), ystage,
                      reads=[("ys", qb) for qb in range(TB)])

            mtog = [0]

            def mask_eng():
                mtog[0] ^= 1
                return "pool" if mtog[0] else "dve"

            for h in range(NH):
                i = load_head(0, h)
                hk = [("qT", i), ("kT", i), ("vx", i), ("vx1", i)]
                s.op("dve", lambda g: g.tensor_reduce(out=km, in_=kT[i].rearrange("p (n k) -> p n k", k=MOBA_BLOCK),
                                                      axis=AX.X, op=ALU.add), reads=[("kT", i)], writes=["km"])
                s.op("dve", lambda g: g.tensor_scalar(out=kmh, in0=km, scalar1=1.0 / MOBA_BLOCK, scalar2=None,
                                                      op0=ALU.mult), reads=["km"], writes=["kmh"])
                s.op("dve", lambda g: g.scalar_tensor_tensor(out=kml, in0=km, scalar=1.0 / MOBA_BLOCK, in1=kmh,
                                                             op0=ALU.mult, op1=ALU.subtract),
                     reads=["km", "kmh"], writes=["kml"])
                for tb in range(TB):
                    b = PS()
                    mm_group(b, NBM, [(qT[i][:, tb * 128:(tb + 1) * 128], kmh),
                                      (qT[i][:, tb * 128:(tb + 1) * 128], kml)], [("qT", i), "kmh", "kml"])
                    blk = tb // 2
                    s.op("dve", lambda g: g.tensor_tensor(out=g8[:, 0:NBM], in0=ps[b][:, 0:NBM], in1=nm[:, blk, 0:NBM],
                                                          op=ALU.add), reads=[("ps", b), "nm"], writes=["g8"])
                    s.op("dve", lambda g: g.max(out=top8, in_=g8), reads=["g8"], writes=["top8"])
                    s.op("dve", lambda g: g.tensor_scalar(out=sel[:, tb, :], in0=g8, scalar1=top8[:, 2:3], scalar2=None,
                                                          op0=ALU.is_ge), reads=["g8", "top8"], writes=[("sel", tb)])
                for qc in range(NQC):
                    for n in range(2 * qc + 2):
                        q0 = max(qc * 512, n * 256)
                        W = (qc + 1) * 512 - q0
                        c0 = q0 - qc * 512
                        own = (n >= 2 * qc)
                        for j in range(2):
                            kb = 2 * n + j
                            b = PS()
                            mm_group(b, W, [(kT[i][:, kb * 128:(kb + 1) * 128], qT[i][:, q0:q0 + W])], hk)
                            s.op("act", lambda g: g.activation(out=Eall[:, j, c0:512], in_=ps[b][:, 0:W], func=AF.Exp,
                                                               scale=scale), reads=[("ps", b)], writes=[("E", j)])
                            if own and j == 0:
                                s.op(mask_eng(), lambda g: g.tensor_tensor(out=Eall[:, 0, c0:c0 + 256], in0=Eall[:, 0, c0:c0 + 256],
                                                                           in1=msc[:, 0:256], op=ALU.mult),
                                     reads=[("E", 0), "msc"], writes=[("E", 0)])
                            elif own:
                                s.op("pool", lambda g: g.memset(Eall[:, 1, c0:c0 + 128], 0.0), reads=[("E", 1)], writes=[("E", 1)])
                                s.op(mask_eng(), lambda g: g.tensor_tensor(out=Eall[:, 1, c0 + 128:c0 + 256],
                                                                           in0=Eall[:, 1, c0 + 128:c0 + 256],
                                                                           in1=msc[:, 0:128], op=ALU.mult),
                                     reads=[("E", 1), "msc"], writes=[("E", 1)])
                        for sub in range(c0 // 128, 4):
                            qb = qc * 4 + sub
                            b = PS()
                            mm_group(b, 129, [(Eall[:, 0, sub * 128:(sub + 1) * 128], vx[i][:, 2 * n, 0:129]),
                                              (Eall[:, 1, sub * 128:(sub + 1) * 128], vx[i][:, 2 * n + 1, 0:129])],
                                     [("E", 0), ("E", 1)] + hk)
                            ak = ("acc", sub)
                            if n == 0:
                                if qb // 2 == 0:
                                    s.op("dve", lambda g: g.tensor_copy(out=acc[:, sub, :], in_=ps[b][:, 0:129]),
                                         reads=[("ps", b)], writes=[ak])
                                else:
                                    s.op("dve", lambda g: g.tensor_scalar(out=acc[:, sub, :], in0=ps[b][:, 0:129],
                                                                          scalar1=sel[:, qb, 0:1], scalar2=None, op0=ALU.mult),
                                         reads=[("ps", b), ("sel", qb)], writes=[ak])
                            elif n == qb // 2:
                                s.op("dve", lambda g: g.tensor_tensor(out=acc[:, sub, :], in0=ps[b][:, 0:129],
                                                                      in1=acc[:, sub, :], op=ALU.add),
                                     reads=[("ps", b), ak], writes=[ak])
                            else:
                                s.op("dve", lambda g: g.scalar_tensor_tensor(out=acc[:, sub, :], in0=ps[b][:, 0:129],
                                                                             scalar=sel[:, qb, n:n + 1], in1=acc[:, sub, :],
                                                                             op0=ALU.mult, op1=ALU.add),
                                     reads=[("ps", b), ak, ("sel", qb)], writes=[ak])
                    for sub in range(4):
                        finalize(acc[:, sub, 0:128], acc[:, sub, 128:129], [("acc", sub)], qc * 4 + sub, h)
            store_mixer(0)

            for h in range(NH):
                i = load_head(1, h)
                hk = [("qT", i), ("kT", i), ("vx", i)]
                for qc in range(NQC):
                    s.op("dve", lambda g: g.memset(Rb, 0.0), writes=["Rb"])
                    nkb = (qc + 1) * 4
                    for kb in reversed(range(nkb)):
                        q0 = max(qc * 512, kb * 128)
                        W = (qc + 1) * 512 - q0
                        c0 = q0 - qc * 512
                        diag = (kb * 128 >= qc * 512)
                        bz = PS()
                        mm_group(bz, W, [(kT[i][:, kb * 128:(kb + 1) * 128], qT[i][:, q0:q0 + W])], hk)
                        s.op("act", lambda g: g.activation(out=t_e[:, 0:W], in_=ps[bz][:, 0:W], func=AF.Exp, scale=scale),
                             reads=[("ps", bz)], writes=["t_e"])
                        s.op("act", lambda g: g.activation(out=t_sp[:, 0:W], in_=t_e[:, 0:W], func=AF.Ln, bias=1.0),
                             reads=["t_e"], writes=["t_sp"])
                        if diag:
                            s.op("dve", lambda g: g.tensor_tensor(out=spm[:, 0:128], in0=t_sp[:, 0:128], in1=mss[:, 0:128],
                                                                  op=ALU.mult), reads=["t_sp", "mss"], writes=["spm"])
                            if W > 128:
                                s.op("pool", lambda g: g.tensor_copy(out=spm[:, 128:W], in_=t_sp[:, 128:W]),
                                     reads=["t_sp"], writes=["spm2"])
                        else:
                            s.op("pool", lambda g: g.tensor_copy(out=spm[:, 0:W], in_=t_sp[:, 0:W]),
                                 reads=["t_sp"], writes=["spm", "spm2"])
                        bc = PS()
                        mm_group(bc, W, [(trib[:], spm[:, 0:W])], ["trib", "spm", "spm2"])
                        br = PS()
                        mm_group(br, W, [(onesb[:], spm[:, 0:W])], ["onesb", "spm", "spm2"])
                        s.op("dve", lambda g: g.tensor_tensor(out=t_1[:, 0:W], in0=ps[bc][:, 0:W], in1=Rb[:, c0:512],
                                                              op=ALU.add), reads=[("ps", bc), "Rb"], writes=["t_1"])
                        s.op("pool", lambda g: g.tensor_tensor(out=t_1[:, 0:W], in0=t_1[:, 0:W], in1=t_sp[:, 0:W],
                                                               op=ALU.add), reads=["t_1", "t_sp"], writes=["t_1"])
                        s.op("dve", lambda g: g.scalar_tensor_tensor(out=t_3[:, 0:W], in0=ps[bz][:, 0:W], scalar=scale,
                                                                     in1=t_1[:, 0:W], op0=ALU.mult, op1=ALU.subtract),
                             reads=[("ps", bz), "t_1"], writes=["t_3"])
                        s.op("act", lambda g: g.activation(out=Eall[:, kb, c0:512], in_=t_3[:, 0:W], func=AF.Exp),
                             reads=["t_3"], writes=[("E", kb)])
                        if diag:
                            s.op("pool", lambda g: g.tensor_tensor(out=Eall[:, kb, c0:c0 + 128], in0=Eall[:, kb, c0:c0 + 128],
                                                                   in1=mss[:, 0:128], op=ALU.mult),
                                 reads=[("E", kb), "mss"], writes=[("E", kb)])
                        s.op("dve", lambda g: g.tensor_tensor(out=Rb[:, c0:512], in0=ps[br][:, 0:W], in1=Rb[:, c0:512],
                                                              op=ALU.add), reads=[("ps", br), "Rb"], writes=["Rb"])
                    for sub in range(4):
                        qb = qc * 4 + sub
                        b = PS()
                        mm_group(b, 128, [(Eall[:, kb, sub * 128:(sub + 1) * 128], vx[i][:, kb, 0:128]) for kb in range(qb + 1)],
                                 [("E", kb) for kb in range(qb + 1)] + hk)
                        copy_op(EV(), ystage[:, qb, h * 128:(h + 1) * 128], ps[b][:, 0:128], [("ps", b)], [("ys", qb)])
            store_mixer(1)

            for h in range(NH):
                i = load_head(2, h)
                hk = [("qT", i), ("kT", i), ("vx", i), ("vx1", i)]
                for qc in range(NQC):
                    nkb = (qc + 1) * 4
                    for kb in range(nkb):
                        q0 = max(qc * 512, kb * 128)
                        W = (qc + 1) * 512 - q0
                        c0 = q0 - qc * 512
                        b = PS()
                        mm_group(b, W, [(kT[i][:, kb * 128:(kb + 1) * 128], qT[i][:, q0:q0 + W])], hk)
                        s.op("act", lambda g: g.activation(out=Eall[:, kb, c0:512], in_=ps[b][:, 0:W], func=AF.Exp, scale=scale),
                             reads=[("ps", b)], writes=[("E", kb)])
                        mo = q0 - 128 * kb
                        s.op(mask_eng(), lambda g: g.tensor_tensor(out=Eall[:, kb, c0:512], in0=Eall[:, kb, c0:512],
                                                                   in1=msd[:, mo:mo + W], op=ALU.mult),
                             reads=[("E", kb), "msd"], writes=[("E", kb)])
                    for sub in range(4):
                        qb = qc * 4 + sub
                        b = PS()
                        mm_group(b, 129, [(Eall[:, kb, sub * 128:(sub + 1) * 128], vx[i][:, kb, 0:129]) for kb in range(qb + 1)],
                                 [("E", kb) for kb in range(qb + 1)] + hk)
                        finalize(ps[b][:, 0:128], ps[b][:, 128:129], [("ps", b)], qb, h)
            store_mixer(3)

            A.reset(p3_mark)
            up = A.alloc([16 + S], F32)
            pa = A.alloc([16 + S], F32)
            pb = A.alloc([16 + S], F32)
            coef = [A.alloc([S], F32) for _ in range(4)]
            tmpf = A.alloc([S], F32)
            tmp2 = A.alloc([S], F32)
            dT = A.alloc([PC, S], BF16)
            wp = A.alloc([PC, PGD], BF16)
            pst = A.alloc([GW], F32)
            s.dma("sp", pst, psb[l], writes=["pst"])
            for t_, k_ in ((up, "up"), (pa, "pa"), (pb, "pb")):
                s.op("dve", lambda g, t_=t_: g.memset(t_[:, 0:16], 0.0), writes=[k_ + "z"])
            for gi in range(PGn):
                s.dma("pool", wp, w_pool[l, gi].rearrange("(c p) d -> p c d", p=128), writes=["wp"])
                for wi in range(4):
                    s.dma("sp", coef[wi], c_pcoef[gi, wi], writes=[("coef", wi)])
                for c in range(PC):
                    ch = gi * PC + c
                    s.dma("sp", up[:, 16:16 + S], u_d[ch], writes=["up"])
                    cur, ck = up, "up"
                    for wi in range(4):
                        sh = 1 << wi
                        nxt, nk = (pa, "pa") if cur is not pa else (pb, "pb")
                        s.op("dve", lambda g, cur=cur, nxt=nxt, sh=sh: g.tensor_tensor(
                            out=nxt[:, 16:16 + S], in0=cur[:, 16:16 + S], in1=cur[:, 16 - sh:16 - sh + S], op=ALU.add),
                             reads=[ck, ck + "z"], writes=[nk])
                        cur, ck = nxt, nk
                        if wi == 0:
                            s.op("pool", lambda g, cur=cur: g.tensor_tensor(out=tmpf, in0=cur[:, 16:16 + S], in1=coef[0],
                                                                            op=ALU.mult), reads=[ck, ("coef", 0)], writes=["tmpf"])
                        else:
                            s.op("pool", lambda g, cur=cur: g.tensor_tensor(out=tmp2, in0=cur[:, 16:16 + S], in1=coef[wi],
                                                                            op=ALU.mult), reads=[ck, ("coef", wi)], writes=["tmp2"])
                            s.op("pool", lambda g: g.tensor_tensor(out=tmpf, in0=tmpf, in1=tmp2, op=ALU.add),
                                 reads=["tmpf", "tmp2"], writes=["tmpf"])
                    s.op("dve", lambda g: g.tensor_tensor(out=dT[:, c, :], in0=tmpf, in1=up[:, 16:16 + S],
                                                          op=ALU.subtract), reads=["tmpf", "up"], writes=[("dT", c)])
                for tb in range(TB):
                    b = PS()
                    mm_group(b, PGD, [(dT[:, c, tb * 128:(tb + 1) * 128], wp[:, c, :]) for c in range(PC)],
                             [("dT", c) for c in range(PC)] + ["wp"])
                    s.op("dve", lambda g: g.tensor_tensor(out=ystage[:, tb, gi * PGD:(gi + 1) * PGD], in0=ps[b][:, 0:PGD],
                                                          in1=pst[:, gi * PGD:(gi + 1) * PGD], op=ALU.mult),
                         reads=[("ps", b), "pst"], writes=[("ys", tb)])
            store_mixer(2)
            if NR > 1:
                yrc = all_gather(y_d, yg_d, S, MWL)
            else:
                yrc = S
                s.barrier()

            def yrow(r, t):
                return (t // yrc) * NR * yrc + r * yrc + t % yrc

            dst = out if l == L - 1 else xres_d
            LOW = 0
            MID1 = 32 * 1024
            MID2 = 64 * 1024
            MSB_OFF = ARENA_BYTES - 4 * D * 4
            ACT_OFF = ARENA_BYTES - ((FC * TG * 2 + 31) // 32) * 32
            assert KC2 * TG * 2 <= MID1 and KC * TG * 2 <= MID1 and 4 * D * 4 <= MID2
            for tg in range(NTG):
                yT, _ = A.at(LOW, [KC2, TG], BF16)
                A.reset(MID1)
                gbm = A.alloc([MW], F32)
                yt = A.alloc([MW], BF16)
                yt1 = A.alloc([MW], BF16)
                yb = A.alloc([MW], BF16)
                assert A.off <= MSB_OFF
                s.dma("sp", gbm, gb_mo[l], writes=["gbm"])
                ytk = ["yt"] if NR == 1 else [("ytp", 0, r) for r in range(NR)]
                for tb in range(4):
                    T0 = tg * TG + tb * 128
                    if NR == 1:
                        s.dma("sp", yt, y_d[T0:T0 + 128, :], writes=["yt"])
                    else:
                        for r in range(NR):
                            s.dma("sp", yt[:, r * MWL:(r + 1) * MWL], yg_d[yrow(r, T0):yrow(r, T0) + 128, :], writes=[("ytp", 0, r)])
                            s.dma("sp", yt1[:, r * MWL:(r + 1) * MWL], yg_d[yrow(r, TO + T0):yrow(r, TO + T0) + 128, :],
                                  writes=[("ytp", 1, r)])
                        s.op("pool", lambda g: g.tensor_scalar(out=yt, in0=yt, scalar1=abt[:, 0:1], scalar2=None, op0=ALU.mult),
                             reads=ytk + ["abt"], writes=ytk)
                        s.op("dve", lambda g: g.scalar_tensor_tensor(out=yt, in0=yt1, scalar=abt[:, 1:2], in1=yt,
                                                                     op0=ALU.mult, op1=ALU.add),
                             reads=[("ytp", 1, r) for r in range(NR)] + ["abt"] + ytk, writes=ytk)
                    for gi, ranges in enumerate(groups):
                        for ri, (c0, cl) in enumerate(ranges):
                            col = 12 + ri
                            s.op("act", lambda g: g.activation(out=yb[:, c0:c0 + cl], in_=yt[:, c0:c0 + cl],
                                                               func=AF.Square, accum_out=stt[:, col:col + 1]),
                                 reads=ytk, writes=[("yb", gi), ("stt", col)])
                        if len(ranges) == 2:
                            s.op("dve", lambda g: g.tensor_tensor(out=stt[:, 12:13], in0=stt[:, 12:13], in1=stt[:, 13:14],
                                                                  op=ALU.add),
                                 reads=[("stt", 12), ("stt", 13)], writes=[("stt", 12)])
                        s.op("act", lambda g: g.activation(out=stt[:, 14:15], in_=stt[:, 12:13], func=AF.Sqrt,
                                                           scale=1.0 / NGW, bias=epsb[:]),
                             reads=[("stt", 12), "epsb"], writes=[("stt", 14)])
                        s.op("dve", lambda g: g.reciprocal(out=stt[:, 16 + gi:17 + gi], in_=stt[:, 14:15]),
                             reads=[("stt", 14)], writes=[("stt", 16 + gi)])
                        for (c0, cl) in ranges:
                            s.op("dve", lambda g: g.scalar_tensor_tensor(out=yb[:, c0:c0 + cl], in0=yt[:, c0:c0 + cl],
                                                                         scalar=stt[:, 16 + gi:17 + gi],
                                                                         in1=gbm[:, c0:c0 + cl], op0=ALU.mult, op1=ALU.mult),
                                 reads=ytk + [("stt", 16 + gi), "gbm"], writes=[("yb", gi)])
                    transposeT(yb, [("yb", gi) for gi in range(len(groups))], KC2, yT, ("yT", tb), tb * 128)
                s.barrier()
                msb, _ = A.at(MSB_OFF, [4, D], F32)
                A.reset(MID1)
                wobuf = [A.alloc([KC2, 512], BF16) for _ in range(2)]
                assert A.off <= MSB_OFF
                for nt in range(D // 512):
                    wb = wobuf[nt % 2]
                    wk = ("wo", nt % 2)
                    s.dma("pool", wb, w_out[l, :, nt * 512:(nt + 1) * 512].rearrange("(k p) n -> p k n", p=128), writes=[wk])
                    for tb in range(4):
                        b = PS()
                        mm_group(b, 512, [(yT[:, k, tb * 128:(tb + 1) * 128], wb[:, k, :]) for k in range(KC2)],
                                 [("yT", tb), wk])
                        copy_op(EV(), msb[:, tb, nt * 512:(nt + 1) * 512], ps[b][:], [("ps", b)], [("msb", tb)])
                s.barrier()
                h2T, _ = A.at(LOW, [KC, TG], BF16)
                A.reset(MID1)
                gb1 = A.alloc([D], F32)
                gb2 = A.alloc([D], F32)
                xt = A.alloc([D], F32)
                xb = A.alloc([D], BF16)
                assert A.off <= MSB_OFF
                s.dma("sp", gb1, gb_post[l], writes=["gb1"])
                s.dma("sp", gb2, gb_fpre[l], writes=["gb2"])
                for tb in range(4):
                    T0 = tg * TG + tb * 128
                    s.dma("sp", xt, xsrc[T0:T0 + 128, :], writes=["xt"])
                    r_ap, rk = rstd(msb[:, tb, :], xb, 0, D, [("msb", tb)], ["xb"])
                    s.op("dve", lambda g: g.scalar_tensor_tensor(out=msb[:, tb, :], in0=msb[:, tb, :], scalar=r_ap,
                                                                 in1=gb1, op0=ALU.mult, op1=ALU.mult),
                         reads=[("msb", tb), rk, "gb1"], writes=[("msb", tb)])
                    s.op("pool", lambda g: g.tensor_tensor(out=xt, in0=xt, in1=msb[:, tb, :], op=ALU.add),
                         reads=["xt", ("msb", tb)], writes=["xt"])
                    s.dma("sp", xres_d[T0:T0 + 128, :], xt, reads=["xt"])
                    r_ap, rk = rstd(xt, xb, 4, D, ["xt"], ["xb"])
                    s.op("dve", lambda g: g.scalar_tensor_tensor(out=xb, in0=xt, scalar=r_ap, in1=gb2,
                                                                 op0=ALU.mult, op1=ALU.mult),
                         reads=["xt", rk, "gb2"], writes=["xb"])
                    transposeT(xb, ["xb"], KC, h2T, ("h2T", tb), tb * 128)
                s.barrier()
                actT, _ = A.at(ACT_OFF, [FC, TG], BF16)
                A.reset(MID1)
                CT2 = 256
                wgu = [A.alloc([KC, CT2], BF16) for _ in range(3)]
                sg = A.alloc([2, TG], F32)
                assert A.off <= ACT_OFF
                wi = 0
                h2keys = [("h2T", tb) for tb in range(4)]
                for j in range(DFF // CT2):
                    wg, wgk = wgu[wi % 3], ("wgu", wi % 3)
                    wi += 1
                    s.dma("pool", wg, w_gate[l, :, j * CT2:(j + 1) * CT2].rearrange("(k p) n -> p k n", p=128), writes=[wgk])
                    wu, wuk = wgu[wi % 3], ("wgu", wi % 3)
                    wi += 1
                    s.dma("pool", wu, w_up[l, :, j * CT2:(j + 1) * CT2].rearrange("(k p) n -> p k n", p=128), writes=[wuk])
                    for c in range(2):
                        b = PS()
                        mm_group(b, TG, [(wg[:, k, c * 128:(c + 1) * 128], h2T[:, k, :]) for k in range(KC)], h2keys + [wgk])
                        s.op("act", lambda g: g.activation(out=sg[:, c, :], in_=ps[b][:], func=AF.Silu),
                             reads=[("ps", b)], writes=[("sg", c)])
                    for c in range(2):
                        b = PS()
                        mm_group(b, TG, [(wu[:, k, c * 128:(c + 1) * 128], h2T[:, k, :]) for k in range(KC)], h2keys + [wuk])
                        s.op("dve", lambda g: g.tensor_tensor(out=actT[:, 2 * j + c, :], in0=ps[b][:], in1=sg[:, c, :],
                                                              op=ALU.mult),
                             reads=[("ps", b), ("sg", c)], writes=[("actT", 2 * j + c)])
                s.barrier()
                fsb, _ = A.at(LOW, [4, D], F32)
                A.reset(MID2)
                FG = 8
                wdb = [A.alloc([FG, 512], BF16) for _ in range(3)]
                assert A.off <= ACT_OFF
                nfg = (FC + FG - 1) // FG
                wdi = 0
                for nt in range(D // 512):
                    banks = [PS() for _ in range(4)]
                    for fg in range(nfg):
                        f0 = fg * FG
                        nf = min(FG, FC - f0)
                        wd, wdk = wdb[wdi % 3], ("wd", wdi % 3)
                        wdi += 1
                        s.dma("pool", wd[:, 0:nf, :],
                              w_down[l, f0 * 128:(f0 + nf) * 128, nt * 512:(nt + 1) * 512].rearrange("(c p) n -> p c n", p=128),
                              writes=[wdk])
                        for tb in range(4):
                            fns = [(lambda e, fc=fc, tb=tb, wd=wd: e.matmul(ps[banks[tb]][:], lhsT=actT[:, fc, tb * 128:(tb + 1) * 128],
                                                                            rhs=wd[:, fc - f0, :], start=(fc == 0), stop=(fc == FC - 1)))
                                   for fc in range(f0, f0 + nf)]
                            s.group("pe", fns, reads=[("actT", fc) for fc in range(f0, f0 + nf)] + [wdk],
                                    writes=[("ps", banks[tb])])
                    for tb in range(4):
                        copy_op(EV(), fsb[:, tb, nt * 512:(nt + 1) * 512], ps[banks[tb]][:], [("ps", banks[tb])], [("fsb", tb)])
                s.barrier()
                A.reset(MID2)
                gb1 = A.alloc([D], F32)
                xt = A.alloc([D], F32)
                xb = A.alloc([D], BF16)
                s.dma("sp", gb1, gb_fpost[l], writes=["gb1"])
                for tb in range(4):
                    T0 = tg * TG + tb * 128
                    s.dma("sp", xt, xres_d[T0:T0 + 128, :], writes=["xt"])
                    r_ap, rk = rstd(fsb[:, tb, :], xb, 0, D, [("fsb", tb)], ["xb"])
                    s.op("dve", lambda g: g.scalar_tensor_tensor(out=fsb[:, tb, :], in0=fsb[:, tb, :], scalar=r_ap,
                                                                 in1=gb1, op0=ALU.mult, op1=ALU.mult),
                         reads=[("fsb", tb), rk, "gb1"], writes=[("fsb", tb)])
                    s.op("pool", lambda g: g.tensor_tensor(out=xt, in0=xt, in1=fsb[:, tb, :], op=ALU.add),
                         reads=["xt", ("fsb", tb)], writes=["xt"])
                    s.dma("sp", dst[T0:T0 + 128, :], xt, reads=["xt"])
                s.barrier()
        s.finish()
    print("instructions:", s.n_inst, "sems:", s.nsem)
    return nc


def make_consts(S):
    kin = np.arange(128)[:, None]
    j = np.arange(S)[None, :]
    delta = j - kin
    msd = np.zeros((128, S), np.float32)
    for window, dil in DILATED_PATTERNS:
        msd += ((delta >= 0) & (delta <= window) & (delta % dil == 0)).astype(np.float32)
    msc = (delta >= 0).astype(np.float32)
    mss = (delta > 0).astype(np.float32)
    nbm = S // MOBA_BLOCK
    nm = np.zeros((128, nbm, 8), np.float32)
    for blk in range(nbm):
        for n in range(8):
            if n >= blk:
                nm[:, blk, n] = -1e30
    jj = np.arange(128)[:, None]
    ss = np.arange(128)[None, :]
    tri = (jj > ss).astype(np.float32)
    return {"c_ident": np.eye(128, dtype=np.float32), "c_tri": tri, "c_msd": msd, "c_msc": msc, "c_mss": mss, "c_nm": nm}


def pool_coef(S, windows):
    t = np.arange(S)
    out = np.zeros((len(windows), 4, 128, S), np.float32)
    for g, w in enumerate(windows):
        wi = {2: 0, 4: 1, 8: 2, 16: 3}[w]
        out[g, wi] = np.broadcast_to(1.0 / np.minimum(w, t + 1).astype(np.float32), (128, S))
    return out


def bcast128(v):
    return np.ascontiguousarray(np.broadcast_to(v[:, None, :], (v.shape[0], 128, v.shape[1]))).astype(np.float32)


_NC_CACHE = {}


def kernel(x, ln_mix_pre, w_in, w_pool, pool_scale, mix_out_norm, w_out, ln_mix_post, ln_ffn_pre,
           w_gate, w_up, w_down, ln_ffn_post):
    x = np.asarray(x, np.float32)
    B, S, D = x.shape
    L = w_in.shape[0]
    DFF = w_gate.shape[2]
    GWF = D // 4
    NR = 2
    GW = GWF // NR
    NH = GW // 128
    TO = S // NR
    PGD = GWF // 4
    PGn = 4 // NR
    n_cores = B * NR
    perm = np.concatenate([np.arange(m * GWF + r * GW, m * GWF + (r + 1) * GW) for r in range(NR) for m in range(4)])
    groups = [[(r * 4 * GW + m * GW, GW) for r in range(NR)] for m in range(4)]
    cfg = dict(S=S, D=D, NH=NH, DFF=DFF, TO=TO, L=L, PGn=PGn, PGD=PGD, NGW=GWF, windows=None, groups=groups,
               NR=NR, RG=[[2 * i, 2 * i + 1] for i in range(B)])
    key = (S, D, NH, DFF, L, NR, B)
    if key not in _NC_CACHE:
        _NC_CACHE[key] = build(cfg)
    nc = _NC_CACHE[key]
    w_in = np.asarray(w_in, np.float32)
    common = {
        "w_out": np.ascontiguousarray(np.asarray(w_out, np.float32)[:, perm, :]),
        "w_gate": np.ascontiguousarray(w_gate, np.float32), "w_up": np.ascontiguousarray(w_up, np.float32),
        "w_down": np.ascontiguousarray(w_down, np.float32),
        "gb_pre": bcast128(np.asarray(ln_mix_pre)), "gb_post": bcast128(np.asarray(ln_mix_post)),
        "gb_fpre": bcast128(np.asarray(ln_ffn_pre)), "gb_fpost": bcast128(np.asarray(ln_ffn_post)),
        "gb_mo": bcast128(np.asarray(mix_out_norm)[:, perm]),
    }
    common.update(make_consts(S))
    per_rank = []
    for r in range(NR):
        cols = np.concatenate([np.arange(sg * GWF + r * GW, sg * GWF + (r + 1) * GW) for sg in range(10)])
        ab = np.zeros((128, 2), np.float32)
        ab[:, r] = 1.0
        per_rank.append({
            "w_in": np.ascontiguousarray(w_in[:, :, cols]),
            "w_pool": np.ascontiguousarray(np.asarray(w_pool, np.float32)[:, r * PGn:(r + 1) * PGn]),
            "psb": bcast128(np.asarray(pool_scale)[:, r * GW:(r + 1) * GW]),
            "c_pcoef": pool_coef(S, POOL_WINDOWS[r * PGn:(r + 1) * PGn]),
            "c_ab": ab,
        })
    in_maps = []
    for c in range(n_cores):
        b, r = divmod(c, NR)
        m = dict(common)
        m.update(per_rank[r])
        m["x_seq"] = np.ascontiguousarray(x[b, r * TO:(r + 1) * TO])
        in_maps.append(m)
    res = run_bass_kernel_spmd(nc, in_maps, core_ids=list(range(n_cores)))
    out = np.empty((B, S, D), np.float32)
    for c in range(n_cores):
        b, r = divmod(c, NR)
        out[b, r * TO:(r + 1) * TO] = res.results[c]["out"]
    return out
```

```python
import contextlib
import numpy as np
import concourse.bass as bass
import concourse.mybir as mybir
from concourse.bass_utils import run_bass_kernel_spmd

F32 = mybir.dt.float32
BF16 = mybir.dt.bfloat16
ALU = mybir.AluOpType
AF = mybir.ActivationFunctionType
AX = mybir.AxisListType

SEM_CAP = 30000
RMS_EPS = 1e-6
MOBA_BLOCK = 256
POOL_WINDOWS = (2, 4, 8, 16)
DILATED_PATTERNS = ((128, 1), (512, 4), (2048, 16))


class Sched:
    ENG = ("pe", "act", "dve", "pool", "sp")

    def __init__(self, nc, stack, n_lanes=8):
        self.nc = nc
        self.stack = stack
        self.engs = {"pe": nc.tensor, "act": nc.scalar, "dve": nc.vector, "pool": nc.gpsimd, "sp": nc.sync}
        self.prog = {}
        self.seen = {e: {} for e in self.ENG}
        self.nsem = 0
        self.pe_sems = set()
        for e in self.ENG:
            self.prog[e] = [self._newsem("p_" + e), 0]
        self.pe_sems.add(self.prog["pe"][0].num)
        self.lanes = {}
        for e in ("sp", "pool"):
            self.lanes[e] = [[self._newsem("l_%s%d" % (e, i)), 0] for i in range(n_lanes)]
        self.lane_rr = {e: 0 for e in self.lanes}
        self.all_sems = {}
        self.last_w = {}
        self.readers = {}
        self.n_inst = {e: 0 for e in self.ENG}

    def _newsem(self, name):
        self.nsem += 1
        return self.stack.enter_context(self.nc.semaphore("%s_%d" % (name, self.nsem)))

    def _emit(self, e, fn):
        self.n_inst[e] += 1
        return fn(self.engs[e])

    def _wait(self, e, tok):
        if tok is None:
            return
        sem, val = tok
        if e == "pe" and sem.num in self.pe_sems:
            return
        if self.seen[e].get(sem.num, 0) >= val:
            return
        self.seen[e][sem.num] = val
        self._emit(e, lambda eng: eng.wait_ge(sem, val))

    def _deps(self, e, reads, writes):
        for k in reads:
            self._wait(e, self.last_w.get(k))
        for k in writes:
            self._wait(e, self.last_w.get(k))
            for t in self.readers.get(k, ()):
                self._wait(e, t)

    def _commit(self, tok, reads, writes):
        self.all_sems[tok[0].num] = tok
        for k in writes:
            self.last_w[k] = tok
            self.readers[k] = []
        for k in reads:
            self.readers.setdefault(k, []).append(tok)

    def _next_tok(self, e):
        p = self.prog[e]
        if p[1] >= SEM_CAP:
            p[0] = self._newsem("p_" + e)
            p[1] = 0
            if e == "pe":
                self.pe_sems.add(p[0].num)
        p[1] += 1
        return (p[0], p[1])

    def op(self, e, fn, reads=(), writes=()):
        self._deps(e, reads, writes)
        tok = self._next_tok(e)
        self._emit(e, lambda eng: fn(eng).then_inc(tok[0], 1))
        self._commit(tok, reads, writes)
        return tok

    def group(self, e, fns, reads=(), writes=()):
        self._deps(e, reads, writes)
        tok = self._next_tok(e)
        for fn in fns[:-1]:
            self._emit(e, fn)
        self._emit(e, lambda eng: fns[-1](eng).then_inc(tok[0], 1))
        self._commit(tok, reads, writes)
        return tok

    def dma(self, e, out, in_, reads=(), writes=()):
        lanes = self.lanes[e]
        i = self.lane_rr[e]
        self.lane_rr[e] = (i + 1) % len(lanes)
        ln = lanes[i]
        if ln[1] >= SEM_CAP // 16:
            ln[0] = self._newsem("l_" + e)
            ln[1] = 0
        if ln[1] > 0:
            self._wait(e, (ln[0], 16 * ln[1]))
        self._deps(e, reads, writes)
        ln[1] += 1
        tok = (ln[0], 16 * ln[1])
        self._emit(e, lambda eng: eng.dma_start(out=out, in_=in_).then_inc(tok[0], 16))
        self._commit(tok, reads, writes)
        return tok

    def barrier(self, engines=None):
        toks = list(self.all_sems.values())
        for e in (engines or self.ENG):
            for tok in toks:
                self._wait(e, tok)

    def finish(self):
        self.barrier(engines=("sp",))


ARENA_BYTES = 200 * 1024
CC_MAX_BYTES = 2 * 1024 * 1024


def build(cfg):
    S, D, NH, DFF, TO, L = cfg["S"], cfg["D"], cfg["NH"], cfg["DFF"], cfg["TO"], cfg["L"]
    PGn, PGD, NGW = cfg["PGn"], cfg["PGD"], cfg["NGW"]
    windows = cfg["windows"]
    groups = cfg["groups"]
    debug = cfg.get("debug", False)
    NR = cfg.get("NR", 1)
    RG = cfg.get("RG", None)
    KC = D // 128
    GW = NH * 128
    INW = 10 * GW
    MWL = 4 * GW
    MW = NR * MWL
    KC2 = MW // 128
    TB = S // 128
    FC = DFF // 128
    PC = PGD // 128
    NBM = S // MOBA_BLOCK
    NQC = S // 512
    scale = 128 ** -0.5
    TG = 512
    NTG = TO // TG

    nc = bass.Bass("TRN2", target_bir_lowering=False)

    def din(name, shape, dt=F32):
        return nc.dram_tensor(name, list(shape), dt, kind="ExternalInput").ap()

    skind = "ExternalOutput" if debug else "Internal"

    def dscr(name, shape, dt):
        return nc.dram_tensor(name, list(shape), dt, kind=skind).ap()

    x_seq = din("x_seq", [TO, D])
    c_ab = din("c_ab", [128, 2])
    w_in = din("w_in", [L, D, INW])
    w_out = din("w_out", [L, MW, D])
    w_gate = din("w_gate", [L, D, DFF])
    w_up = din("w_up", [L, D, DFF])
    w_down = din("w_down", [L, DFF, D])
    w_pool = din("w_pool", [L, PGn, PGD, PGD])
    gb_pre = din("gb_pre", [L, 128, D])
    gb_post = din("gb_post", [L, 128, D])
    gb_fpre = din("gb_fpre", [L, 128, D])
    gb_fpost = din("gb_fpost", [L, 128, D])
    gb_mo = din("gb_mo", [L, 128, MW])
    psb = din("psb", [L, 128, GW])
    c_ident = din("c_ident", [128, 128])
    c_tri = din("c_tri", [128, 128])
    c_msd = din("c_msd", [128, S])
    c_msc = din("c_msc", [128, S])
    c_mss = din("c_mss", [128, S])
    c_pcoef = din("c_pcoef", [PGn, 4, 128, S])
    c_nm = din("c_nm", [128, NBM, 8])
    out = nc.dram_tensor("out", [TO, D], F32, kind="ExternalOutput").ap()

    qkT_d = dscr("qkT_d", [6, NH, 128, S], BF16)
    v_d = dscr("v_d", [3, S, GW], BF16)
    u_d = dscr("u_d", [GW // 128, 128, S], F32)
    y_d = dscr("y_d", [S, MWL], BF16)
    yg_d = dscr("yg_d", [NR * S, MWL], BF16) if NR > 1 else y_d
    hTo_d = dscr("hTo_d", [128 * KC, TO], BF16)
    hTg_d = dscr("hTg_d", [NR * 128 * KC, TO], BF16) if NR > 1 else hTo_d
    xres_d = dscr("xres_d", [TO, D], F32)

    with contextlib.ExitStack() as st:
        s = Sched(nc, st)

        def SB(name, shape, dt):
            return st.enter_context(nc.sbuf_tensor(name, list(shape), dt))

        ps = [st.enter_context(nc.psum_tensor("ps%d" % i, [128, 512], F32)) for i in range(8)]
        psb16 = [p[:].bitcast(BF16) for p in ps]
        rr = [0]

        def PS():
            i = rr[0]
            rr[0] = (i + 1) % 8
            return i

        evt = [0]

        def EV():
            evt[0] ^= 1
            return "act" if evt[0] else "dve"

        def copy_op(e, out_ap, in_ap, reads, writes):
            if e == "act":
                s.op("act", lambda g: g.copy(out=out_ap, in_=in_ap), reads=reads, writes=writes)
            else:
                s.op(e, lambda g: g.tensor_copy(out=out_ap, in_=in_ap), reads=reads, writes=writes)

        idb = SB("idb", [128, 128], BF16)
        trib = SB("trib", [128, 128], BF16)
        onesb = SB("onesb", [128, 128], BF16)
        epsb = SB("epsb", [128, 1], F32)
        stt = SB("stt", [128, 32], F32)
        arena = SB("arena", [128, ARENA_BYTES // 2], BF16)
        s.dma("pool", idb[:], c_ident, writes=["idb"])
        s.dma("pool", trib[:], c_tri, writes=["trib"])
        s.op("dve", lambda g: g.memset(onesb[:], 1.0), writes=["onesb"])
        s.op("dve", lambda g: g.memset(epsb[:], RMS_EPS), writes=["epsb"])

        class Arena:
            def __init__(self):
                self.off = 0

            def at(self, off, shape, dt):
                n = int(np.prod(shape))
                nb = n * (2 if dt == BF16 else 4)
                assert off % 4 == 0 and off + nb <= ARENA_BYTES, (off, nb)
                if dt == BF16:
                    ap = arena[:, off // 2: off // 2 + n]
                else:
                    ap = arena[:, off // 2: off // 2 + 2 * n].bitcast(F32)
                if len(shape) == 2:
                    ap = ap.rearrange("p (a b) -> p a b", a=shape[0])
                return ap, off + ((nb + 31) // 32) * 32

            def reset(self, off=0):
                self.off = off

            def alloc(self, shape, dt):
                ap, self.off = self.at(self.off, shape, dt)
                return ap

        A = Arena()
        ccsem = [s._newsem("cc"), 0]
        abt = SB("abt", [128, 2], F32)
        s.dma("sp", abt[:], c_ab, writes=["abt"])

        def cc_chunks(rows, cols):
            rc = rows
            while rc * cols * 2 > CC_MAX_BYTES and rc % 2 == 0:
                rc //= 2
            return rc

        def all_gather(src, dst, rows, cols):
            rc = cc_chunks(rows, cols)
            s.barrier()
            for c in range(rows // rc):
                ccsem[1] += 1
                tok = (ccsem[0], ccsem[1])
                s._emit("pool", lambda eng: eng.collective_compute(
                    "AllGather", ALU.bypass, replica_groups=RG, ins=[src[c * rc:(c + 1) * rc, :].opt()],
                    outs=[dst[c * NR * rc:(c + 1) * NR * rc, :].opt()]).then_inc(ccsem[0]))
                s._commit(tok, [], [])
            s.barrier()
            return rc

        def rstd(src_ap, junk_ap, col, denom, rkeys, jkeys):
            s.op("act", lambda g: g.activation(out=junk_ap, in_=src_ap, func=AF.Square, accum_out=stt[:, col:col + 1]),
                 reads=rkeys, writes=list(jkeys) + [("stt", col)])
            s.op("act", lambda g: g.activation(out=stt[:, col + 1:col + 2], in_=stt[:, col:col + 1], func=AF.Sqrt,
                                               scale=1.0 / denom, bias=epsb[:]),
                 reads=[("stt", col), "epsb"], writes=[("stt", col + 1)])
            s.op("dve", lambda g: g.reciprocal(out=stt[:, col + 2:col + 3], in_=stt[:, col + 1:col + 2]),
                 reads=[("stt", col + 1)], writes=[("stt", col + 2)])
            return stt[:, col + 2:col + 3], ("stt", col + 2)

        def transposeT(src, skeys, nchunk, dst, dkey, t_off):
            for g0 in range(0, nchunk, 8):
                n = min(8, nchunk - g0)
                b = PS()
                fns = [(lambda e, j=j: e.transpose(out=psb16[b][:, j * 128:(j + 1) * 128],
                                                   in_=src[:, (g0 + j) * 128:(g0 + j + 1) * 128], identity=idb[:]))
                       for j in range(n)]
                s.group("pe", fns, reads=list(skeys) + ["idb"], writes=[("ps", b)])
                copy_op(EV(), dst[:, g0:g0 + n, t_off:t_off + 128],
                        psb16[b][:, 0:n * 128].rearrange("p (k t) -> p k t", k=n), [("ps", b)], [dkey])

        def mm_group(b, ncols, pairs, reads):
            n = len(pairs)
            fns = [(lambda e, i=i: e.matmul(ps[b][:, 0:ncols], lhsT=pairs[i][0], rhs=pairs[i][1],
                                            start=(i == 0), stop=(i == n - 1))) for i in range(n)]
            s.group("pe", fns, reads=reads, writes=[("ps", b)])

        for l in range(L):
            xsrc = x_seq if l == 0 else xres_d
            A.reset()
            hTo = A.alloc([KC, TO], BF16)
            gb = A.alloc([D], F32)
            xt = A.alloc([D], F32)
            xb = A.alloc([D], BF16)
            s.dma("sp", gb, gb_pre[l], writes=["gb"])
            for tb in range(TO // 128):
                s.dma("sp", xt, xsrc[tb * 128:(tb + 1) * 128, :], writes=["xt"])
                r_ap, rk = rstd(xt, xb, 0, D, ["xt"], ["xb"])
                s.op("dve", lambda g: g.scalar_tensor_tensor(out=xb, in0=xt, scalar=r_ap, in1=gb,
                                                             op0=ALU.mult, op1=ALU.mult),
                     reads=["xt", rk, "gb"], writes=["xb"])
                transposeT(xb, ["xb"], KC, hTo, ("hTo", tb), tb * 128)
            s.dma("sp", hTo_d.rearrange("(p k) t -> p k t", k=KC), hTo, reads=[("hTo", tb) for tb in range(TO // 128)])
            if NR > 1:
                rc = all_gather(hTo_d, hTg_d, 128 * KC, TO)
            else:
                rc = 128 * KC
                s.barrier()
            A.reset()
            hT = A.alloc([KC, S], BF16)
            p_mark = A.off
            ppc = rc // KC
            assert rc % KC == 0 and (ppc % 32 == 0 or ppc == 128)
            for c in range(128 * KC // rc):
                for r in range(NR):
                    s.dma("sp", hT[c * ppc:(c + 1) * ppc, :, r * TO:(r + 1) * TO],
                          hTg_d[(c * NR + r) * rc:(c * NR + r + 1) * rc, :].rearrange("(p k) t -> p k t", k=KC),
                          writes=[("hT", tb, c) for tb in range(r * TO // 128, (r + 1) * TO // 128)])
            NHC = 128 * KC // rc
            A.reset(p_mark)
            CT = 256
            wbufs = [A.alloc([KC, CT], BF16) for _ in range(3)]
            stg = [A.alloc([512], BF16) for _ in range(2)]
            stgf = [A.alloc([512], F32) for _ in range(2)]
            si = 0
            for ct in range(INW // CT):
                wb = wbufs[ct % 3]
                wk = ("w2", ct % 3)
                s.dma("pool", wb, w_in[l, :, ct * CT:(ct + 1) * CT].rearrange("(k p) n -> p k n", p=128), writes=[wk])
                seg = (ct * CT) // GW
                off = ct * CT - seg * GW
                if seg in (2, 5, 8):
                    m = {2: 0, 5: 1, 8: 2}[seg]
                    for tb in range(TB):
                        b = PS()
                        mm_group(b, CT, [(hT[:, k, tb * 128:(tb + 1) * 128], wb[:, k, :]) for k in range(KC)],
                                 [("hT", tb, c_) for c_ in range(NHC)] + [wk])
                        si ^= 1
                        copy_op(EV(), stg[si][:, 0:CT], ps[b][:, 0:CT], [("ps", b)], [("stg", si)])
                        s.dma("sp", v_d[m, tb * 128:(tb + 1) * 128, off:off + CT], stg[si][:, 0:CT], reads=[("stg", si)])
                else:
                    for sub in range(CT // 128):
                        h = (off + sub * 128) // 128
                        for tc in range(NQC):
                            b = PS()
                            mm_group(b, 512, [(wb[:, k, sub * 128:(sub + 1) * 128], hT[:, k, tc * 512:(tc + 1) * 512])
                                              for k in range(KC)],
                                     [("hT", tc * 4 + i, c_) for i in range(4) for c_ in range(NHC)] + [wk])
                            si ^= 1
                            if seg == 9:
                                copy_op(EV(), stgf[si], ps[b][:], [("ps", b)], [("stgf", si)])
                                s.dma("sp", u_d[h, :, tc * 512:(tc + 1) * 512], stgf[si], reads=[("stgf", si)])
                            else:
                                idx = {0: 0, 1: 1, 3: 2, 4: 3, 6: 4, 7: 5}[seg]
                                copy_op(EV(), stg[si], ps[b][:], [("ps", b)], [("stg", si)])
                                s.dma("sp", qkT_d[idx, h, :, tc * 512:(tc + 1) * 512], stg[si], reads=[("stg", si)])
            s.barrier()

            A.reset()
            ystage = A.alloc([TB, GW], BF16)
            msd = A.alloc([S], BF16)
            msc = A.alloc([S], BF16)
            mss = A.alloc([S], BF16)
            s.dma("pool", msd, c_msd, writes=["msd"])
            s.dma("pool", msc, c_msc, writes=["msc"])
            s.dma("pool", mss, c_mss, writes=["mss"])
            qT = [A.alloc([S], BF16) for _ in range(2)]
            kT = [A.alloc([S], BF16) for _ in range(2)]
            vx = [A.alloc([TB, 130], BF16) for _ in range(2)]
            for i in range(2):
                s.op("dve", lambda g, i=i: g.memset(vx[i][:, :, 128:129], 1.0), writes=[("vx1", i)])
            Eall = A.alloc([TB, 512], BF16)
            acc = A.alloc([4, 129], F32)
            nm = A.alloc([NBM, 8], F32)
            s.dma("sp", nm, c_nm, writes=["nm"])
            km = A.alloc([NBM], F32)
            kmh = A.alloc([NBM], BF16)
            kml = A.alloc([NBM], BF16)
            g8 = A.alloc([8], F32)
            top8 = A.alloc([8], F32)
            sel = A.alloc([TB, 8], F32)
            s.op("dve", lambda g: g.memset(g8, -1e30), writes=["g8"])
            t_e = A.alloc([512], F32)
            t_sp = A.alloc([512], F32)
            t_1 = A.alloc([512], F32)
            t_3 = A.alloc([512], F32)
            spm = A.alloc([512], BF16)
            Rb = A.alloc([512], F32)
            p3_mark = A.off
            hcount = [0]

            def load_head(m, h):
                i = hcount[0] % 2
                hcount[0] += 1
                s.dma("sp", qT[i], qkT_d[2 * m, h], writes=[("qT", i)])
                s.dma("sp", kT[i], qkT_d[2 * m + 1, h], writes=[("kT", i)])
                s.dma("sp", vx[i][:, :, 0:128],
                      v_d[m, :, h * 128:(h + 1) * 128].rearrange("(tb p) d -> p tb d", p=128), writes=[("vx", i)])
                return i

            def finalize(src_ap, den_ap, rkeys, qb, h):
                s.op("dve", lambda g: g.reciprocal(out=stt[:, 8:9], in_=den_ap), reads=rkeys, writes=[("stt", 8)])
                s.op("dve", lambda g: g.tensor_scalar(out=ystage[:, qb, h * 128:(h + 1) * 128], in0=src_ap,
                                                      scalar1=stt[:, 8:9], scalar2=None, op0=ALU.mult),
                     reads=list(rkeys) + [("stt", 8)], writes=[("ys", qb)])

            def store_mixer(mi):
                s.dma("sp", y_d[:, mi * GW:(mi + 1) * GW].rearrange("(tb p) c -> p tb c", p=128), ystage,
                      reads=[("ys", qb) for qb in range(TB)])

            mtog = [0]

            def mask_eng():
                mtog[0] ^= 1
                return "pool" if mtog[0] else "dve"

            for h in range(NH):
                i = load_head(0, h)
                hk = [("qT", i), ("kT", i), ("vx", i), ("vx1", i)]
                s.op("dve", lambda g: g.tensor_reduce(out=km, in_=kT[i].rearrange("p (n k) -> p n k", k=MOBA_BLOCK),
                                                      axis=AX.X, op=ALU.add), reads=[("kT", i)], writes=["km"])
                s.op("dve", lambda g: g.tensor_scalar(out=kmh, in0=km, scalar1=1.0 / MOBA_BLOCK, scalar2=None,
                                                      op0=ALU.mult), reads=["km"], writes=["kmh"])
                s.op("dve", lambda g: g.scalar_tensor_tensor(out=kml, in0=km, scalar=1.0 / MOBA_BLOCK, in1=kmh,
                                                             op0=ALU.mult, op1=ALU.subtract),
                     reads=["km", "kmh"], writes=["kml"])
                for tb in range(TB):
                    b = PS()
                    mm_group(b, NBM, [(qT[i][:, tb * 128:(tb + 1) * 128], kmh),
                                      (qT[i][:, tb * 128:(tb + 1) * 128], kml)], [("qT", i), "kmh", "kml"])
                    blk = tb // 2
                    s.op("dve", lambda g: g.tensor_tensor(out=g8[:, 0:NBM], in0=ps[b][:, 0:NBM], in1=nm[:, blk, 0:NBM],
                                                          op=ALU.add), reads=[("ps", b), "nm"], writes=["g8"])
                    s.op("dve", lambda g: g.max(out=top8, in_=g8), reads=["g8"], writes=["top8"])
                    s.op("dve", lambda g: g.tensor_scalar(out=sel[:, tb, :], in0=g8, scalar1=top8[:, 2:3], scalar2=None,
                                                          op0=ALU.is_ge), reads=["g8", "top8"], writes=[("sel", tb)])
                for qc in range(NQC):
                    for n in range(2 * qc + 2):
                        q0 = max(qc * 512, n * 256)
                        W = (qc + 1) * 512 - q0
                        c0 = q0 - qc * 512
                        own = (n >= 2 * qc)
                        for j in range(2):
                            kb = 2 * n + j
                            b = PS()
                            mm_group(b, W, [(kT[i][:, kb * 128:(kb + 1) * 128], qT[i][:, q0:q0 + W])], hk)
                            s.op("act", lambda g: g.activation(out=Eall[:, j, c0:512], in_=ps[b][:, 0:W], func=AF.Exp,
                                                               scale=scale), reads=[("ps", b)], writes=[("E", j)])
                            if own and j == 0:
                                s.op(mask_eng(), lambda g: g.tensor_tensor(out=Eall[:, 0, c0:c0 + 256], in0=Eall[:, 0, c0:c0 + 256],
                                                                           in1=msc[:, 0:256], op=ALU.mult),
                                     reads=[("E", 0), "msc"], writes=[("E", 0)])
                            elif own:
                                s.op("pool", lambda g: g.memset(Eall[:, 1, c0:c0 + 128], 0.0), reads=[("E", 1)], writes=[("E", 1)])
                                s.op(mask_eng(), lambda g: g.tensor_tensor(out=Eall[:, 1, c0 + 128:c0 + 256],
                                                                           in0=Eall[:, 1, c0 + 128:c0 + 256],
                                                                           in1=msc[:, 0:128], op=ALU.mult),
                                     reads=[("E", 1), "msc"], writes=[("E", 1)])
                        for sub in range(c0 // 128, 4):
                            qb = qc * 4 + sub
                            b = PS()
                            mm_group(b, 129, [(Eall[:, 0, sub * 128:(sub + 1) * 128], vx[i][:, 2 * n, 0:129]),
                                              (Eall[:, 1, sub * 128:(sub + 1) * 128], vx[i][:, 2 * n + 1, 0:129])],
                                     [("E", 0), ("E", 1)] + hk)
                            ak = ("acc", sub)
                            if n == 0:
                                if qb // 2 == 0:
                                    s.op("dve", lambda g: g.tensor_copy(out=acc[:, sub, :], in_=ps[b][:, 0:129]),
                                         reads=[("ps", b)], writes=[ak])
                                else:
                                    s.op("dve", lambda g: g.tensor_scalar(out=acc[:, sub, :], in0=ps[b][:, 0:129],
                                                                          scalar1=sel[:, qb, 0:1], scalar2=None, op0=ALU.mult),
                                         reads=[("ps", b), ("sel", qb)], writes=[ak])
                            elif n == qb // 2:
                                s.op("dve", lambda g: g.tensor_tensor(out=acc[:, sub, :], in0=ps[b][:, 0:129],
                                                                      in1=acc[:, sub, :], op=ALU.add),
                                     reads=[("ps", b), ak], writes=[ak])
                            else:
                                s.op("dve", lambda g: g.scalar_tensor_tensor(out=acc[:, sub, :], in0=ps[b][:, 0:129],
                                                                             scalar=sel[:, qb, n:n + 1], in1=acc[:, sub, :],
                                                                             op0=ALU.mult, op1=ALU.add),
                                     reads=[("ps", b), ak, ("sel", qb)], writes=[ak])
                    for sub in range(4):
                        finalize(acc[:, sub, 0:128], acc[:, sub, 128:129], [("acc", sub)], qc * 4 + sub, h)
            store_mixer(0)

            A.reset(p3_mark)
            spf = A.alloc([TB, 512], F32)
            spmA = A.alloc([TB, 512], BF16)
            t_e2 = [t_e, A.alloc([512], F32)]
            t_12 = [t_1, A.alloc([512], F32)]
            t_32 = [t_3, A.alloc([512], F32)]
            bi = 0
            for h in range(NH):
                i = load_head(1, h)
                hk = [("qT", i), ("kT", i), ("vx", i)]
                for qc in range(NQC):
                    nkb = (qc + 1) * 4
                    s.op("pool", lambda g: g.memset(spmA[:, 4 * qc:4 * qc + 4, :], 0.0),
                         writes=[("spm", kb) for kb in range(4 * qc, 4 * qc + 4)])
                    geo = {}
                    for kb in range(nkb):
                        q0 = max(qc * 512, kb * 128)
                        geo[kb] = (q0, (qc + 1) * 512 - q0, q0 - qc * 512, kb * 128 >= qc * 512)
                    for kb in range(nkb):
                        q0, W, c0, diag = geo[kb]
                        bi ^= 1
                        te, tek = t_e2[bi], ("t_e", bi)
                        bz = PS()
                        mm_group(bz, W, [(kT[i][:, kb * 128:(kb + 1) * 128], qT[i][:, q0:q0 + W])], hk)
                        s.op("act", lambda g: g.activation(out=te[:, 0:W], in_=ps[bz][:, 0:W], func=AF.Exp, scale=scale),
                             reads=[("ps", bz)], writes=[tek])
                        s.op("act", lambda g: g.activation(out=spf[:, kb, c0:512], in_=te[:, 0:W], func=AF.Ln, bias=1.0),
                             reads=[tek], writes=[("spf", kb)])
                        if diag:
                            s.op("dve", lambda g: g.tensor_tensor(out=spmA[:, kb, c0:c0 + 128], in0=spf[:, kb, c0:c0 + 128],
                                                                  in1=mss[:, 0:128], op=ALU.mult),
                                 reads=[("spf", kb), "mss"], writes=[("spm", kb)])
                            if W > 128:
                                s.op("pool", lambda g: g.tensor_copy(out=spmA[:, kb, c0 + 128:512], in_=spf[:, kb, c0 + 128:512]),
                                     reads=[("spf", kb)], writes=[("spm2", kb)])
                        else:
                            s.op("pool", lambda g: g.tensor_copy(out=spmA[:, kb, :], in_=spf[:, kb, :]),
                                 reads=[("spf", kb)], writes=[("spm", kb), ("spm2", kb)])
                    for kb in range(nkb):
                        q0, W, c0, diag = geo[kb]
                        bi ^= 1
                        t1, t1k = t_12[bi], ("t_1", bi)
                        t3, t3k = t_32[bi], ("t_3", bi)
                        bc = PS()
                        mm_group(bc, W, [(trib[:], spmA[:, kb, c0:512])] +
                                 [(onesb[:], spmA[:, k2, c0:512]) for k2 in range(kb + 1, nkb)],
                                 ["trib", "onesb"] + [("spm", k2) for k2 in range(kb, nkb)] + [("spm2", k2) for k2 in range(kb, nkb)])
                        bz = PS()
                        mm_group(bz, W, [(kT[i][:, kb * 128:(kb + 1) * 128], qT[i][:, q0:q0 + W])], hk)
                        s.op("dve", lambda g: g.tensor_tensor(out=t1[:, 0:W], in0=ps[bc][:, 0:W], in1=spf[:, kb, c0:512],
                                                              op=ALU.add), reads=[("ps", bc), ("spf", kb)], writes=[t1k])
                        s.op("dve", lambda g: g.scalar_tensor_tensor(out=t3[:, 0:W], in0=ps[bz][:, 0:W], scalar=scale,
                                                                     in1=t1[:, 0:W], op0=ALU.mult, op1=ALU.subtract),
                             reads=[("ps", bz), t1k], writes=[t3k])
                        s.op("act", lambda g: g.activation(out=Eall[:, kb, c0:512], in_=t3[:, 0:W], func=AF.Exp),
                             reads=[t3k], writes=[("E", kb)])
                        if diag:
                            s.op("pool", lambda g: g.tensor_tensor(out=Eall[:, kb, c0:c0 + 128], in0=Eall[:, kb, c0:c0 + 128],
                                                                   in1=mss[:, 0:128], op=ALU.mult),
                                 reads=[("E", kb), "mss"], writes=[("E", kb)])
                    for sub in range(4):
                        qb = qc * 4 + sub
                        b = PS()
                        mm_group(b, 128, [(Eall[:, kb, sub * 128:(sub + 1) * 128], vx[i][:, kb, 0:128]) for kb in range(qb + 1)],
                                 [("E", kb) for kb in range(qb + 1)] + hk)
                        copy_op(EV(), ystage[:, qb, h * 128:(h + 1) * 128], ps[b][:, 0:128], [("ps", b)], [("ys", qb)])
            store_mixer(1)

            for h in range(NH):
                i = load_head(2, h)
                hk = [("qT", i), ("kT", i), ("vx", i), ("vx1", i)]
                for qc in range(NQC):
                    nkb = (qc + 1) * 4
                    for kb in range(nkb):
                        q0 = max(qc * 512, kb * 128)
                        W = (qc + 1) * 512 - q0
                        c0 = q0 - qc * 512
                        b = PS()
                        mm_group(b, W, [(kT[i][:, kb * 128:(kb + 1) * 128], qT[i][:, q0:q0 + W])], hk)
                        s.op("act", lambda g: g.activation(out=Eall[:, kb, c0:512], in_=ps[b][:, 0:W], func=AF.Exp, scale=scale),
                             reads=[("ps", b)], writes=[("E", kb)])
                        mo = q0 - 128 * kb
                        s.op(mask_eng(), lambda g: g.tensor_tensor(out=Eall[:, kb, c0:512], in0=Eall[:, kb, c0:512],
                                                                   in1=msd[:, mo:mo + W], op=ALU.mult),
                             reads=[("E", kb), "msd"], writes=[("E", kb)])
                    for sub in range(4):
                        qb = qc * 4 + sub
                        b = PS()
                        mm_group(b, 129, [(Eall[:, kb, sub * 128:(sub + 1) * 128], vx[i][:, kb, 0:129]) for kb in range(qb + 1)],
                                 [("E", kb) for kb in range(qb + 1)] + hk)
                        finalize(ps[b][:, 0:128], ps[b][:, 128:129], [("ps", b)], qb, h)
            store_mixer(3)

            s.barrier()
            A.reset(p3_mark)
            up = A.alloc([16 + S], F32)
            pa = A.alloc([16 + S], F32)
            pb = A.alloc([16 + S], F32)
            coef = [A.alloc([S], F32) for _ in range(4)]
            tmpf = A.alloc([S], F32)
            tmp2 = A.alloc([S], F32)
            dT = A.alloc([PC, S], BF16)
            wp = A.alloc([PC, PGD], BF16)
            pst = A.alloc([GW], F32)
            s.dma("sp", pst, psb[l], writes=["pst"])
            for t_, k_ in ((up, "up"), (pa, "pa"), (pb, "pb")):
                s.op("dve", lambda g, t_=t_: g.memset(t_[:, 0:16], 0.0), writes=[k_ + "z"])
            for gi in range(PGn):
                s.dma("pool", wp, w_pool[l, gi].rearrange("(c p) d -> p c d", p=128), writes=["wp"])
                for wi in range(4):
                    s.dma("sp", coef[wi], c_pcoef[gi, wi], writes=[("coef", wi)])
                for c in range(PC):
                    ch = gi * PC + c
                    s.dma("sp", up[:, 16:16 + S], u_d[ch], writes=["up"])
                    cur, ck = up, "up"
                    for wi in range(4):
                        sh = 1 << wi
                        nxt, nk = (pa, "pa") if cur is not pa else (pb, "pb")
                        s.op("dve", lambda g, cur=cur, nxt=nxt, sh=sh: g.tensor_tensor(
                            out=nxt[:, 16:16 + S], in0=cur[:, 16:16 + S], in1=cur[:, 16 - sh:16 - sh + S], op=ALU.add),
                             reads=[ck, ck + "z"], writes=[nk])
                        cur, ck = nxt, nk
                        if wi == 0:
                            s.op("pool", lambda g, cur=cur: g.tensor_tensor(out=tmpf, in0=cur[:, 16:16 + S], in1=coef[0],
                                                                            op=ALU.mult), reads=[ck, ("coef", 0)], writes=["tmpf"])
                        else:
                            s.op("pool", lambda g, cur=cur: g.tensor_tensor(out=tmp2, in0=cur[:, 16:16 + S], in1=coef[wi],
                                                                            op=ALU.mult), reads=[ck, ("coef", wi)], writes=["tmp2"])
                            s.op("pool", lambda g: g.tensor_tensor(out=tmpf, in0=tmpf, in1=tmp2, op=ALU.add),
                                 reads=["tmpf", "tmp2"], writes=["tmpf"])
                    s.op("dve", lambda g: g.tensor_tensor(out=dT[:, c, :], in0=tmpf, in1=up[:, 16:16 + S],
                                                          op=ALU.subtract), reads=["tmpf", "up"], writes=[("dT", c)])
                for tb in range(TB):
                    b = PS()
                    mm_group(b, PGD, [(dT[:, c, tb * 128:(tb + 1) * 128], wp[:, c, :]) for c in range(PC)],
                             [("dT", c) for c in range(PC)] + ["wp"])
                    s.op("dve", lambda g: g.tensor_tensor(out=ystage[:, tb, gi * PGD:(gi + 1) * PGD], in0=ps[b][:, 0:PGD],
                                                          in1=pst[:, gi * PGD:(gi + 1) * PGD], op=ALU.mult),
                         reads=[("ps", b), "pst"], writes=[("ys", tb)])
            store_mixer(2)
            if NR > 1:
                yrc = all_gather(y_d, yg_d, S, MWL)
            else:
                yrc = S
                s.barrier()

            def yrow(r, t):
                return (t // yrc) * NR * yrc + r * yrc + t % yrc

            dst = out if l == L - 1 else xres_d
            LOW = 0
            MID1 = 32 * 1024
            MID2 = 64 * 1024
            MSB_OFF = ARENA_BYTES - 4 * D * 4
            ACT_OFF = ARENA_BYTES - ((FC * TG * 2 + 31) // 32) * 32
            assert KC2 * TG * 2 <= MID1 and KC * TG * 2 <= MID1 and 4 * D * 4 <= MID2
            for tg in range(NTG):
                yT, _ = A.at(LOW, [KC2, TG], BF16)
                A.reset(MID1)
                gbm = A.alloc([MW], F32)
                yt = A.alloc([MW], BF16)
                yt1 = A.alloc([MW], BF16)
                yb = A.alloc([MW], BF16)
                assert A.off <= MSB_OFF
                s.dma("sp", gbm, gb_mo[l], writes=["gbm"])
                ytk = ["yt"] if NR == 1 else [("ytp", 0, r) for r in range(NR)]
                for tb in range(4):
                    T0 = tg * TG + tb * 128
                    if NR == 1:
                        s.dma("sp", yt, y_d[T0:T0 + 128, :], writes=["yt"])
                    else:
                        for r in range(NR):
                            s.dma("sp", yt[:, r * MWL:(r + 1) * MWL], yg_d[yrow(r, T0):yrow(r, T0) + 128, :], writes=[("ytp", 0, r)])
                            s.dma("sp", yt1[:, r * MWL:(r + 1) * MWL], yg_d[yrow(r, TO + T0):yrow(r, TO + T0) + 128, :],
                                  writes=[("ytp", 1, r)])
                        s.op("pool", lambda g: g.tensor_scalar(out=yt, in0=yt, scalar1=abt[:, 0:1], scalar2=None, op0=ALU.mult),
                             reads=ytk + ["abt"], writes=ytk)
                        s.op("dve", lambda g: g.scalar_tensor_tensor(out=yt, in0=yt1, scalar=abt[:, 1:2], in1=yt,
                                                                     op0=ALU.mult, op1=ALU.add),
                             reads=[("ytp", 1, r) for r in range(NR)] + ["abt"] + ytk, writes=ytk)
                    for gi, ranges in enumerate(groups):
                        for ri, (c0, cl) in enumerate(ranges):
                            col = 12 + ri
                            s.op("act", lambda g: g.activation(out=yb[:, c0:c0 + cl], in_=yt[:, c0:c0 + cl],
                                                               func=AF.Square, accum_out=stt[:, col:col + 1]),
                                 reads=ytk, writes=[("yb", gi), ("stt", col)])
                        if len(ranges) == 2:
                            s.op("dve", lambda g: g.tensor_tensor(out=stt[:, 12:13], in0=stt[:, 12:13], in1=stt[:, 13:14],
                                                                  op=ALU.add),
                                 reads=[("stt", 12), ("stt", 13)], writes=[("stt", 12)])
                        s.op("act", lambda g: g.activation(out=stt[:, 14:15], in_=stt[:, 12:13], func=AF.Sqrt,
                                                           scale=1.0 / NGW, bias=epsb[:]),
                             reads=[("stt", 12), "epsb"], writes=[("stt", 14)])
                        s.op("dve", lambda g: g.reciprocal(out=stt[:, 16 + gi:17 + gi], in_=stt[:, 14:15]),
                             reads=[("stt", 14)], writes=[("stt", 16 + gi)])
                        for (c0, cl) in ranges:
                            s.op("dve", lambda g: g.scalar_tensor_tensor(out=yb[:, c0:c0 + cl], in0=yt[:, c0:c0 + cl],
                                                                         scalar=stt[:, 16 + gi:17 + gi],
                                                                         in1=gbm[:, c0:c0 + cl], op0=ALU.mult, op1=ALU.mult),
                                 reads=ytk + [("stt", 16 + gi), "gbm"], writes=[("yb", gi)])
                    transposeT(yb, [("yb", gi) for gi in range(len(groups))], KC2, yT, ("yT", tb), tb * 128)
                s.barrier()
                msb, _ = A.at(MSB_OFF, [4, D], F32)
                A.reset(MID1)
                wobuf = [A.alloc([KC2, 512], BF16) for _ in range(2)]
                assert A.off <= MSB_OFF
                for nt in range(D // 512):
                    wb = wobuf[nt % 2]
                    wk = ("wo", nt % 2)
                    s.dma("pool", wb, w_out[l, :, nt * 512:(nt + 1) * 512].rearrange("(k p) n -> p k n", p=128), writes=[wk])
                    for tb in range(4):
                        b = PS()
                        mm_group(b, 512, [(yT[:, k, tb * 128:(tb + 1) * 128], wb[:, k, :]) for k in range(KC2)],
                                 [("yT", tb), wk])
                        copy_op(EV(), msb[:, tb, nt * 512:(nt + 1) * 512], ps[b][:], [("ps", b)], [("msb", tb)])
                s.barrier()
                h2T, _ = A.at(LOW, [KC, TG], BF16)
                A.reset(MID1)
                gb1 = A.alloc([D], F32)
                gb2 = A.alloc([D], F32)
                xt = A.alloc([D], F32)
                xb = A.alloc([D], BF16)
                assert A.off <= MSB_OFF
                s.dma("sp", gb1, gb_post[l], writes=["gb1"])
                s.dma("sp", gb2, gb_fpre[l], writes=["gb2"])
                for tb in range(4):
                    T0 = tg * TG + tb * 128
                    s.dma("sp", xt, xsrc[T0:T0 + 128, :], writes=["xt"])
                    r_ap, rk = rstd(msb[:, tb, :], xb, 0, D, [("msb", tb)], ["xb"])
                    s.op("dve", lambda g: g.scalar_tensor_tensor(out=msb[:, tb, :], in0=msb[:, tb, :], scalar=r_ap,
                                                                 in1=gb1, op0=ALU.mult, op1=ALU.mult),
                         reads=[("msb", tb), rk, "gb1"], writes=[("msb", tb)])
                    s.op("pool", lambda g: g.tensor_tensor(out=xt, in0=xt, in1=msb[:, tb, :], op=ALU.add),
                         reads=["xt", ("msb", tb)], writes=["xt"])
                    s.dma("sp", xres_d[T0:T0 + 128, :], xt, reads=["xt"])
                    r_ap, rk = rstd(xt, xb, 4, D, ["xt"], ["xb"])
                    s.op("dve", lambda g: g.scalar_tensor_tensor(out=xb, in0=xt, scalar=r_ap, in1=gb2,
                                                                 op0=ALU.mult, op1=ALU.mult),
                         reads=["xt", rk, "gb2"], writes=["xb"])
                    transposeT(xb, ["xb"], KC, h2T, ("h2T", tb), tb * 128)
                s.barrier()
                actT, _ = A.at(ACT_OFF, [FC, TG], BF16)
                A.reset(MID1)
                CT2 = 256
                wgu = [A.alloc([KC, CT2], BF16) for _ in range(3)]
                sg = A.alloc([2, TG], F32)
                assert A.off <= ACT_OFF
                wi = 0
                h2keys = [("h2T", tb) for tb in range(4)]
                for j in range(DFF // CT2):
                    wg, wgk = wgu[wi % 3], ("wgu", wi % 3)
                    wi += 1
                    s.dma("pool", wg, w_gate[l, :, j * CT2:(j + 1) * CT2].rearrange("(k p) n -> p k n", p=128), writes=[wgk])
                    wu, wuk = wgu[wi % 3], ("wgu", wi % 3)
                    wi += 1
                    s.dma("pool", wu, w_up[l, :, j * CT2:(j + 1) * CT2].rearrange("(k p) n -> p k n", p=128), writes=[wuk])
                    for c in range(2):
                        b = PS()
                        mm_group(b, TG, [(wg[:, k, c * 128:(c + 1) * 128], h2T[:, k, :]) for k in range(KC)], h2keys + [wgk])
                        s.op("act", lambda g: g.activation(out=sg[:, c, :], in_=ps[b][:], func=AF.Silu),
                             reads=[("ps", b)], writes=[("sg", c)])
                    for c in range(2):
                        b = PS()
                        mm_group(b, TG, [(wu[:, k, c * 128:(c + 1) * 128], h2T[:, k, :]) for k in range(KC)], h2keys + [wuk])
                        s.op("dve", lambda g: g.tensor_tensor(out=actT[:, 2 * j + c, :], in0=ps[b][:], in1=sg[:, c, :],
                                                              op=ALU.mult),
                             reads=[("ps", b), ("sg", c)], writes=[("actT", 2 * j + c)])
                s.barrier()
                fsb, _ = A.at(LOW, [4, D], F32)
                A.reset(MID2)
                FG = 8
                wdb = [A.alloc([FG, 512], BF16) for _ in range(3)]
                assert A.off <= ACT_OFF
                nfg = (FC + FG - 1) // FG
                wdi = 0
                for nt in range(D // 512):
                    banks = [PS() for _ in range(4)]
                    for fg in range(nfg):
                        f0 = fg * FG
                        nf = min(FG, FC - f0)
                        wd, wdk = wdb[wdi % 3], ("wd", wdi % 3)
                        wdi += 1
                        s.dma("pool", wd[:, 0:nf, :],
                              w_down[l, f0 * 128:(f0 + nf) * 128, nt * 512:(nt + 1) * 512].rearrange("(c p) n -> p c n", p=128),
                              writes=[wdk])
                        for tb in range(4):
                            fns = [(lambda e, fc=fc, tb=tb, wd=wd: e.matmul(ps[banks[tb]][:], lhsT=actT[:, fc, tb * 128:(tb + 1) * 128],
                                                                            rhs=wd[:, fc - f0, :], start=(fc == 0), stop=(fc == FC - 1)))
                                   for fc in range(f0, f0 + nf)]
                            s.group("pe", fns, reads=[("actT", fc) for fc in range(f0, f0 + nf)] + [wdk],
                                    writes=[("ps", banks[tb])])
                    for tb in range(4):
                        copy_op(EV(), fsb[:, tb, nt * 512:(nt + 1) * 512], ps[banks[tb]][:], [("ps", banks[tb])], [("fsb", tb)])
                s.barrier()
                A.reset(MID2)
                gb1 = A.alloc([D], F32)
                xt = A.alloc([D], F32)
                xb = A.alloc([D], BF16)
                s.dma("sp", gb1, gb_fpost[l], writes=["gb1"])
                for tb in range(4):
                    T0 = tg * TG + tb * 128
                    s.dma("sp", xt, xres_d[T0:T0 + 128, :], writes=["xt"])
                    r_ap, rk = rstd(fsb[:, tb, :], xb, 0, D, [("fsb", tb)], ["xb"])
                    s.op("dve", lambda g: g.scalar_tensor_tensor(out=fsb[:, tb, :], in0=fsb[:, tb, :], scalar=r_ap,
                                                                 in1=gb1, op0=ALU.mult, op1=ALU.mult),
                         reads=[("fsb", tb), rk, "gb1"], writes=[("fsb", tb)])
                    s.op("pool", lambda g: g.tensor_tensor(out=xt, in0=xt, in1=fsb[:, tb, :], op=ALU.add),
                         reads=["xt", ("fsb", tb)], writes=["xt"])
                    s.dma("sp", dst[T0:T0 + 128, :], xt, reads=["xt"])
                s.barrier()
        s.finish()
    print("instructions:", s.n_inst, "sems:", s.nsem)
    return nc


def make_consts(S):
    kin = np.arange(128)[:, None]
    j = np.arange(S)[None, :]
    delta = j - kin
    msd = np.zeros((128, S), np.float32)
    for window, dil in DILATED_PATTERNS:
        msd += ((delta >= 0) & (delta <= window) & (delta % dil == 0)).astype(np.float32)
    msc = (delta >= 0).astype(np.float32)
    mss = (delta > 0).astype(np.float32)
    nbm = S // MOBA_BLOCK
    nm = np.zeros((128, nbm, 8), np.float32)
    for blk in range(nbm):
        for n in range(8):
            if n >= blk:
                nm[:, blk, n] = -1e30
    jj = np.arange(128)[:, None]
    ss = np.arange(128)[None, :]
    tri = (jj > ss).astype(np.float32)
    return {"c_ident": np.eye(128, dtype=np.float32), "c_tri": tri, "c_msd": msd, "c_msc": msc, "c_mss": mss, "c_nm": nm}


def pool_coef(S, windows):
    t = np.arange(S)
    out = np.zeros((len(windows), 4, 128, S), np.float32)
    for g, w in enumerate(windows):
        wi = {2: 0, 4: 1, 8: 2, 16: 3}[w]
        out[g, wi] = np.broadcast_to(1.0 / np.minimum(w, t + 1).astype(np.float32), (128, S))
    return out


def bcast128(v):
    return np.ascontiguousarray(np.broadcast_to(v[:, None, :], (v.shape[0], 128, v.shape[1]))).astype(np.float32)


_NC_CACHE = {}


def kernel(x, ln_mix_pre, w_in, w_pool, pool_scale, mix_out_norm, w_out, ln_mix_post, ln_ffn_pre,
           w_gate, w_up, w_down, ln_ffn_post):
    x = np.asarray(x, np.float32)
    B, S, D = x.shape
    L = w_in.shape[0]
    DFF = w_gate.shape[2]
    GWF = D // 4
    NR = 2
    GW = GWF // NR
    NH = GW // 128
    TO = S // NR
    PGD = GWF // 4
    PGn = 4 // NR
    n_cores = B * NR
    perm = np.concatenate([np.arange(m * GWF + r * GW, m * GWF + (r + 1) * GW) for r in range(NR) for m in range(4)])
    groups = [[(r * 4 * GW + m * GW, GW) for r in range(NR)] for m in range(4)]
    cfg = dict(S=S, D=D, NH=NH, DFF=DFF, TO=TO, L=L, PGn=PGn, PGD=PGD, NGW=GWF, windows=None, groups=groups,
               NR=NR, RG=[[2 * i, 2 * i + 1] for i in range(B)])
    key = (S, D, NH, DFF, L, NR, B)
    if key not in _NC_CACHE:
        _NC_CACHE[key] = build(cfg)
    nc = _NC_CACHE[key]
    w_in = np.asarray(w_in, np.float32)
    common = {
        "w_out": np.ascontiguousarray(np.asarray(w_out, np.float32)[:, perm, :]),
        "w_gate": np.ascontiguousarray(w_gate, np.float32), "w_up": np.ascontiguousarray(w_up, np.float32),
        "w_down": np.ascontiguousarray(w_down, np.float32),
        "gb_pre": bcast128(np.asarray(ln_mix_pre)), "gb_post": bcast128(np.asarray(ln_mix_post)),
        "gb_fpre": bcast128(np.asarray(ln_ffn_pre)), "gb_fpost": bcast128(np.asarray(ln_ffn_post)),
        "gb_mo": bcast128(np.asarray(mix_out_norm)[:, perm]),
    }
    common.update(make_consts(S))
    per_rank = []
    for r in range(NR):
        cols = np.concatenate([np.arange(sg * GWF + r * GW, sg * GWF + (r + 1) * GW) for sg in range(10)])
        ab = np.zeros((128, 2), np.float32)
        ab[:, r] = 1.0
        per_rank.append({
            "w_in": np.ascontiguousarray(w_in[:, :, cols]),
            "w_pool": np.ascontiguousarray(np.asarray(w_pool, np.float32)[:, r * PGn:(r + 1) * PGn]),
            "psb": bcast128(np.asarray(pool_scale)[:, r * GW:(r + 1) * GW]),
            "c_pcoef": pool_coef(S, POOL_WINDOWS[r * PGn:(r + 1) * PGn]),
            "c_ab": ab,
        })
    in_maps = []
    for c in range(n_cores):
        b, r = divmod(c, NR)
        m = dict(common)
        m.update(per_rank[r])
        m["x_seq"] = np.ascontiguousarray(x[b, r * TO:(r + 1) * TO])
        in_maps.append(m)
    res = run_bass_kernel_spmd(nc, in_maps, core_ids=list(range(n_cores)))
    out = np.empty((B, S, D), np.float32)
    for c in range(n_cores):
        b, r = divmod(c, NR)
        out[b, r * TO:(r + 1) * TO] = res.results[c]["out"]
    return out
```

```python
import contextlib
import numpy as np
import concourse.bass as bass
import concourse.mybir as mybir
from concourse.bass_utils import run_bass_kernel_spmd

F32 = mybir.dt.float32
BF16 = mybir.dt.bfloat16
ALU = mybir.AluOpType
AF = mybir.ActivationFunctionType
AX = mybir.AxisListType

SEM_CAP = 30000
RMS_EPS = 1e-6
MOBA_BLOCK = 256
POOL_WINDOWS = (2, 4, 8, 16)
DILATED_PATTERNS = ((128, 1), (512, 4), (2048, 16))


class Sched:
    ENG = ("pe", "act", "dve", "pool", "sp")

    def __init__(self, nc, stack, n_lanes=8):
        self.nc = nc
        self.stack = stack
        self.engs = {"pe": nc.tensor, "act": nc.scalar, "dve": nc.vector, "pool": nc.gpsimd, "sp": nc.sync}
        self.prog = {}
        self.seen = {e: {} for e in self.ENG}
        self.nsem = 0
        self.pe_sems = set()
        for e in self.ENG:
            self.prog[e] = [self._newsem("p_" + e), 0]
        self.pe_sems.add(self.prog["pe"][0].num)
        self.lanes = {}
        for e in ("sp", "pool"):
            self.lanes[e] = [[self._newsem("l_%s%d" % (e, i)), 0] for i in range(n_lanes)]
        self.lane_rr = {e: 0 for e in self.lanes}
        self.all_sems = {}
        self.last_w = {}
        self.readers = {}
        self.n_inst = {e: 0 for e in self.ENG}

    def _newsem(self, name):
        self.nsem += 1
        return self.stack.enter_context(self.nc.semaphore("%s_%d" % (name, self.nsem)))

    def _emit(self, e, fn):
        self.n_inst[e] += 1
        return fn(self.engs[e])

    def _wait(self, e, tok):
        if tok is None:
            return
        sem, val = tok
        if e == "pe" and sem.num in self.pe_sems:
            return
        if self.seen[e].get(sem.num, 0) >= val:
            return
        self.seen[e][sem.num] = val
        self._emit(e, lambda eng: eng.wait_ge(sem, val))

    def _deps(self, e, reads, writes):
        for k in reads:
            self._wait(e, self.last_w.get(k))
        for k in writes:
            self._wait(e, self.last_w.get(k))
            for t in self.readers.get(k, ()):
                self._wait(e, t)

    def _commit(self, tok, reads, writes):
        self.all_sems[tok[0].num] = tok
        for k in writes:
            self.last_w[k] = tok
            self.readers[k] = []
        for k in reads:
            self.readers.setdefault(k, []).append(tok)

    def _next_tok(self, e):
        p = self.prog[e]
        if p[1] >= SEM_CAP:
            p[0] = self._newsem("p_" + e)
            p[1] = 0
            if e == "pe":
                self.pe_sems.add(p[0].num)
        p[1] += 1
        return (p[0], p[1])

    def op(self, e, fn, reads=(), writes=()):
        self._deps(e, reads, writes)
        tok = self._next_tok(e)
        self._emit(e, lambda eng: fn(eng).then_inc(tok[0], 1))
        self._commit(tok, reads, writes)
        return tok

    def group(self, e, fns, reads=(), writes=()):
        self._deps(e, reads, writes)
        tok = self._next_tok(e)
        for fn in fns[:-1]:
            self._emit(e, fn)
        self._emit(e, lambda eng: fns[-1](eng).then_inc(tok[0], 1))
        self._commit(tok, reads, writes)
        return tok

    def dma(self, e, out, in_, reads=(), writes=()):
        lanes = self.lanes[e]
        i = self.lane_rr[e]
        self.lane_rr[e] = (i + 1) % len(lanes)
        ln = lanes[i]
        if ln[1] >= SEM_CAP // 16:
            ln[0] = self._newsem("l_" + e)
            ln[1] = 0
        if ln[1] > 0:
            self._wait(e, (ln[0], 16 * ln[1]))
        self._deps(e, reads, writes)
        ln[1] += 1
        tok = (ln[0], 16 * ln[1])
        self._emit(e, lambda eng: eng.dma_start(out=out, in_=in_).then_inc(tok[0], 16))
        self._commit(tok, reads, writes)
        return tok

    def barrier(self, engines=None):
        toks = list(self.all_sems.values())
        for e in (engines or self.ENG):
            for tok in toks:
                self._wait(e, tok)

    def finish(self):
        self.barrier(engines=("sp",))


ARENA_BYTES = 200 * 1024
CC_MAX_BYTES = 2 * 1024 * 1024


def build(cfg):
    S, D, NH, DFF, TO, L = cfg["S"], cfg["D"], cfg["NH"], cfg["DFF"], cfg["TO"], cfg["L"]
    PGn, PGD, NGW = cfg["PGn"], cfg["PGD"], cfg["NGW"]
    windows = cfg["windows"]
    groups = cfg["groups"]
    debug = cfg.get("debug", False)
    NR = cfg.get("NR", 1)
    RG = cfg.get("RG", None)
    KC = D // 128
    GW = NH * 128
    INW = 10 * GW
    MWL = 4 * GW
    MW = NR * MWL
    KC2 = MW // 128
    TB = S // 128
    FC = DFF // 128
    PC = PGD // 128
    NBM = S // MOBA_BLOCK
    NQC = S // 512
    scale = 128 ** -0.5
    TG = 512
    NTG = TO // TG

    nc = bass.Bass("TRN2", target_bir_lowering=False)

    def din(name, shape, dt=F32):
        return nc.dram_tensor(name, list(shape), dt, kind="ExternalInput").ap()

    skind = "ExternalOutput" if debug else "Internal"

    def dscr(name, shape, dt):
        return nc.dram_tensor(name, list(shape), dt, kind=skind).ap()

    x_seq = din("x_seq", [TO, D])
    c_ab = din("c_ab", [128, 2])
    w_in = din("w_in", [L, D, INW])
    w_out = din("w_out", [L, MW, D])
    w_gate = din("w_gate", [L, D, DFF])
    w_up = din("w_up", [L, D, DFF])
    w_down = din("w_down", [L, DFF, D])
    w_pool = din("w_pool", [L, PGn, PGD, PGD])
    gb_pre = din("gb_pre", [L, 128, D])
    gb_post = din("gb_post", [L, 128, D])
    gb_fpre = din("gb_fpre", [L, 128, D])
    gb_fpost = din("gb_fpost", [L, 128, D])
    gb_mo = din("gb_mo", [L, 128, MW])
    psb = din("psb", [L, 128, GW])
    c_ident = din("c_ident", [128, 128])
    c_tri = din("c_tri", [128, 128])
    c_msd = din("c_msd", [128, S])
    c_msc = din("c_msc", [128, S])
    c_mss = din("c_mss", [128, S])
    c_pcoef = din("c_pcoef", [PGn, 4, 128, S])
    c_nm = din("c_nm", [128, NBM, 8])
    out = nc.dram_tensor("out", [TO, D], F32, kind="ExternalOutput").ap()

    qkT_d = dscr("qkT_d", [6, NH, 128, S], BF16)
    v_d = dscr("v_d", [3, S, GW], BF16)
    u_d = dscr("u_d", [GW // 128, 128, S], F32)
    assert S * GW * 2 <= CC_MAX_BYTES
    y_d = dscr("y_d", [4, S, GW], BF16)
    yg_d = dscr("yg_d", [4, NR * S, GW], BF16)
    TCH = min(TO, max(128, (CC_MAX_BYTES // (128 * KC * 2)) // 128 * 128))
    NCH = TO // TCH
    hTo_d = dscr("hTo_d", [NCH, 128 * KC, TCH], BF16)
    hTg_d = dscr("hTg_d", [NCH, NR * 128 * KC, TCH], BF16)
    xres_d = dscr("xres_d", [TO, D], F32)

    with contextlib.ExitStack() as st:
        s = Sched(nc, st)

        def SB(name, shape, dt):
            return st.enter_context(nc.sbuf_tensor(name, list(shape), dt))

        ps = [st.enter_context(nc.psum_tensor("ps%d" % i, [128, 512], F32)) for i in range(8)]
        psb16 = [p[:].bitcast(BF16) for p in ps]
        rr = [0]

        def PS():
            i = rr[0]
            rr[0] = (i + 1) % 8
            return i

        evt = [0]

        def EV():
            evt[0] ^= 1
            return "act" if evt[0] else "dve"

        def copy_op(e, out_ap, in_ap, reads, writes):
            if e == "act":
                s.op("act", lambda g: g.copy(out=out_ap, in_=in_ap), reads=reads, writes=writes)
            else:
                s.op(e, lambda g: g.tensor_copy(out=out_ap, in_=in_ap), reads=reads, writes=writes)

        idb = SB("idb", [128, 128], BF16)
        trib = SB("trib", [128, 128], BF16)
        onesb = SB("onesb", [128, 128], BF16)
        epsb = SB("epsb", [128, 1], F32)
        stt = SB("stt", [128, 64], F32)
        arena = SB("arena", [128, ARENA_BYTES // 2], BF16)
        s.dma("pool", idb[:], c_ident, writes=["idb"])
        s.dma("pool", trib[:], c_tri, writes=["trib"])
        s.op("dve", lambda g: g.memset(onesb[:], 1.0), writes=["onesb"])
        s.op("dve", lambda g: g.memset(epsb[:], RMS_EPS), writes=["epsb"])

        class Arena:
            def __init__(self):
                self.off = 0

            def at(self, off, shape, dt):
                n = int(np.prod(shape))
                nb = n * (2 if dt == BF16 else 4)
                assert off % 4 == 0 and off + nb <= ARENA_BYTES, (off, nb)
                if dt == BF16:
                    ap = arena[:, off // 2: off // 2 + n]
                else:
                    ap = arena[:, off // 2: off // 2 + 2 * n].bitcast(F32)
                if len(shape) == 2:
                    ap = ap.rearrange("p (a b) -> p a b", a=shape[0])
                return ap, off + ((nb + 31) // 32) * 32

            def reset(self, off=0):
                self.off = off

            def alloc(self, shape, dt):
                ap, self.off = self.at(self.off, shape, dt)
                return ap

        A = Arena()
        ccsem = [s._newsem("cc"), 0]
        abt = SB("abt", [128, 2], F32)
        s.dma("sp", abt[:], c_ab, writes=["abt"])

        def cc_gather(src, dst, reads, writes):
            s._deps("pool", reads, writes)
            ccsem[1] += 1
            tok = (ccsem[0], ccsem[1])
            s._emit("pool", lambda eng: eng.collective_compute(
                "AllGather", ALU.bypass, replica_groups=RG, ins=[src.opt()], outs=[dst.opt()]).then_inc(ccsem[0]))
            s._commit(tok, reads, writes)

        def rstd(src_ap, junk_ap, col, denom, rkeys, jkeys):
            s.op("act", lambda g: g.activation(out=junk_ap, in_=src_ap, func=AF.Square, accum_out=stt[:, col:col + 1]),
                 reads=rkeys, writes=list(jkeys) + [("stt", col)])
            s.op("act", lambda g: g.activation(out=stt[:, col + 1:col + 2], in_=stt[:, col:col + 1], func=AF.Sqrt,
                                               scale=1.0 / denom, bias=epsb[:]),
                 reads=[("stt", col), "epsb"], writes=[("stt", col + 1)])
            s.op("dve", lambda g: g.reciprocal(out=stt[:, col + 2:col + 3], in_=stt[:, col + 1:col + 2]),
                 reads=[("stt", col + 1)], writes=[("stt", col + 2)])
            return stt[:, col + 2:col + 3], ("stt", col + 2)

        def transposeT(src, skeys, nchunk, dst, dkey, t_off):
            for g0 in range(0, nchunk, 8):
                n = min(8, nchunk - g0)
                b = PS()
                fns = [(lambda e, j=j: e.transpose(out=psb16[b][:, j * 128:(j + 1) * 128],
                                                   in_=src[:, (g0 + j) * 128:(g0 + j + 1) * 128], identity=idb[:]))
                       for j in range(n)]
                s.group("pe", fns, reads=list(skeys) + ["idb"], writes=[("ps", b)])
                copy_op(EV(), dst[:, g0:g0 + n, t_off:t_off + 128],
                        psb16[b][:, 0:n * 128].rearrange("p (k t) -> p k t", k=n), [("ps", b)], [dkey])

        def mm_group(b, ncols, pairs, reads):
            n = len(pairs)
            fns = [(lambda e, i=i: e.matmul(ps[b][:, 0:ncols], lhsT=pairs[i][0], rhs=pairs[i][1],
                                            start=(i == 0), stop=(i == n - 1))) for i in range(n)]
            s.group("pe", fns, reads=reads, writes=[("ps", b)])

        for l in range(L):
            xsrc = x_seq if l == 0 else xres_d
            A.reset()
            gb = A.alloc([D], F32)
            xts = [A.alloc([D], F32) for _ in range(2)]
            xbs = [A.alloc([D], BF16) for _ in range(2)]
            p1keys = ["gb", ("xt", 0), ("xt", 1), ("xb", 0), ("xb", 1)]
            assert A.off <= KC * S * 2
            hTo, _ = A.at(KC * S * 2, [KC, TO], BF16)
            s.dma("sp", gb, gb_pre[l], writes=["gb"])
            tpc = TCH // 128
            for tb in range(TO // 128):
                par = tb % 2
                xt, xb = xts[par], xbs[par]
                s.dma("sp", xt, xsrc[tb * 128:(tb + 1) * 128, :], writes=[("xt", par)])
                r_ap, rk = rstd(xt, xb, 32 * par, D, [("xt", par)], [("xb", par)])
                s.op("dve", lambda g: g.scalar_tensor_tensor(out=xb, in0=xt, scalar=r_ap, in1=gb,
                                                             op0=ALU.mult, op1=ALU.mult),
                     reads=[("xt", par), rk, "gb"], writes=[("xb", par)])
                transposeT(xb, [("xb", par)], KC, hTo, ("hTo", tb), tb * 128)
                if (tb + 1) % tpc == 0:
                    c = tb // tpc
                    s.dma("sp", hTo_d[c].rearrange("(p k) t -> p k t", k=KC), hTo[:, :, c * TCH:(c + 1) * TCH],
                          reads=[("hTo", t_) for t_ in range(c * tpc, (c + 1) * tpc)], writes=[("hTod", c)])
                    cc_gather(hTo_d[c], hTg_d[c], [("hTod", c)], [("hTgd", c)])
            hT, p_mark = A.at(0, [KC, S], BF16)
            hTo_keys = [("hTo", t_) for t_ in range(TO // 128)]
            for c in range(NCH):
                for r in range(NR):
                    t0 = r * TO + c * TCH
                    s.dma("sp", hT[:, :, t0:t0 + TCH],
                          hTg_d[c, r * 128 * KC:(r + 1) * 128 * KC, :].rearrange("(p k) t -> p k t", k=KC),
                          reads=[("hTgd", c)], writes=[("hT", tb_) for tb_ in range(t0 // 128, (t0 + TCH) // 128)] + p1keys)
            A.reset(p_mark)
            CT = 256
            wbufs = [A.alloc([KC, CT], BF16) for _ in range(3)]
            stg = [A.alloc([512], BF16) for _ in range(2)]
            stgf = [A.alloc([512], F32) for _ in range(2)]
            si = 0
            for ct in range(INW // CT):
                wb = wbufs[ct % 3]
                wk = ("w2", ct % 3)
                s.dma("pool", wb, w_in[l, :, ct * CT:(ct + 1) * CT].rearrange("(k p) n -> p k n", p=128),
                      writes=[wk] + (hTo_keys if ct < 3 else []))
                seg = (ct * CT) // GW
                off = ct * CT - seg * GW
                if seg in (2, 5, 8):
                    m = {2: 0, 5: 1, 8: 2}[seg]
                    for tb in range(TB):
                        b = PS()
                        mm_group(b, CT, [(hT[:, k, tb * 128:(tb + 1) * 128], wb[:, k, :]) for k in range(KC)],
                                 [("hT", tb), wk])
                        si ^= 1
                        copy_op(EV(), stg[si][:, 0:CT], ps[b][:, 0:CT], [("ps", b)], [("stg", si)])
                        s.dma("sp", v_d[m, tb * 128:(tb + 1) * 128, off:off + CT], stg[si][:, 0:CT], reads=[("stg", si)])
                else:
                    for sub in range(CT // 128):
                        h = (off + sub * 128) // 128
                        for tc in range(NQC):
                            b = PS()
                            mm_group(b, 512, [(wb[:, k, sub * 128:(sub + 1) * 128], hT[:, k, tc * 512:(tc + 1) * 512])
                                              for k in range(KC)],
                                     [("hT", tc * 4 + i) for i in range(4)] + [wk])
                            si ^= 1
                            if seg == 9:
                                copy_op(EV(), stgf[si], ps[b][:], [("ps", b)], [("stgf", si)])
                                s.dma("sp", u_d[h, :, tc * 512:(tc + 1) * 512], stgf[si], reads=[("stgf", si)])
                            else:
                                idx = {0: 0, 1: 1, 3: 2, 4: 3, 6: 4, 7: 5}[seg]
                                copy_op(EV(), stg[si], ps[b][:], [("ps", b)], [("stg", si)])
                                s.dma("sp", qkT_d[idx, h, :, tc * 512:(tc + 1) * 512], stg[si], reads=[("stg", si)])
            s.barrier()

            A.reset()
            ystage = A.alloc([TB, GW], BF16)
            msd = A.alloc([S], BF16)
            msc = A.alloc([S], BF16)
            mss = A.alloc([S], BF16)
            s.dma("pool", msd, c_msd, writes=["msd"])
            s.dma("pool", msc, c_msc, writes=["msc"])
            s.dma("pool", mss, c_mss, writes=["mss"])
            qT = [A.alloc([S], BF16) for _ in range(2)]
            kT = [A.alloc([S], BF16) for _ in range(2)]
            vx = [A.alloc([TB, 130], BF16) for _ in range(2)]
            for i in range(2):
                s.op("dve", lambda g, i=i: g.memset(vx[i][:, :, 128:129], 1.0), writes=[("vx1", i)])
            Eall = A.alloc([TB, 512], BF16)
            acc = A.alloc([4, 129], F32)
            nm = A.alloc([NBM, 8], F32)
            s.dma("sp", nm, c_nm, writes=["nm"])
            km = A.alloc([NBM], F32)
            kmh = A.alloc([NBM], BF16)
            kml = A.alloc([NBM], BF16)
            g8 = A.alloc([8], F32)
            top8 = A.alloc([8], F32)
            sel = A.alloc([TB, 8], F32)
            s.op("dve", lambda g: g.memset(g8, -1e30), writes=["g8"])
            t_e = A.alloc([512], F32)
            t_sp = A.alloc([512], F32)
            t_1 = A.alloc([512], F32)
            t_3 = A.alloc([512], F32)
            spm = A.alloc([512], BF16)
            Rb = A.alloc([512], F32)
            p3_mark = A.off
            hcount = [0]

            def load_head(m, h):
                i = hcount[0] % 2
                hcount[0] += 1
                s.dma("sp", qT[i], qkT_d[2 * m, h], writes=[("qT", i)])
                s.dma("sp", kT[i], qkT_d[2 * m + 1, h], writes=[("kT", i)])
                s.dma("sp", vx[i][:, :, 0:128],
                      v_d[m, :, h * 128:(h + 1) * 128].rearrange("(tb p) d -> p tb d", p=128), writes=[("vx", i)])
                return i

            def finalize(src_ap, den_ap, rkeys, qb, h):
                s.op("dve", lambda g: g.reciprocal(out=stt[:, 8:9], in_=den_ap), reads=rkeys, writes=[("stt", 8)])
                s.op("dve", lambda g: g.tensor_scalar(out=ystage[:, qb, h * 128:(h + 1) * 128], in0=src_ap,
                                                      scalar1=stt[:, 8:9], scalar2=None, op0=ALU.mult),
                     reads=list(rkeys) + [("stt", 8)], writes=[("ys", qb)])

            def store_mixer(mi):
                s.dma("sp", y_d[mi].rearrange("(tb p) c -> p tb c", p=128), ystage,
                      reads=[("ys", qb) for qb in range(TB)], writes=[("yd", mi)])
                cc_gather(y_d[mi], yg_d[mi], [("yd", mi)], [("ygd", mi)])

            mtog = [0]

            def mask_eng():
                mtog[0] ^= 1
                return "pool" if mtog[0] else "dve"

            for h in range(NH):
                i = load_head(0, h)
                hk = [("qT", i), ("kT", i), ("vx", i), ("vx1", i)]
                s.op("dve", lambda g: g.tensor_reduce(out=km, in_=kT[i].rearrange("p (n k) -> p n k", k=MOBA_BLOCK),
                                                      axis=AX.X, op=ALU.add), reads=[("kT", i)], writes=["km"])
                s.op("dve", lambda g: g.tensor_scalar(out=kmh, in0=km, scalar1=1.0 / MOBA_BLOCK, scalar2=None,
                                                      op0=ALU.mult), reads=["km"], writes=["kmh"])
                s.op("dve", lambda g: g.scalar_tensor_tensor(out=kml, in0=km, scalar=1.0 / MOBA_BLOCK, in1=kmh,
                                                             op0=ALU.mult, op1=ALU.subtract),
                     reads=["km", "kmh"], writes=["kml"])
                for tb in range(TB):
                    b = PS()
                    mm_group(b, NBM, [(qT[i][:, tb * 128:(tb + 1) * 128], kmh),
                                      (qT[i][:, tb * 128:(tb + 1) * 128], kml)], [("qT", i), "kmh", "kml"])
                    blk = tb // 2
                    s.op("dve", lambda g: g.tensor_tensor(out=g8[:, 0:NBM], in0=ps[b][:, 0:NBM], in1=nm[:, blk, 0:NBM],
                                                          op=ALU.add), reads=[("ps", b), "nm"], writes=["g8"])
                    s.op("dve", lambda g: g.max(out=top8, in_=g8), reads=["g8"], writes=["top8"])
                    s.op("dve", lambda g: g.tensor_scalar(out=sel[:, tb, :], in0=g8, scalar1=top8[:, 2:3], scalar2=None,
                                                          op0=ALU.is_ge), reads=["g8", "top8"], writes=[("sel", tb)])
                for qc in range(NQC):
                    for n in range(2 * qc + 2):
                        q0 = max(qc * 512, n * 256)
                        W = (qc + 1) * 512 - q0
                        c0 = q0 - qc * 512
                        own = (n >= 2 * qc)
                        for j in range(2):
                            kb = 2 * n + j
                            b = PS()
                            mm_group(b, W, [(kT[i][:, kb * 128:(kb + 1) * 128], qT[i][:, q0:q0 + W])], hk)
                            s.op("act", lambda g: g.activation(out=Eall[:, j, c0:512], in_=ps[b][:, 0:W], func=AF.Exp,
                                                               scale=scale), reads=[("ps", b)], writes=[("E", j)])
                            if own and j == 0:
                                s.op(mask_eng(), lambda g: g.tensor_tensor(out=Eall[:, 0, c0:c0 + 256], in0=Eall[:, 0, c0:c0 + 256],
                                                                           in1=msc[:, 0:256], op=ALU.mult),
                                     reads=[("E", 0), "msc"], writes=[("E", 0)])
                            elif own:
                                s.op("pool", lambda g: g.memset(Eall[:, 1, c0:c0 + 128], 0.0), reads=[("E", 1)], writes=[("E", 1)])
                                s.op(mask_eng(), lambda g: g.tensor_tensor(out=Eall[:, 1, c0 + 128:c0 + 256],
                                                                           in0=Eall[:, 1, c0 + 128:c0 + 256],
                                                                           in1=msc[:, 0:128], op=ALU.mult),
                                     reads=[("E", 1), "msc"], writes=[("E", 1)])
                        for sub in range(c0 // 128, 4):
                            qb = qc * 4 + sub
                            b = PS()
                            mm_group(b, 129, [(Eall[:, 0, sub * 128:(sub + 1) * 128], vx[i][:, 2 * n, 0:129]),
                                              (Eall[:, 1, sub * 128:(sub + 1) * 128], vx[i][:, 2 * n + 1, 0:129])],
                                     [("E", 0), ("E", 1)] + hk)
                            ak = ("acc", sub)
                            if n == 0:
                                if qb // 2 == 0:
                                    s.op("dve", lambda g: g.tensor_copy(out=acc[:, sub, :], in_=ps[b][:, 0:129]),
                                         reads=[("ps", b)], writes=[ak])
                                else:
                                    s.op("dve", lambda g: g.tensor_scalar(out=acc[:, sub, :], in0=ps[b][:, 0:129],
                                                                          scalar1=sel[:, qb, 0:1], scalar2=None, op0=ALU.mult),
                                         reads=[("ps", b), ("sel", qb)], writes=[ak])
                            elif n == qb // 2:
                                s.op("dve", lambda g: g.tensor_tensor(out=acc[:, sub, :], in0=ps[b][:, 0:129],
                                                                      in1=acc[:, sub, :], op=ALU.add),
                                     reads=[("ps", b), ak], writes=[ak])
                            else:
                                s.op("dve", lambda g: g.scalar_tensor_tensor(out=acc[:, sub, :], in0=ps[b][:, 0:129],
                                                                             scalar=sel[:, qb, n:n + 1], in1=acc[:, sub, :],
                                                                             op0=ALU.mult, op1=ALU.add),
                                     reads=[("ps", b), ak, ("sel", qb)], writes=[ak])
                    for sub in range(4):
                        finalize(acc[:, sub, 0:128], acc[:, sub, 128:129], [("acc", sub)], qc * 4 + sub, h)
            store_mixer(0)

            A.reset(p3_mark)
            spf = A.alloc([TB, 512], F32)
            spmA = A.alloc([TB, 512], BF16)
            t_e2 = [t_e, A.alloc([512], F32)]
            t_12 = [t_1, A.alloc([512], F32)]
            t_32 = [t_3, A.alloc([512], F32)]
            bi = 0
            for h in range(NH):
                i = load_head(1, h)
                hk = [("qT", i), ("kT", i), ("vx", i)]
                for qc in range(NQC):
                    nkb = (qc + 1) * 4
                    s.op("pool", lambda g: g.memset(spmA[:, 4 * qc:4 * qc + 4, :], 0.0),
                         writes=[("spm", kb) for kb in range(4 * qc, 4 * qc + 4)])
                    geo = {}
                    for kb in range(nkb):
                        q0 = max(qc * 512, kb * 128)
                        geo[kb] = (q0, (qc + 1) * 512 - q0, q0 - qc * 512, kb * 128 >= qc * 512)
                    for kb in range(nkb):
                        q0, W, c0, diag = geo[kb]
                        bi ^= 1
                        te, tek = t_e2[bi], ("t_e", bi)
                        bz = PS()
                        mm_group(bz, W, [(kT[i][:, kb * 128:(kb + 1) * 128], qT[i][:, q0:q0 + W])], hk)
                        s.op("act", lambda g: g.activation(out=te[:, 0:W], in_=ps[bz][:, 0:W], func=AF.Exp, scale=scale),
                             reads=[("ps", bz)], writes=[tek])
                        s.op("act", lambda g: g.activation(out=spf[:, kb, c0:512], in_=te[:, 0:W], func=AF.Ln, bias=1.0),
                             reads=[tek], writes=[("spf", kb)])
                        if diag:
                            s.op("dve", lambda g: g.tensor_tensor(out=spmA[:, kb, c0:c0 + 128], in0=spf[:, kb, c0:c0 + 128],
                                                                  in1=mss[:, 0:128], op=ALU.mult),
                                 reads=[("spf", kb), "mss"], writes=[("spm", kb)])
                            if W > 128:
                                s.op("pool", lambda g: g.tensor_copy(out=spmA[:, kb, c0 + 128:512], in_=spf[:, kb, c0 + 128:512]),
                                     reads=[("spf", kb)], writes=[("spm2", kb)])
                        else:
                            s.op("pool", lambda g: g.tensor_copy(out=spmA[:, kb, :], in_=spf[:, kb, :]),
                                 reads=[("spf", kb)], writes=[("spm", kb), ("spm2", kb)])
                    for kb in range(nkb):
                        q0, W, c0, diag = geo[kb]
                        bi ^= 1
                        t1, t1k = t_12[bi], ("t_1", bi)
                        t3, t3k = t_32[bi], ("t_3", bi)
                        bc = PS()
                        mm_group(bc, W, [(trib[:], spmA[:, kb, c0:512])] +
                                 [(onesb[:], spmA[:, k2, c0:512]) for k2 in range(kb + 1, nkb)],
                                 ["trib", "onesb"] + [("spm", k2) for k2 in range(kb, nkb)] + [("spm2", k2) for k2 in range(kb, nkb)])
                        bz = PS()
                        mm_group(bz, W, [(kT[i][:, kb * 128:(kb + 1) * 128], qT[i][:, q0:q0 + W])], hk)
                        s.op("dve", lambda g: g.tensor_tensor(out=t1[:, 0:W], in0=ps[bc][:, 0:W], in1=spf[:, kb, c0:512],
                                                              op=ALU.add), reads=[("ps", bc), ("spf", kb)], writes=[t1k])
                        s.op("dve", lambda g: g.scalar_tensor_tensor(out=t3[:, 0:W], in0=ps[bz][:, 0:W], scalar=scale,
                                                                     in1=t1[:, 0:W], op0=ALU.mult, op1=ALU.subtract),
                             reads=[("ps", bz), t1k], writes=[t3k])
                        s.op("act", lambda g: g.activation(out=Eall[:, kb, c0:512], in_=t3[:, 0:W], func=AF.Exp),
                             reads=[t3k], writes=[("E", kb)])
                        if diag:
                            s.op("pool", lambda g: g.tensor_tensor(out=Eall[:, kb, c0:c0 + 128], in0=Eall[:, kb, c0:c0 + 128],
                                                                   in1=mss[:, 0:128], op=ALU.mult),
                                 reads=[("E", kb), "mss"], writes=[("E", kb)])
                    for sub in range(4):
                        qb = qc * 4 + sub
                        b = PS()
                        mm_group(b, 128, [(Eall[:, kb, sub * 128:(sub + 1) * 128], vx[i][:, kb, 0:128]) for kb in range(qb + 1)],
                                 [("E", kb) for kb in range(qb + 1)] + hk)
                        copy_op(EV(), ystage[:, qb, h * 128:(h + 1) * 128], ps[b][:, 0:128], [("ps", b)], [("ys", qb)])
            store_mixer(1)

            for h in range(NH):
                i = load_head(2, h)
                hk = [("qT", i), ("kT", i), ("vx", i), ("vx1", i)]
                for qc in range(NQC):
                    nkb = (qc + 1) * 4
                    for kb in range(nkb):
                        q0 = max(qc * 512, kb * 128)
                        W = (qc + 1) * 512 - q0
                        c0 = q0 - qc * 512
                        b = PS()
                        mm_group(b, W, [(kT[i][:, kb * 128:(kb + 1) * 128], qT[i][:, q0:q0 + W])], hk)
                        s.op("act", lambda g: g.activation(out=Eall[:, kb, c0:512], in_=ps[b][:, 0:W], func=AF.Exp, scale=scale),
                             reads=[("ps", b)], writes=[("E", kb)])
                        mo = q0 - 128 * kb
                        s.op(mask_eng(), lambda g: g.tensor_tensor(out=Eall[:, kb, c0:512], in0=Eall[:, kb, c0:512],
                                                                   in1=msd[:, mo:mo + W], op=ALU.mult),
                             reads=[("E", kb), "msd"], writes=[("E", kb)])
                    for sub in range(4):
                        qb = qc * 4 + sub
                        b = PS()
                        mm_group(b, 129, [(Eall[:, kb, sub * 128:(sub + 1) * 128], vx[i][:, kb, 0:129]) for kb in range(qb + 1)],
                                 [("E", kb) for kb in range(qb + 1)] + hk)
                        finalize(ps[b][:, 0:128], ps[b][:, 128:129], [("ps", b)], qb, h)
            store_mixer(3)

            s.barrier()
            A.reset(p3_mark)
            up = A.alloc([16 + S], F32)
            pa = A.alloc([16 + S], F32)
            pb = A.alloc([16 + S], F32)
            coef = [A.alloc([S], F32) for _ in range(4)]
            tmpf = A.alloc([S], F32)
            tmp2 = A.alloc([S], F32)
            dT = A.alloc([PC, S], BF16)
            wp = A.alloc([PC, PGD], BF16)
            pst = A.alloc([GW], F32)
            s.dma("sp", pst, psb[l], writes=["pst"])
            for t_, k_ in ((up, "up"), (pa, "pa"), (pb, "pb")):
                s.op("dve", lambda g, t_=t_: g.memset(t_[:, 0:16], 0.0), writes=[k_ + "z"])
            for gi in range(PGn):
                s.dma("pool", wp, w_pool[l, gi].rearrange("(c p) d -> p c d", p=128), writes=["wp"])
                for wi in range(4):
                    s.dma("sp", coef[wi], c_pcoef[gi, wi], writes=[("coef", wi)])
                for c in range(PC):
                    ch = gi * PC + c
                    s.dma("sp", up[:, 16:16 + S], u_d[ch], writes=["up"])
                    cur, ck = up, "up"
                    for wi in range(4):
                        sh = 1 << wi
                        nxt, nk = (pa, "pa") if cur is not pa else (pb, "pb")
                        s.op("dve", lambda g, cur=cur, nxt=nxt, sh=sh: g.tensor_tensor(
                            out=nxt[:, 16:16 + S], in0=cur[:, 16:16 + S], in1=cur[:, 16 - sh:16 - sh + S], op=ALU.add),
                             reads=[ck, ck + "z"], writes=[nk])
                        cur, ck = nxt, nk
                        if wi == 0:
                            s.op("pool", lambda g, cur=cur: g.tensor_tensor(out=tmpf, in0=cur[:, 16:16 + S], in1=coef[0],
                                                                            op=ALU.mult), reads=[ck, ("coef", 0)], writes=["tmpf"])
                        else:
                            s.op("pool", lambda g, cur=cur: g.tensor_tensor(out=tmp2, in0=cur[:, 16:16 + S], in1=coef[wi],
                                                                            op=ALU.mult), reads=[ck, ("coef", wi)], writes=["tmp2"])
                            s.op("pool", lambda g: g.tensor_tensor(out=tmpf, in0=tmpf, in1=tmp2, op=ALU.add),
                                 reads=["tmpf", "tmp2"], writes=["tmpf"])
                    s.op("dve", lambda g: g.tensor_tensor(out=dT[:, c, :], in0=tmpf, in1=up[:, 16:16 + S],
                                                          op=ALU.subtract), reads=["tmpf", "up"], writes=[("dT", c)])
                for tb in range(TB):
                    b = PS()
                    mm_group(b, PGD, [(dT[:, c, tb * 128:(tb + 1) * 128], wp[:, c, :]) for c in range(PC)],
                             [("dT", c) for c in range(PC)] + ["wp"])
                    s.op("dve", lambda g: g.tensor_tensor(out=ystage[:, tb, gi * PGD:(gi + 1) * PGD], in0=ps[b][:, 0:PGD],
                                                          in1=pst[:, gi * PGD:(gi + 1) * PGD], op=ALU.mult),
                         reads=[("ps", b), "pst"], writes=[("ys", tb)])
            store_mixer(2)
            s.barrier()

            dst = out if l == L - 1 else xres_d
            LOW = 0
            MID1 = 32 * 1024
            MID2 = 64 * 1024
            MSB_OFF = ARENA_BYTES - 4 * D * 4
            ACT_OFF = ARENA_BYTES - ((FC * TG * 2 + 31) // 32) * 32
            assert KC2 * TG * 2 <= MID1 and KC * TG * 2 <= MID1 and 4 * D * 4 <= MID2
            for tg in range(NTG):
                yT, _ = A.at(LOW, [KC2, TG], BF16)
                A.reset(MID1)
                gbm = A.alloc([MW], F32)
                yts = [A.alloc([MW], BF16) for _ in range(2)]
                yt1s = [A.alloc([MW], BF16) for _ in range(2)]
                ybs = [A.alloc([MW], BF16) for _ in range(2)]
                assert A.off <= MSB_OFF
                s.dma("sp", gbm, gb_mo[l], writes=["gbm"])
                ygk = [("ygd", m) for m in range(4)]
                for tb in range(4):
                    T0 = tg * TG + tb * 128
                    par = tb % 2
                    so = 32 * par
                    yt, yt1, yb = yts[par], yt1s[par], ybs[par]
                    ytk = [("ytp", par, 0, r) for r in range(NR)]
                    for r in range(NR):
                        s.dma("sp", yt[:, r * MWL:(r + 1) * MWL].rearrange("p (m c) -> p m c", m=4),
                              yg_d[:, r * S + T0:r * S + T0 + 128, :].rearrange("m p c -> p m c"),
                              reads=ygk, writes=[("ytp", par, 0, r)])
                        if NR > 1:
                            s.dma("sp", yt1[:, r * MWL:(r + 1) * MWL].rearrange("p (m c) -> p m c", m=4),
                                  yg_d[:, r * S + TO + T0:r * S + TO + T0 + 128, :].rearrange("m p c -> p m c"),
                                  reads=ygk, writes=[("ytp", par, 1, r)])
                    if NR > 1:
                        s.op("pool", lambda g: g.tensor_scalar(out=yt, in0=yt, scalar1=abt[:, 0:1], scalar2=None, op0=ALU.mult),
                             reads=ytk + ["abt"], writes=ytk)
                        s.op("dve", lambda g: g.scalar_tensor_tensor(out=yt, in0=yt1, scalar=abt[:, 1:2], in1=yt,
                                                                     op0=ALU.mult, op1=ALU.add),
                             reads=[("ytp", par, 1, r) for r in range(NR)] + ["abt"] + ytk, writes=ytk)
                    for gi, ranges in enumerate(groups):
                        for ri, (c0, cl) in enumerate(ranges):
                            col = so + 12 + ri
                            s.op("act", lambda g: g.activation(out=yb[:, c0:c0 + cl], in_=yt[:, c0:c0 + cl],
                                                               func=AF.Square, accum_out=stt[:, col:col + 1]),
                                 reads=ytk, writes=[("yb", par, gi), ("stt", col)])
                        if len(ranges) == 2:
                            s.op("dve", lambda g: g.tensor_tensor(out=stt[:, so + 12:so + 13], in0=stt[:, so + 12:so + 13],
                                                                  in1=stt[:, so + 13:so + 14], op=ALU.add),
                                 reads=[("stt", so + 12), ("stt", so + 13)], writes=[("stt", so + 12)])
                        s.op("act", lambda g: g.activation(out=stt[:, so + 14:so + 15], in_=stt[:, so + 12:so + 13], func=AF.Sqrt,
                                                           scale=1.0 / NGW, bias=epsb[:]),
                             reads=[("stt", so + 12), "epsb"], writes=[("stt", so + 14)])
                        s.op("dve", lambda g: g.reciprocal(out=stt[:, so + 16 + gi:so + 17 + gi], in_=stt[:, so + 14:so + 15]),
                             reads=[("stt", so + 14)], writes=[("stt", so + 16 + gi)])
                        for (c0, cl) in ranges:
                            s.op("dve", lambda g: g.scalar_tensor_tensor(out=yb[:, c0:c0 + cl], in0=yt[:, c0:c0 + cl],
                                                                         scalar=stt[:, so + 16 + gi:so + 17 + gi],
                                                                         in1=gbm[:, c0:c0 + cl], op0=ALU.mult, op1=ALU.mult),
                                 reads=ytk + [("stt", so + 16 + gi), "gbm"], writes=[("yb", par, gi)])
                    transposeT(yb, [("yb", par, gi) for gi in range(len(groups))], KC2, yT, ("yT", tb), tb * 128)
                s.barrier()
                msb, _ = A.at(MSB_OFF, [4, D], F32)
                A.reset(MID1)
                wobuf = [A.alloc([KC2, 512], BF16) for _ in range(2)]
                assert A.off <= MSB_OFF
                for nt in range(D // 512):
                    wb = wobuf[nt % 2]
                    wk = ("wo", nt % 2)
                    s.dma("pool", wb, w_out[l, :, nt * 512:(nt + 1) * 512].rearrange("(k p) n -> p k n", p=128), writes=[wk])
                    for tb in range(4):
                        b = PS()
                        mm_group(b, 512, [(yT[:, k, tb * 128:(tb + 1) * 128], wb[:, k, :]) for k in range(KC2)],
                                 [("yT", tb), wk])
                        copy_op(EV(), msb[:, tb, nt * 512:(nt + 1) * 512], ps[b][:], [("ps", b)], [("msb", tb)])
                s.barrier()
                h2T, _ = A.at(LOW, [KC, TG], BF16)
                A.reset(MID1)
                gb1 = A.alloc([D], F32)
                gb2 = A.alloc([D], F32)
                xts = [A.alloc([D], F32) for _ in range(2)]
                xbs = [A.alloc([D], BF16) for _ in range(2)]
                assert A.off <= MSB_OFF
                s.dma("sp", gb1, gb_post[l], writes=["gb1"])
                s.dma("sp", gb2, gb_fpre[l], writes=["gb2"])
                for tb in range(4):
                    T0 = tg * TG + tb * 128
                    par = tb % 2
                    xt, xb = xts[par], xbs[par]
                    xk, bk = ("xt", par), ("xb", par)
                    s.dma("sp", xt, xsrc[T0:T0 + 128, :], writes=[xk])
                    r_ap, rk = rstd(msb[:, tb, :], xb, 32 * par, D, [("msb", tb)], [bk])
                    s.op("dve", lambda g: g.scalar_tensor_tensor(out=msb[:, tb, :], in0=msb[:, tb, :], scalar=r_ap,
                                                                 in1=gb1, op0=ALU.mult, op1=ALU.mult),
                         reads=[("msb", tb), rk, "gb1"], writes=[("msb", tb)])
                    s.op("pool", lambda g: g.tensor_tensor(out=xt, in0=xt, in1=msb[:, tb, :], op=ALU.add),
                         reads=[xk, ("msb", tb)], writes=[xk])
                    s.dma("sp", xres_d[T0:T0 + 128, :], xt, reads=[xk])
                    r_ap, rk = rstd(xt, xb, 32 * par + 4, D, [xk], [bk])
                    s.op("dve", lambda g: g.scalar_tensor_tensor(out=xb, in0=xt, scalar=r_ap, in1=gb2,
                                                                 op0=ALU.mult, op1=ALU.mult),
                         reads=[xk, rk, "gb2"], writes=[bk])
                    transposeT(xb, [bk], KC, h2T, ("h2T", tb), tb * 128)
                s.barrier()
                actT, _ = A.at(ACT_OFF, [FC, TG], BF16)
                A.reset(MID1)
                CT2 = 256
                wgu = [A.alloc([KC, CT2], BF16) for _ in range(3)]
                sg = A.alloc([2, TG], F32)
                assert A.off <= ACT_OFF
                wi = 0
                h2keys = [("h2T", tb) for tb in range(4)]
                for j in range(DFF // CT2):
                    wg, wgk = wgu[wi % 3], ("wgu", wi % 3)
                    wi += 1
                    s.dma("pool", wg, w_gate[l, :, j * CT2:(j + 1) * CT2].rearrange("(k p) n -> p k n", p=128), writes=[wgk])
                    wu, wuk = wgu[wi % 3], ("wgu", wi % 3)
                    wi += 1
                    s.dma("pool", wu, w_up[l, :, j * CT2:(j + 1) * CT2].rearrange("(k p) n -> p k n", p=128), writes=[wuk])
                    for c in range(2):
                        b = PS()
                        mm_group(b, TG, [(wg[:, k, c * 128:(c + 1) * 128], h2T[:, k, :]) for k in range(KC)], h2keys + [wgk])
                        s.op("act", lambda g: g.activation(out=sg[:, c, :], in_=ps[b][:], func=AF.Silu),
                             reads=[("ps", b)], writes=[("sg", c)])
                    for c in range(2):
                        b = PS()
                        mm_group(b, TG, [(wu[:, k, c * 128:(c + 1) * 128], h2T[:, k, :]) for k in range(KC)], h2keys + [wuk])
                        s.op("dve", lambda g: g.tensor_tensor(out=actT[:, 2 * j + c, :], in0=ps[b][:], in1=sg[:, c, :],
                                                              op=ALU.mult),
                             reads=[("ps", b), ("sg", c)], writes=[("actT", 2 * j + c)])
                s.barrier()
                fsb, _ = A.at(LOW, [4, D], F32)
                A.reset(MID2)
                FG = 8
                wdb = [A.alloc([FG, 512], BF16) for _ in range(3)]
                assert A.off <= ACT_OFF
                nfg = (FC + FG - 1) // FG
                wdi = 0
                for nt in range(D // 512):
                    banks = [PS() for _ in range(4)]
                    for fg in range(nfg):
                        f0 = fg * FG
                        nf = min(FG, FC - f0)
                        wd, wdk = wdb[wdi % 3], ("wd", wdi % 3)
                        wdi += 1
                        s.dma("pool", wd[:, 0:nf, :],
                              w_down[l, f0 * 128:(f0 + nf) * 128, nt * 512:(nt + 1) * 512].rearrange("(c p) n -> p c n", p=128),
                              writes=[wdk])
                        for tb in range(4):
                            fns = [(lambda e, fc=fc, tb=tb, wd=wd: e.matmul(ps[banks[tb]][:], lhsT=actT[:, fc, tb * 128:(tb + 1) * 128],
                                                                            rhs=wd[:, fc - f0, :], start=(fc == 0), stop=(fc == FC - 1)))
                                   for fc in range(f0, f0 + nf)]
                            s.group("pe", fns, reads=[("actT", fc) for fc in range(f0, f0 + nf)] + [wdk],
                                    writes=[("ps", banks[tb])])
                    for tb in range(4):
                        copy_op(EV(), fsb[:, tb, nt * 512:(nt + 1) * 512], ps[banks[tb]][:], [("ps", banks[tb])], [("fsb", tb)])
                s.barrier()
                A.reset(MID2)
                gb1 = A.alloc([D], F32)
                xts = [A.alloc([D], F32) for _ in range(2)]
                xbs = [A.alloc([D], BF16) for _ in range(2)]
                s.dma("sp", gb1, gb_fpost[l], writes=["gb1"])
                for tb in range(4):
                    T0 = tg * TG + tb * 128
                    par = tb % 2
                    xt, xb = xts[par], xbs[par]
                    xk, bk = ("xt", par), ("xb", par)
                    s.dma("sp", xt, xres_d[T0:T0 + 128, :], writes=[xk])
                    r_ap, rk = rstd(fsb[:, tb, :], xb, 32 * par, D, [("fsb", tb)], [bk])
                    s.op("dve", lambda g: g.scalar_tensor_tensor(out=fsb[:, tb, :], in0=fsb[:, tb, :], scalar=r_ap,
                                                                 in1=gb1, op0=ALU.mult, op1=ALU.mult),
                         reads=[("fsb", tb), rk, "gb1"], writes=[("fsb", tb)])
                    s.op("pool", lambda g: g.tensor_tensor(out=xt, in0=xt, in1=fsb[:, tb, :], op=ALU.add),
                         reads=[xk, ("fsb", tb)], writes=[xk])
                    s.dma("sp", dst[T0:T0 + 128, :], xt, reads=[xk])
                s.barrier()
        s.finish()
    print("instructions:", s.n_inst, "sems:", s.nsem)
    return nc


def make_consts(S):
    kin = np.arange(128)[:, None]
    j = np.arange(S)[None, :]
    delta = j - kin
    msd = np.zeros((128, S), np.float32)
    for window, dil in DILATED_PATTERNS:
        msd += ((delta >= 0) & (delta <= window) & (delta % dil == 0)).astype(np.float32)
    msc = (delta >= 0).astype(np.float32)
    mss = (delta > 0).astype(np.float32)
    nbm = S // MOBA_BLOCK
    nm = np.zeros((128, nbm, 8), np.float32)
    for blk in range(nbm):
        for n in range(8):
            if n >= blk:
                nm[:, blk, n] = -1e30
    jj = np.arange(128)[:, None]
    ss = np.arange(128)[None, :]
    tri = (jj > ss).astype(np.float32)
    return {"c_ident": np.eye(128, dtype=np.float32), "c_tri": tri, "c_msd": msd, "c_msc": msc, "c_mss": mss, "c_nm": nm}


def pool_coef(S, windows):
    t = np.arange(S)
    out = np.zeros((len(windows), 4, 128, S), np.float32)
    for g, w in enumerate(windows):
        wi = {2: 0, 4: 1, 8: 2, 16: 3}[w]
        out[g, wi] = np.broadcast_to(1.0 / np.minimum(w, t + 1).astype(np.float32), (128, S))
    return out


def bcast128(v):
    return np.ascontiguousarray(np.broadcast_to(v[:, None, :], (v.shape[0], 128, v.shape[1]))).astype(np.float32)


_NC_CACHE = {}


def kernel(x, ln_mix_pre, w_in, w_pool, pool_scale, mix_out_norm, w_out, ln_mix_post, ln_ffn_pre,
           w_gate, w_up, w_down, ln_ffn_post):
    x = np.asarray(x, np.float32)
    B, S, D = x.shape
    L = w_in.shape[0]
    DFF = w_gate.shape[2]
    GWF = D // 4
    NR = 2
    GW = GWF // NR
    NH = GW // 128
    TO = S // NR
    PGD = GWF // 4
    PGn = 4 // NR
    n_cores = B * NR
    perm = np.concatenate([np.arange(m * GWF + r * GW, m * GWF + (r + 1) * GW) for r in range(NR) for m in range(4)])
    groups = [[(r * 4 * GW + m * GW, GW) for r in range(NR)] for m in range(4)]
    cfg = dict(S=S, D=D, NH=NH, DFF=DFF, TO=TO, L=L, PGn=PGn, PGD=PGD, NGW=GWF, windows=None, groups=groups,
               NR=NR, RG=[[2 * i, 2 * i + 1] for i in range(B)])
    key = (S, D, NH, DFF, L, NR, B)
    if key not in _NC_CACHE:
        _NC_CACHE[key] = build(cfg)
    nc = _NC_CACHE[key]
    w_in = np.asarray(w_in, np.float32)
    common = {
        "w_out": np.ascontiguousarray(np.asarray(w_out, np.float32)[:, perm, :]),
        "w_gate": np.ascontiguousarray(w_gate, np.float32), "w_up": np.ascontiguousarray(w_up, np.float32),
        "w_down": np.ascontiguousarray(w_down, np.float32),
        "gb_pre": bcast128(np.asarray(ln_mix_pre)), "gb_post": bcast128(np.asarray(ln_mix_post)),
        "gb_fpre": bcast128(np.asarray(ln_ffn_pre)), "gb_fpost": bcast128(np.asarray(ln_ffn_post)),
        "gb_mo": bcast128(np.asarray(mix_out_norm)[:, perm]),
    }
    common.update(make_consts(S))
    per_rank = []
    for r in range(NR):
        cols = np.concatenate([np.arange(sg * GWF + r * GW, sg * GWF + (r + 1) * GW) for sg in range(10)])
        ab = np.zeros((128, 2), np.float32)
        ab[:, r] = 1.0
        per_rank.append({
            "w_in": np.ascontiguousarray(w_in[:, :, cols]),
            "w_pool": np.ascontiguousarray(np.asarray(w_pool, np.float32)[:, r * PGn:(r + 1) * PGn]),
            "psb": bcast128(np.asarray(pool_scale)[:, r * GW:(r + 1) * GW]),
            "c_pcoef": pool_coef(S, POOL_WINDOWS[r * PGn:(r + 1) * PGn]),
            "c_ab": ab,
        })
    in_maps = []
    for c in range(n_cores):
        b, r = divmod(c, NR)
        m = dict(common)
        m.update(per_rank[r])
        m["x_seq"] = np.ascontiguousarray(x[b, r * TO:(r + 1) * TO])
        in_maps.append(m)
    res = run_bass_kernel_spmd(nc, in_maps, core_ids=list(range(n_cores)))
    out = np.empty((B, S, D), np.float32)
    for c in range(n_cores):
        b, r = divmod(c, NR)
        out[b, r * TO:(r + 1) * TO] = res.results[c]["out"]
    return out
```

```python
import contextlib
import numpy as np
import concourse.bass as bass
import concourse.mybir as mybir
from concourse.bass_utils import run_bass_kernel_spmd

F32 = mybir.dt.float32
BF16 = mybir.dt.bfloat16
ALU = mybir.AluOpType
AF = mybir.ActivationFunctionType
AX = mybir.AxisListType

SEM_CAP = 30000
RMS_EPS = 1e-6
MOBA_BLOCK = 256
POOL_WINDOWS = (2, 4, 8, 16)
DILATED_PATTERNS = ((128, 1), (512, 4), (2048, 16))


class Sched:
    ENG = ("pe", "act", "dve", "pool", "sp")

    def __init__(self, nc, stack, n_lanes=8):
        self.nc = nc
        self.stack = stack
        self.engs = {"pe": nc.tensor, "act": nc.scalar, "dve": nc.vector, "pool": nc.gpsimd, "sp": nc.sync}
        self.prog = {}
        self.seen = {e: {} for e in self.ENG}
        self.nsem = 0
        self.pe_sems = set()
        for e in self.ENG:
            self.prog[e] = [self._newsem("p_" + e), 0]
        self.pe_sems.add(self.prog["pe"][0].num)
        self.lanes = {}
        for e in ("sp", "pool"):
            self.lanes[e] = [[self._newsem("l_%s%d" % (e, i)), 0] for i in range(n_lanes)]
        self.lane_rr = {e: 0 for e in self.lanes}
        self.all_sems = {}
        self.last_w = {}
        self.readers = {}
        self.n_inst = {e: 0 for e in self.ENG}

    def _newsem(self, name):
        self.nsem += 1
        return self.stack.enter_context(self.nc.semaphore("%s_%d" % (name, self.nsem)))

    def _emit(self, e, fn):
        self.n_inst[e] += 1
        return fn(self.engs[e])

    def _wait(self, e, tok):
        if tok is None:
            return
        sem, val = tok
        if e == "pe" and sem.num in self.pe_sems:
            return
        if self.seen[e].get(sem.num, 0) >= val:
            return
        self.seen[e][sem.num] = val
        self._emit(e, lambda eng: eng.wait_ge(sem, val))

    def _deps(self, e, reads, writes):
        for k in reads:
            self._wait(e, self.last_w.get(k))
        for k in writes:
            self._wait(e, self.last_w.get(k))
            for t in self.readers.get(k, ()):
                self._wait(e, t)

    def _commit(self, tok, reads, writes):
        self.all_sems[tok[0].num] = tok
        for k in writes:
            self.last_w[k] = tok
            self.readers[k] = []
        for k in reads:
            self.readers.setdefault(k, []).append(tok)

    def _next_tok(self, e):
        p = self.prog[e]
        if p[1] >= SEM_CAP:
            p[0] = self._newsem("p_" + e)
            p[1] = 0
            if e == "pe":
                self.pe_sems.add(p[0].num)
        p[1] += 1
        return (p[0], p[1])

    def op(self, e, fn, reads=(), writes=()):
        self._deps(e, reads, writes)
        tok = self._next_tok(e)
        self._emit(e, lambda eng: fn(eng).then_inc(tok[0], 1))
        self._commit(tok, reads, writes)
        return tok

    def group(self, e, fns, reads=(), writes=()):
        self._deps(e, reads, writes)
        tok = self._next_tok(e)
        for fn in fns[:-1]:
            self._emit(e, fn)
        self._emit(e, lambda eng: fns[-1](eng).then_inc(tok[0], 1))
        self._commit(tok, reads, writes)
        return tok

    def dma(self, e, out, in_, reads=(), writes=()):
        lanes = self.lanes[e]
        i = self.lane_rr[e]
        self.lane_rr[e] = (i + 1) % len(lanes)
        ln = lanes[i]
        if ln[1] >= SEM_CAP // 16:
            ln[0] = self._newsem("l_" + e)
            ln[1] = 0
        if ln[1] > 0:
            self._wait(e, (ln[0], 16 * ln[1]))
        self._deps(e, reads, writes)
        ln[1] += 1
        tok = (ln[0], 16 * ln[1])
        self._emit(e, lambda eng: eng.dma_start(out=out, in_=in_).then_inc(tok[0], 16))
        self._commit(tok, reads, writes)
        return tok

    def barrier(self, engines=None):
        toks = list(self.all_sems.values())
        for e in (engines or self.ENG):
            for tok in toks:
                self._wait(e, tok)

    def finish(self):
        self.barrier(engines=("sp",))


ARENA_BYTES = 200 * 1024
CC_MAX_BYTES = 2 * 1024 * 1024


def build(cfg):
    S, D, NH, DFF, TO, L = cfg["S"], cfg["D"], cfg["NH"], cfg["DFF"], cfg["TO"], cfg["L"]
    PGn, PGD, NGW = cfg["PGn"], cfg["PGD"], cfg["NGW"]
    windows = cfg["windows"]
    groups = cfg["groups"]
    debug = cfg.get("debug", False)
    NR = cfg.get("NR", 1)
    RG = cfg.get("RG", None)
    KC = D // 128
    GW = NH * 128
    INW = 10 * GW
    MWL = 4 * GW
    MW = NR * MWL
    KC2 = MW // 128
    TB = S // 128
    FC = DFF // 128
    PC = PGD // 128
    NBM = S // MOBA_BLOCK
    NQC = S // 512
    scale = 128 ** -0.5
    TG = 512
    NTG = TO // TG

    nc = bass.Bass("TRN2", target_bir_lowering=False)

    def din(name, shape, dt=F32):
        return nc.dram_tensor(name, list(shape), dt, kind="ExternalInput").ap()

    skind = "ExternalOutput" if debug else "Internal"

    def dscr(name, shape, dt):
        return nc.dram_tensor(name, list(shape), dt, kind=skind).ap()

    x_seq = din("x_seq", [TO, D])
    c_ab = din("c_ab", [128, 2])
    w_in = din("w_in", [L, D, INW])
    w_out = din("w_out", [L, MW, D])
    w_gate = din("w_gate", [L, D, DFF])
    w_up = din("w_up", [L, D, DFF])
    w_down = din("w_down", [L, DFF, D])
    w_pool = din("w_pool", [L, PGn, PGD, PGD])
    gb_pre = din("gb_pre", [L, 128, D])
    gb_post = din("gb_post", [L, 128, D])
    gb_fpre = din("gb_fpre", [L, 128, D])
    gb_fpost = din("gb_fpost", [L, 128, D])
    gb_mo = din("gb_mo", [L, 128, MW])
    psb = din("psb", [L, 128, GW])
    c_ident = din("c_ident", [128, 128])
    c_tri = din("c_tri", [128, 128])
    c_msd = din("c_msd", [128, S])
    c_msc = din("c_msc", [128, S])
    c_mss = din("c_mss", [128, S])
    c_pcoef = din("c_pcoef", [PGn, 4, 128, S])
    c_nm = din("c_nm", [128, NBM, 8])
    out = nc.dram_tensor("out", [TO, D], F32, kind="ExternalOutput").ap()

    qkT_d = dscr("qkT_d", [6, NH, 128, S], BF16)
    v_d = dscr("v_d", [3, S, GW], BF16)
    u_d = dscr("u_d", [GW // 128, 128, S], F32)
    assert S * GW * 2 <= CC_MAX_BYTES
    y_d = dscr("y_d", [4, S, GW], BF16)
    yg_d = dscr("yg_d", [4, NR * S, GW], BF16)
    TCH = min(TO, max(128, (CC_MAX_BYTES // (128 * KC * 2)) // 128 * 128))
    NCH = TO // TCH
    hTo_d = dscr("hTo_d", [NCH, 128 * KC, TCH], BF16)
    hTg_d = dscr("hTg_d", [NCH, NR * 128 * KC, TCH], BF16)
    xres_d = dscr("xres_d", [TO, D], F32)

    with contextlib.ExitStack() as st:
        s = Sched(nc, st)

        def SB(name, shape, dt):
            return st.enter_context(nc.sbuf_tensor(name, list(shape), dt))

        ps = [st.enter_context(nc.psum_tensor("ps%d" % i, [128, 512], F32)) for i in range(8)]
        psb16 = [p[:].bitcast(BF16) for p in ps]
        rr = [0]

        def PS():
            i = rr[0]
            rr[0] = (i + 1) % 8
            return i

        evt = [0]

        def EV():
            evt[0] ^= 1
            return "act" if evt[0] else "dve"

        def copy_op(e, out_ap, in_ap, reads, writes):
            if e == "act":
                s.op("act", lambda g: g.copy(out=out_ap, in_=in_ap), reads=reads, writes=writes)
            else:
                s.op(e, lambda g: g.tensor_copy(out=out_ap, in_=in_ap), reads=reads, writes=writes)

        idb = SB("idb", [128, 128], BF16)
        trib = SB("trib", [128, 128], BF16)
        onesb = SB("onesb", [128, 128], BF16)
        epsb = SB("epsb", [128, 1], F32)
        stt = SB("stt", [128, 64], F32)
        arena = SB("arena", [128, ARENA_BYTES // 2], BF16)
        s.dma("pool", idb[:], c_ident, writes=["idb"])
        s.dma("pool", trib[:], c_tri, writes=["trib"])
        s.op("dve", lambda g: g.memset(onesb[:], 1.0), writes=["onesb"])
        s.op("dve", lambda g: g.memset(epsb[:], RMS_EPS), writes=["epsb"])

        class Arena:
            def __init__(self):
                self.off = 0

            def at(self, off, shape, dt):
                n = int(np.prod(shape))
                nb = n * (2 if dt == BF16 else 4)
                assert off % 4 == 0 and off + nb <= ARENA_BYTES, (off, nb)
                if dt == BF16:
                    ap = arena[:, off // 2: off // 2 + n]
                else:
                    ap = arena[:, off // 2: off // 2 + 2 * n].bitcast(F32)
                if len(shape) == 2:
                    ap = ap.rearrange("p (a b) -> p a b", a=shape[0])
                return ap, off + ((nb + 31) // 32) * 32

            def reset(self, off=0):
                self.off = off

            def alloc(self, shape, dt):
                ap, self.off = self.at(self.off, shape, dt)
                return ap

        A = Arena()
        ccsem = [s._newsem("cc"), 0]
        abt = SB("abt", [128, 2], F32)
        s.dma("sp", abt[:], c_ab, writes=["abt"])

        def cc_gather(src, dst, reads, writes):
            s._deps("pool", reads, writes)
            ccsem[1] += 1
            tok = (ccsem[0], ccsem[1])
            s._emit("pool", lambda eng: eng.collective_compute(
                "AllGather", ALU.bypass, replica_groups=RG, ins=[src.opt()], outs=[dst.opt()]).then_inc(ccsem[0]))
            s._commit(tok, reads, writes)

        def rstd(src_ap, junk_ap, col, denom, rkeys, jkeys):
            s.op("act", lambda g: g.activation(out=junk_ap, in_=src_ap, func=AF.Square, accum_out=stt[:, col:col + 1]),
                 reads=rkeys, writes=list(jkeys) + [("stt", col)])
            s.op("act", lambda g: g.activation(out=stt[:, col + 1:col + 2], in_=stt[:, col:col + 1], func=AF.Sqrt,
                                               scale=1.0 / denom, bias=epsb[:]),
                 reads=[("stt", col), "epsb"], writes=[("stt", col + 1)])
            s.op("dve", lambda g: g.reciprocal(out=stt[:, col + 2:col + 3], in_=stt[:, col + 1:col + 2]),
                 reads=[("stt", col + 1)], writes=[("stt", col + 2)])
            return stt[:, col + 2:col + 3], ("stt", col + 2)

        def transposeT(src, skeys, nchunk, dst, dkey, t_off):
            for g0 in range(0, nchunk, 8):
                n = min(8, nchunk - g0)
                b = PS()
                fns = [(lambda e, j=j: e.transpose(out=psb16[b][:, j * 128:(j + 1) * 128],
                                                   in_=src[:, (g0 + j) * 128:(g0 + j + 1) * 128], identity=idb[:]))
                       for j in range(n)]
                s.group("pe", fns, reads=list(skeys) + ["idb"], writes=[("ps", b)])
                copy_op(EV(), dst[:, g0:g0 + n, t_off:t_off + 128],
                        psb16[b][:, 0:n * 128].rearrange("p (k t) -> p k t", k=n), [("ps", b)], [dkey])

        def mm_group(b, ncols, pairs, reads):
            n = len(pairs)
            fns = [(lambda e, i=i: e.matmul(ps[b][:, 0:ncols], lhsT=pairs[i][0], rhs=pairs[i][1],
                                            start=(i == 0), stop=(i == n - 1))) for i in range(n)]
            s.group("pe", fns, reads=reads, writes=[("ps", b)])

        for l in range(L):
            xsrc = x_seq if l == 0 else xres_d
            A.reset()
            gb = A.alloc([D], F32)
            xts = [A.alloc([D], F32) for _ in range(2)]
            xbs = [A.alloc([D], BF16) for _ in range(2)]
            p1keys = ["gb", ("xt", 0), ("xt", 1), ("xb", 0), ("xb", 1)]
            assert A.off <= KC * S * 2
            hTo, _ = A.at(KC * S * 2, [KC, TO], BF16)
            s.dma("sp", gb, gb_pre[l], writes=["gb"])
            tpc = TCH // 128
            for tb in range(TO // 128):
                par = tb % 2
                xt, xb = xts[par], xbs[par]
                s.dma("sp", xt, xsrc[tb * 128:(tb + 1) * 128, :], writes=[("xt", par)])
                r_ap, rk = rstd(xt, xb, 32 * par, D, [("xt", par)], [("xb", par)])
                s.op("dve", lambda g: g.scalar_tensor_tensor(out=xb, in0=xt, scalar=r_ap, in1=gb,
                                                             op0=ALU.mult, op1=ALU.mult),
                     reads=[("xt", par), rk, "gb"], writes=[("xb", par)])
                transposeT(xb, [("xb", par)], KC, hTo, ("hTo", tb), tb * 128)
                if (tb + 1) % tpc == 0:
                    c = tb // tpc
                    s.dma("sp", hTo_d[c].rearrange("(p k) t -> p k t", k=KC), hTo[:, :, c * TCH:(c + 1) * TCH],
                          reads=[("hTo", t_) for t_ in range(c * tpc, (c + 1) * tpc)], writes=[("hTod", c)])
                    cc_gather(hTo_d[c], hTg_d[c], [("hTod", c)], [("hTgd", c)])
            hT, p_mark = A.at(0, [KC, S], BF16)
            hTo_keys = [("hTo", t_) for t_ in range(TO // 128)]
            for c in range(NCH):
                for r in range(NR):
                    t0 = r * TO + c * TCH
                    s.dma("sp", hT[:, :, t0:t0 + TCH],
                          hTg_d[c, r * 128 * KC:(r + 1) * 128 * KC, :].rearrange("(p k) t -> p k t", k=KC),
                          reads=[("hTgd", c)], writes=[("hT", tb_) for tb_ in range(t0 // 128, (t0 + TCH) // 128)] + p1keys)
            A.reset(p_mark)
            CT = 256
            wbufs = [A.alloc([KC, CT], BF16) for _ in range(3)]
            stg = [A.alloc([512], BF16) for _ in range(2)]
            stgf = [A.alloc([512], F32) for _ in range(2)]
            si = 0
            for ct in range(INW // CT):
                wb = wbufs[ct % 3]
                wk = ("w2", ct % 3)
                s.dma("pool", wb, w_in[l, :, ct * CT:(ct + 1) * CT].rearrange("(k p) n -> p k n", p=128),
                      writes=[wk] + (hTo_keys if ct < 3 else []))
                seg = (ct * CT) // GW
                off = ct * CT - seg * GW
                if seg in (2, 5, 8):
                    m = {2: 0, 5: 1, 8: 2}[seg]
                    for tb in range(TB):
                        b = PS()
                        mm_group(b, CT, [(hT[:, k, tb * 128:(tb + 1) * 128], wb[:, k, :]) for k in range(KC)],
                                 [("hT", tb), wk])
                        si ^= 1
                        copy_op(EV(), stg[si][:, 0:CT], ps[b][:, 0:CT], [("ps", b)], [("stg", si)])
                        s.dma("sp", v_d[m, tb * 128:(tb + 1) * 128, off:off + CT], stg[si][:, 0:CT], reads=[("stg", si)])
                else:
                    for sub in range(CT // 128):
                        h = (off + sub * 128) // 128
                        for tc in range(NQC):
                            b = PS()
                            mm_group(b, 512, [(wb[:, k, sub * 128:(sub + 1) * 128], hT[:, k, tc * 512:(tc + 1) * 512])
                                              for k in range(KC)],
                                     [("hT", tc * 4 + i) for i in range(4)] + [wk])
                            si ^= 1
                            if seg == 9:
                                copy_op(EV(), stgf[si], ps[b][:], [("ps", b)], [("stgf", si)])
                                s.dma("sp", u_d[h, :, tc * 512:(tc + 1) * 512], stgf[si], reads=[("stgf", si)])
                            else:
                                idx = {0: 0, 1: 1, 3: 2, 4: 3, 6: 4, 7: 5}[seg]
                                copy_op(EV(), stg[si], ps[b][:], [("ps", b)], [("stg", si)])
                                s.dma("sp", qkT_d[idx, h, :, tc * 512:(tc + 1) * 512], stg[si], reads=[("stg", si)])
            s.barrier()

            A.reset()
            ystage = A.alloc([TB, GW], BF16)
            msd = A.alloc([S], BF16)
            msc = A.alloc([S], BF16)
            mss = A.alloc([S], BF16)
            s.dma("pool", msd, c_msd, writes=["msd"])
            s.dma("pool", msc, c_msc, writes=["msc"])
            s.dma("pool", mss, c_mss, writes=["mss"])
            qT = [A.alloc([S], BF16) for _ in range(2)]
            kT = [A.alloc([S], BF16) for _ in range(2)]
            vx = [A.alloc([TB, 130], BF16) for _ in range(2)]
            for i in range(2):
                s.op("dve", lambda g, i=i: g.memset(vx[i][:, :, 128:129], 1.0), writes=[("vx1", i)])
            Ealls = [A.alloc([TB, 512], BF16) for _ in range(2)]
            E2s = [A.alloc([2, 512], BF16) for _ in range(2)]
            acc = A.alloc([4, 129], F32)
            nm = A.alloc([NBM, 8], F32)
            s.dma("sp", nm, c_nm, writes=["nm"])
            km = A.alloc([NBM], F32)
            kmh = A.alloc([NBM], BF16)
            kml = A.alloc([NBM], BF16)
            g8 = A.alloc([8], F32)
            top8 = A.alloc([8], F32)
            sels = [A.alloc([TB, 8], F32) for _ in range(2)]
            s.op("dve", lambda g: g.memset(g8, -1e30), writes=["g8"])
            t_e = A.alloc([512], F32)
            t_sp = A.alloc([512], F32)
            t_1 = A.alloc([512], F32)
            t_3 = A.alloc([512], F32)
            spm = A.alloc([512], BF16)
            Rb = A.alloc([512], F32)
            p3_mark = A.off
            hcount = [0]

            def load_head(m, h):
                i = hcount[0] % 2
                hcount[0] += 1
                s.dma("sp", qT[i], qkT_d[2 * m, h], writes=[("qT", i)])
                s.dma("sp", kT[i], qkT_d[2 * m + 1, h], writes=[("kT", i)])
                s.dma("sp", vx[i][:, :, 0:128],
                      v_d[m, :, h * 128:(h + 1) * 128].rearrange("(tb p) d -> p tb d", p=128), writes=[("vx", i)])
                return i

            def finalize(src_ap, den_ap, rkeys, qb, h):
                s.op("dve", lambda g: g.reciprocal(out=stt[:, 8:9], in_=den_ap), reads=rkeys, writes=[("stt", 8)])
                s.op("dve", lambda g: g.tensor_scalar(out=ystage[:, qb, h * 128:(h + 1) * 128], in0=src_ap,
                                                      scalar1=stt[:, 8:9], scalar2=None, op0=ALU.mult),
                     reads=list(rkeys) + [("stt", 8)], writes=[("ys", qb)])

            def store_mixer(mi):
                s.dma("sp", y_d[mi].rearrange("(tb p) c -> p tb c", p=128), ystage,
                      reads=[("ys", qb) for qb in range(TB)], writes=[("yd", mi)])
                cc_gather(y_d[mi], yg_d[mi], [("yd", mi)], [("ygd", mi)])

            mtog = [0]

            def mask_eng():
                mtog[0] ^= 1
                return "pool" if mtog[0] else "dve"

            def moba_gates(i):
                s.op("dve", lambda g: g.tensor_reduce(out=km, in_=kT[i].rearrange("p (n k) -> p n k", k=MOBA_BLOCK),
                                                      axis=AX.X, op=ALU.add), reads=[("kT", i)], writes=["km"])
                s.op("dve", lambda g: g.tensor_scalar(out=kmh, in0=km, scalar1=1.0 / MOBA_BLOCK, scalar2=None,
                                                      op0=ALU.mult), reads=["km"], writes=["kmh"])
                s.op("dve", lambda g: g.scalar_tensor_tensor(out=kml, in0=km, scalar=1.0 / MOBA_BLOCK, in1=kmh,
                                                             op0=ALU.mult, op1=ALU.subtract),
                     reads=["km", "kmh"], writes=["kml"])
                for tb in range(TB):
                    b = PS()
                    mm_group(b, NBM, [(qT[i][:, tb * 128:(tb + 1) * 128], kmh),
                                      (qT[i][:, tb * 128:(tb + 1) * 128], kml)], [("qT", i), "kmh", "kml"])
                    blk = tb // 2
                    s.op("dve", lambda g: g.tensor_tensor(out=g8[:, 0:NBM], in0=ps[b][:, 0:NBM], in1=nm[:, blk, 0:NBM],
                                                          op=ALU.add), reads=[("ps", b), "nm"], writes=["g8"])
                    s.op("dve", lambda g: g.max(out=top8, in_=g8), reads=["g8"], writes=["top8"])
                    s.op("dve", lambda g: g.tensor_scalar(out=sels[i][:, tb, :], in0=g8, scalar1=top8[:, 2:3], scalar2=None,
                                                          op0=ALU.is_ge), reads=["g8", "top8"], writes=[("sel", i, tb)])

            heads_i = {}

            def a_s1(u, h, qc, n):
                if (qc, n) == (0, 0):
                    heads_i[h] = load_head(0, h)
                    moba_gates(heads_i[h])
                i = heads_i[h]
                hk = [("qT", i), ("kT", i), ("vx", i), ("vx1", i)]
                E2, ek = E2s[u % 2], ("E2", u % 2)
                q0 = max(qc * 512, n * 256)
                W = (qc + 1) * 512 - q0
                c0 = q0 - qc * 512
                own = (n >= 2 * qc)
                for j in range(2):
                    kb = 2 * n + j
                    b = PS()
                    mm_group(b, W, [(kT[i][:, kb * 128:(kb + 1) * 128], qT[i][:, q0:q0 + W])], hk)
                    s.op("act", lambda g: g.activation(out=E2[:, j, c0:512], in_=ps[b][:, 0:W], func=AF.Exp,
                                                       scale=scale), reads=[("ps", b)], writes=[ek + (j,)])
                    if own and j == 0:
                        s.op(mask_eng(), lambda g: g.tensor_tensor(out=E2[:, 0, c0:c0 + 256], in0=E2[:, 0, c0:c0 + 256],
                                                                   in1=msc[:, 0:256], op=ALU.mult),
                             reads=[ek + (0,), "msc"], writes=[ek + (0,)])
                    elif own:
                        s.op("pool", lambda g: g.memset(E2[:, 1, c0:c0 + 128], 0.0), reads=[ek + (1,)], writes=[ek + (1,)])
                        s.op(mask_eng(), lambda g: g.tensor_tensor(out=E2[:, 1, c0 + 128:c0 + 256],
                                                                   in0=E2[:, 1, c0 + 128:c0 + 256],
                                                                   in1=msc[:, 0:128], op=ALU.mult),
                             reads=[ek + (1,), "msc"], writes=[ek + (1,)])

            def a_s2(u, h, qc, n):
                i = heads_i[h]
                hk = [("qT", i), ("kT", i), ("vx", i), ("vx1", i)]
                E2, ek = E2s[u % 2], ("E2", u % 2)
                q0 = max(qc * 512, n * 256)
                c0 = q0 - qc * 512
                sel = sels[i]
                for sub in range(c0 // 128, 4):
                    qb = qc * 4 + sub
                    b = PS()
                    mm_group(b, 129, [(E2[:, 0, sub * 128:(sub + 1) * 128], vx[i][:, 2 * n, 0:129]),
                                      (E2[:, 1, sub * 128:(sub + 1) * 128], vx[i][:, 2 * n + 1, 0:129])],
                             [ek + (0,), ek + (1,)] + hk)
                    ak = ("acc", sub)
                    if n == 0:
                        if qb // 2 == 0:
                            s.op("dve", lambda g: g.tensor_copy(out=acc[:, sub, :], in_=ps[b][:, 0:129]),
                                 reads=[("ps", b)], writes=[ak])
                        else:
                            s.op("dve", lambda g: g.tensor_scalar(out=acc[:, sub, :], in0=ps[b][:, 0:129],
                                                                  scalar1=sel[:, qb, 0:1], scalar2=None, op0=ALU.mult),
                                 reads=[("ps", b), ("sel", i, qb)], writes=[ak])
                    elif n == qb // 2:
                        s.op("dve", lambda g: g.tensor_tensor(out=acc[:, sub, :], in0=ps[b][:, 0:129],
                                                              in1=acc[:, sub, :], op=ALU.add),
                             reads=[("ps", b), ak], writes=[ak])
                    else:
                        s.op("dve", lambda g: g.scalar_tensor_tensor(out=acc[:, sub, :], in0=ps[b][:, 0:129],
                                                                     scalar=sel[:, qb, n:n + 1], in1=acc[:, sub, :],
                                                                     op0=ALU.mult, op1=ALU.add),
                             reads=[("ps", b), ak, ("sel", i, qb)], writes=[ak])
                if n == 2 * qc + 1:
                    for sub in range(4):
                        finalize(acc[:, sub, 0:128], acc[:, sub, 128:129], [("acc", sub)], qc * 4 + sub, h)

            unitsA = [(h, qc, n) for h in range(NH) for qc in range(NQC) for n in range(2 * qc + 2)]
            for u, un in enumerate(unitsA):
                a_s1(u, *un)
                if u > 0:
                    a_s2(u - 1, *unitsA[u - 1])
            a_s2(len(unitsA) - 1, *unitsA[-1])
            store_mixer(0)

            A.reset(p3_mark)
            spf = A.alloc([TB, 512], F32)
            spmA = A.alloc([TB, 512], BF16)
            t_e2 = [t_e, A.alloc([512], F32)]
            t_12 = [t_1, A.alloc([512], F32)]
            t_32 = [t_3, A.alloc([512], F32)]
            bcnt = [0, 0]

            def b_geo(qc, kb):
                q0 = max(qc * 512, kb * 128)
                return q0, (qc + 1) * 512 - q0, q0 - qc * 512, kb * 128 >= qc * 512

            def b_s1(i, qc, kb):
                hk = [("qT", i), ("kT", i), ("vx", i)]
                q0, W, c0, diag = b_geo(qc, kb)
                bcnt[0] ^= 1
                te, tek = t_e2[bcnt[0]], ("t_e", bcnt[0])
                bz = PS()
                mm_group(bz, W, [(kT[i][:, kb * 128:(kb + 1) * 128], qT[i][:, q0:q0 + W])], hk)
                s.op("act", lambda g: g.activation(out=te[:, 0:W], in_=ps[bz][:, 0:W], func=AF.Exp, scale=scale),
                     reads=[("ps", bz)], writes=[tek])
                s.op("act", lambda g: g.activation(out=spf[:, kb, c0:512], in_=te[:, 0:W], func=AF.Ln, bias=1.0),
                     reads=[tek], writes=[("spf", kb)])
                if diag:
                    if c0 > 0:
                        s.op("pool", lambda g: g.memset(spmA[:, kb, 0:c0], 0.0), writes=[("spm0", kb)])
                    s.op("dve", lambda g: g.tensor_tensor(out=spmA[:, kb, c0:c0 + 128], in0=spf[:, kb, c0:c0 + 128],
                                                          in1=mss[:, 0:128], op=ALU.mult),
                         reads=[("spf", kb), "mss"], writes=[("spm", kb)])
                    if W > 128:
                        s.op("pool", lambda g: g.tensor_copy(out=spmA[:, kb, c0 + 128:512], in_=spf[:, kb, c0 + 128:512]),
                             reads=[("spf", kb)], writes=[("spm2", kb)])
                else:
                    s.op("pool", lambda g: g.tensor_copy(out=spmA[:, kb, :], in_=spf[:, kb, :]),
                         reads=[("spf", kb)], writes=[("spm", kb), ("spm2", kb), ("spm0", kb)])

            def b_s2(i, qc, kb, nkb, Eall, ek):
                hk = [("qT", i), ("kT", i), ("vx", i)]
                q0, W, c0, diag = b_geo(qc, kb)
                bcnt[1] ^= 1
                t1, t1k = t_12[bcnt[1]], ("t_1", bcnt[1])
                t3, t3k = t_32[bcnt[1]], ("t_3", bcnt[1])
                bc = PS()
                mm_group(bc, W, [(trib[:], spmA[:, kb, c0:512])] +
                         [(onesb[:], spmA[:, k2, c0:512]) for k2 in range(kb + 1, nkb)],
                         ["trib", "onesb"] + [(nm_, k2) for k2 in range(kb, nkb) for nm_ in ("spm", "spm2", "spm0")])
                bz = PS()
                mm_group(bz, W, [(kT[i][:, kb * 128:(kb + 1) * 128], qT[i][:, q0:q0 + W])], hk)
                s.op("dve", lambda g: g.tensor_tensor(out=t1[:, 0:W], in0=ps[bc][:, 0:W], in1=spf[:, kb, c0:512],
                                                      op=ALU.add), reads=[("ps", bc), ("spf", kb)], writes=[t1k])
                s.op("dve", lambda g: g.scalar_tensor_tensor(out=t3[:, 0:W], in0=ps[bz][:, 0:W], scalar=scale,
                                                             in1=t1[:, 0:W], op0=ALU.mult, op1=ALU.subtract),
                     reads=[("ps", bz), t1k], writes=[t3k])
                s.op("act", lambda g: g.activation(out=Eall[:, kb, c0:512], in_=t3[:, 0:W], func=AF.Exp),
                     reads=[t3k], writes=[ek + (kb,)])
                if diag:
                    s.op("pool", lambda g: g.tensor_tensor(out=Eall[:, kb, c0:c0 + 128], in0=Eall[:, kb, c0:c0 + 128],
                                                           in1=mss[:, 0:128], op=ALU.mult),
                         reads=[ek + (kb,), "mss"], writes=[ek + (kb,)])

            def b_pv(i, h, qc, Eall, ek):
                hk = [("qT", i), ("kT", i), ("vx", i)]
                for sub in range(4):
                    qb = qc * 4 + sub
                    b = PS()
                    mm_group(b, 128, [(Eall[:, kb, sub * 128:(sub + 1) * 128], vx[i][:, kb, 0:128]) for kb in range(qb + 1)],
                             [ek + (kb,) for kb in range(qb + 1)] + hk)
                    copy_op(EV(), ystage[:, qb, h * 128:(h + 1) * 128], ps[b][:, 0:128], [("ps", b)], [("ys", qb)])

            pend = None
            ucnt = 0
            for h in range(NH):
                i = load_head(1, h)
                for qc in range(NQC):
                    nkb = (qc + 1) * 4
                    Eall, ek = Ealls[ucnt % 2], ("E", ucnt % 2)
                    ucnt += 1
                    kbs = list(reversed(range(nkb)))
                    LA = 2
                    for kb in kbs[:LA]:
                        b_s1(i, qc, kb)
                    if pend is not None:
                        b_pv(*pend)
                        pend = None
                    for idx, kb in enumerate(kbs):
                        if idx + LA < len(kbs):
                            b_s1(i, qc, kbs[idx + LA])
                        b_s2(i, qc, kb, nkb, Eall, ek)
                    pend = (i, h, qc, Eall, ek)
            b_pv(*pend)
            store_mixer(1)

            def d_s1(u, h, qc):
                if qc == 0:
                    heads_i[("d", h)] = load_head(2, h)
                i = heads_i[("d", h)]
                hk = [("qT", i), ("kT", i), ("vx", i), ("vx1", i)]
                Eall, ek = Ealls[u % 2], ("E", u % 2)
                for kb in range((qc + 1) * 4):
                    q0 = max(qc * 512, kb * 128)
                    W = (qc + 1) * 512 - q0
                    c0 = q0 - qc * 512
                    b = PS()
                    mm_group(b, W, [(kT[i][:, kb * 128:(kb + 1) * 128], qT[i][:, q0:q0 + W])], hk)
                    s.op("act", lambda g: g.activation(out=Eall[:, kb, c0:512], in_=ps[b][:, 0:W], func=AF.Exp, scale=scale),
                         reads=[("ps", b)], writes=[ek + (kb,)])
                    mo = q0 - 128 * kb
                    s.op(mask_eng(), lambda g: g.tensor_tensor(out=Eall[:, kb, c0:512], in0=Eall[:, kb, c0:512],
                                                               in1=msd[:, mo:mo + W], op=ALU.mult),
                         reads=[ek + (kb,), "msd"], writes=[ek + (kb,)])

            def d_s2(u, h, qc):
                i = heads_i[("d", h)]
                hk = [("qT", i), ("kT", i), ("vx", i), ("vx1", i)]
                Eall, ek = Ealls[u % 2], ("E", u % 2)
                for sub in range(4):
                    qb = qc * 4 + sub
                    b = PS()
                    mm_group(b, 129, [(Eall[:, kb, sub * 128:(sub + 1) * 128], vx[i][:, kb, 0:129]) for kb in range(qb + 1)],
                             [ek + (kb,) for kb in range(qb + 1)] + hk)
                    finalize(ps[b][:, 0:128], ps[b][:, 128:129], [("ps", b)], qb, h)

            unitsD = [(h, qc) for h in range(NH) for qc in range(NQC)]
            for u, un in enumerate(unitsD):
                d_s1(u, *un)
                if u > 0:
                    d_s2(u - 1, *unitsD[u - 1])
            d_s2(len(unitsD) - 1, *unitsD[-1])
            store_mixer(3)

            s.barrier()
            A.reset(p3_mark)
            up = A.alloc([16 + S], F32)
            pa = A.alloc([16 + S], F32)
            pb = A.alloc([16 + S], F32)
            coef = [A.alloc([S], F32) for _ in range(4)]
            tmpf = A.alloc([S], F32)
            tmp2 = A.alloc([S], F32)
            dT = A.alloc([PC, S], BF16)
            wp = A.alloc([PC, PGD], BF16)
            pst = A.alloc([GW], F32)
            s.dma("sp", pst, psb[l], writes=["pst"])
            for t_, k_ in ((up, "up"), (pa, "pa"), (pb, "pb")):
                s.op("dve", lambda g, t_=t_: g.memset(t_[:, 0:16], 0.0), writes=[k_ + "z"])
            for gi in range(PGn):
                s.dma("pool", wp, w_pool[l, gi].rearrange("(c p) d -> p c d", p=128), writes=["wp"])
                for wi in range(4):
                    s.dma("sp", coef[wi], c_pcoef[gi, wi], writes=[("coef", wi)])
                for c in range(PC):
                    ch = gi * PC + c
                    s.dma("sp", up[:, 16:16 + S], u_d[ch], writes=["up"])
                    cur, ck = up, "up"
                    for wi in range(4):
                        sh = 1 << wi
                        nxt, nk = (pa, "pa") if cur is not pa else (pb, "pb")
                        s.op("dve", lambda g, cur=cur, nxt=nxt, sh=sh: g.tensor_tensor(
                            out=nxt[:, 16:16 + S], in0=cur[:, 16:16 + S], in1=cur[:, 16 - sh:16 - sh + S], op=ALU.add),
                             reads=[ck, ck + "z"], writes=[nk])
                        cur, ck = nxt, nk
                        if wi == 0:
                            s.op("pool", lambda g, cur=cur: g.tensor_tensor(out=tmpf, in0=cur[:, 16:16 + S], in1=coef[0],
                                                                            op=ALU.mult), reads=[ck, ("coef", 0)], writes=["tmpf"])
                        else:
                            s.op("pool", lambda g, cur=cur: g.tensor_tensor(out=tmp2, in0=cur[:, 16:16 + S], in1=coef[wi],
                                                                            op=ALU.mult), reads=[ck, ("coef", wi)], writes=["tmp2"])
                            s.op("pool", lambda g: g.tensor_tensor(out=tmpf, in0=tmpf, in1=tmp2, op=ALU.add),
                                 reads=["tmpf", "tmp2"], writes=["tmpf"])
                    s.op("dve", lambda g: g.tensor_tensor(out=dT[:, c, :], in0=tmpf, in1=up[:, 16:16 + S],
                                                          op=ALU.subtract), reads=["tmpf", "up"], writes=[("dT", c)])
                for tb in range(TB):
                    b = PS()
                    mm_group(b, PGD, [(dT[:, c, tb * 128:(tb + 1) * 128], wp[:, c, :]) for c in range(PC)],
                             [("dT", c) for c in range(PC)] + ["wp"])
                    s.op("dve", lambda g: g.tensor_tensor(out=ystage[:, tb, gi * PGD:(gi + 1) * PGD], in0=ps[b][:, 0:PGD],
                                                          in1=pst[:, gi * PGD:(gi + 1) * PGD], op=ALU.mult),
                         reads=[("ps", b), "pst"], writes=[("ys", tb)])
            store_mixer(2)
            s.barrier()

            dst = out if l == L - 1 else xres_d
            LOW = 0
            MID1 = 32 * 1024
            MID2 = 64 * 1024
            MSB_OFF = ARENA_BYTES - 4 * D * 4
            ACT_OFF = ARENA_BYTES - ((FC * TG * 2 + 31) // 32) * 32
            assert KC2 * TG * 2 <= MID1 and KC * TG * 2 <= MID1 and 4 * D * 4 <= MID2
            for tg in range(NTG):
                yT, _ = A.at(LOW, [KC2, TG], BF16)
                A.reset(MID1)
                gbm = A.alloc([MW], F32)
                yts = [A.alloc([MW], BF16) for _ in range(2)]
                yt1s = [A.alloc([MW], BF16) for _ in range(2)]
                ybs = [A.alloc([MW], BF16) for _ in range(2)]
                assert A.off <= MSB_OFF
                s.dma("sp", gbm, gb_mo[l], writes=["gbm"])
                ygk = [("ygd", m) for m in range(4)]
                for tb in range(4):
                    T0 = tg * TG + tb * 128
                    par = tb % 2
                    so = 32 * par
                    yt, yt1, yb = yts[par], yt1s[par], ybs[par]
                    ytk = [("ytp", par, 0, r) for r in range(NR)]
                    for r in range(NR):
                        s.dma("sp", yt[:, r * MWL:(r + 1) * MWL].rearrange("p (m c) -> p m c", m=4),
                              yg_d[:, r * S + T0:r * S + T0 + 128, :].rearrange("m p c -> p m c"),
                              reads=ygk, writes=[("ytp", par, 0, r)])
                        if NR > 1:
                            s.dma("sp", yt1[:, r * MWL:(r + 1) * MWL].rearrange("p (m c) -> p m c", m=4),
                                  yg_d[:, r * S + TO + T0:r * S + TO + T0 + 128, :].rearrange("m p c -> p m c"),
                                  reads=ygk, writes=[("ytp", par, 1, r)])
                    if NR > 1:
                        s.op("pool", lambda g: g.tensor_scalar(out=yt, in0=yt, scalar1=abt[:, 0:1], scalar2=None, op0=ALU.mult),
                             reads=ytk + ["abt"], writes=ytk)
                        s.op("dve", lambda g: g.scalar_tensor_tensor(out=yt, in0=yt1, scalar=abt[:, 1:2], in1=yt,
                                                                     op0=ALU.mult, op1=ALU.add),
                             reads=[("ytp", par, 1, r) for r in range(NR)] + ["abt"] + ytk, writes=ytk)
                    for gi, ranges in enumerate(groups):
                        for ri, (c0, cl) in enumerate(ranges):
                            col = so + 12 + ri
                            s.op("act", lambda g: g.activation(out=yb[:, c0:c0 + cl], in_=yt[:, c0:c0 + cl],
                                                               func=AF.Square, accum_out=stt[:, col:col + 1]),
                                 reads=ytk, writes=[("yb", par, gi), ("stt", col)])
                        if len(ranges) == 2:
                            s.op("dve", lambda g: g.tensor_tensor(out=stt[:, so + 12:so + 13], in0=stt[:, so + 12:so + 13],
                                                                  in1=stt[:, so + 13:so + 14], op=ALU.add),
                                 reads=[("stt", so + 12), ("stt", so + 13)], writes=[("stt", so + 12)])
                        s.op("act", lambda g: g.activation(out=stt[:, so + 14:so + 15], in_=stt[:, so + 12:so + 13], func=AF.Sqrt,
                                                           scale=1.0 / NGW, bias=epsb[:]),
                             reads=[("stt", so + 12), "epsb"], writes=[("stt", so + 14)])
                        s.op("dve", lambda g: g.reciprocal(out=stt[:, so + 16 + gi:so + 17 + gi], in_=stt[:, so + 14:so + 15]),
                             reads=[("stt", so + 14)], writes=[("stt", so + 16 + gi)])
                        for (c0, cl) in ranges:
                            s.op("dve", lambda g: g.scalar_tensor_tensor(out=yb[:, c0:c0 + cl], in0=yt[:, c0:c0 + cl],
                                                                         scalar=stt[:, so + 16 + gi:so + 17 + gi],
                                                                         in1=gbm[:, c0:c0 + cl], op0=ALU.mult, op1=ALU.mult),
                                 reads=ytk + [("stt", so + 16 + gi), "gbm"], writes=[("yb", par, gi)])
                    transposeT(yb, [("yb", par, gi) for gi in range(len(groups))], KC2, yT, ("yT", tb), tb * 128)
                s.barrier()
                msb, _ = A.at(MSB_OFF, [4, D], F32)
                A.reset(MID1)
                wobuf = [A.alloc([KC2, 512], BF16) for _ in range(2)]
                assert A.off <= MSB_OFF
                for nt in range(D // 512):
                    wb = wobuf[nt % 2]
                    wk = ("wo", nt % 2)
                    s.dma("pool", wb, w_out[l, :, nt * 512:(nt + 1) * 512].rearrange("(k p) n -> p k n", p=128), writes=[wk])
                    for tb in range(4):
                        b = PS()
                        mm_group(b, 512, [(yT[:, k, tb * 128:(tb + 1) * 128], wb[:, k, :]) for k in range(KC2)],
                                 [("yT", tb), wk])
                        copy_op(EV(), msb[:, tb, nt * 512:(nt + 1) * 512], ps[b][:], [("ps", b)], [("msb", tb)])
                s.barrier()
                h2T, _ = A.at(LOW, [KC, TG], BF16)
                A.reset(MID1)
                gb1 = A.alloc([D], F32)
                gb2 = A.alloc([D], F32)
                xts = [A.alloc([D], F32) for _ in range(2)]
                xbs = [A.alloc([D], BF16) for _ in range(2)]
                assert A.off <= MSB_OFF
                s.dma("sp", gb1, gb_post[l], writes=["gb1"])
                s.dma("sp", gb2, gb_fpre[l], writes=["gb2"])
                for tb in range(4):
                    T0 = tg * TG + tb * 128
                    par = tb % 2
                    xt, xb = xts[par], xbs[par]
                    xk, bk = ("xt", par), ("xb", par)
                    s.dma("sp", xt, xsrc[T0:T0 + 128, :], writes=[xk])
                    r_ap, rk = rstd(msb[:, tb, :], xb, 32 * par, D, [("msb", tb)], [bk])
                    s.op("dve", lambda g: g.scalar_tensor_tensor(out=msb[:, tb, :], in0=msb[:, tb, :], scalar=r_ap,
                                                                 in1=gb1, op0=ALU.mult, op1=ALU.mult),
                         reads=[("msb", tb), rk, "gb1"], writes=[("msb", tb)])
                    s.op("pool", lambda g: g.tensor_tensor(out=xt, in0=xt, in1=msb[:, tb, :], op=ALU.add),
                         reads=[xk, ("msb", tb)], writes=[xk])
                    s.dma("sp", xres_d[T0:T0 + 128, :], xt, reads=[xk])
                    r_ap, rk = rstd(xt, xb, 32 * par + 4, D, [xk], [bk])
                    s.op("dve", lambda g: g.scalar_tensor_tensor(out=xb, in0=xt, scalar=r_ap, in1=gb2,
                                                                 op0=ALU.mult, op1=ALU.mult),
                         reads=[xk, rk, "gb2"], writes=[bk])
                    transposeT(xb, [bk], KC, h2T, ("h2T", tb), tb * 128)
                s.barrier()
                actT, _ = A.at(ACT_OFF, [FC, TG], BF16)
                A.reset(MID1)
                CT2 = 256
                wgu = [A.alloc([KC, CT2], BF16) for _ in range(3)]
                sg = A.alloc([2, TG], F32)
                assert A.off <= ACT_OFF
                wi = 0
                h2keys = [("h2T", tb) for tb in range(4)]
                for j in range(DFF // CT2):
                    wg, wgk = wgu[wi % 3], ("wgu", wi % 3)
                    wi += 1
                    s.dma("pool", wg, w_gate[l, :, j * CT2:(j + 1) * CT2].rearrange("(k p) n -> p k n", p=128), writes=[wgk])
                    wu, wuk = wgu[wi % 3], ("wgu", wi % 3)
                    wi += 1
                    s.dma("pool", wu, w_up[l, :, j * CT2:(j + 1) * CT2].rearrange("(k p) n -> p k n", p=128), writes=[wuk])
                    for c in range(2):
                        b = PS()
                        mm_group(b, TG, [(wg[:, k, c * 128:(c + 1) * 128], h2T[:, k, :]) for k in range(KC)], h2keys + [wgk])
                        s.op("act", lambda g: g.activation(out=sg[:, c, :], in_=ps[b][:], func=AF.Silu),
                             reads=[("ps", b)], writes=[("sg", c)])
                    for c in range(2):
                        b = PS()
                        mm_group(b, TG, [(wu[:, k, c * 128:(c + 1) * 128], h2T[:, k, :]) for k in range(KC)], h2keys + [wuk])
                        s.op("dve", lambda g: g.tensor_tensor(out=actT[:, 2 * j + c, :], in0=ps[b][:], in1=sg[:, c, :],
                                                              op=ALU.mult),
                             reads=[("ps", b), ("sg", c)], writes=[("actT", 2 * j + c)])
                s.barrier()
                fsb, _ = A.at(LOW, [4, D], F32)
                A.reset(MID2)
                FG = 8
                wdb = [A.alloc([FG, 512], BF16) for _ in range(3)]
                assert A.off <= ACT_OFF
                nfg = (FC + FG - 1) // FG
                wdi = 0
                for nt in range(D // 512):
                    banks = [PS() for _ in range(4)]
                    for fg in range(nfg):
                        f0 = fg * FG
                        nf = min(FG, FC - f0)
                        wd, wdk = wdb[wdi % 3], ("wd", wdi % 3)
                        wdi += 1
                        s.dma("pool", wd[:, 0:nf, :],
                              w_down[l, f0 * 128:(f0 + nf) * 128, nt * 512:(nt + 1) * 512].rearrange("(c p) n -> p c n", p=128),
                              writes=[wdk])
                        for tb in range(4):
                            fns = [(lambda e, fc=fc, tb=tb, wd=wd: e.matmul(ps[banks[tb]][:], lhsT=actT[:, fc, tb * 128:(tb + 1) * 128],
                                                                            rhs=wd[:, fc - f0, :], start=(fc == 0), stop=(fc == FC - 1)))
                                   for fc in range(f0, f0 + nf)]
                            s.group("pe", fns, reads=[("actT", fc) for fc in range(f0, f0 + nf)] + [wdk],
                                    writes=[("ps", banks[tb])])
                    for tb in range(4):
                        copy_op(EV(), fsb[:, tb, nt * 512:(nt + 1) * 512], ps[banks[tb]][:], [("ps", banks[tb])], [("fsb", tb)])
                s.barrier()
                A.reset(MID2)
                gb1 = A.alloc([D], F32)
                xts = [A.alloc([D], F32) for _ in range(2)]
                xbs = [A.alloc([D], BF16) for _ in range(2)]
                s.dma("sp", gb1, gb_fpost[l], writes=["gb1"])
                for tb in range(4):
                    T0 = tg * TG + tb * 128
                    par = tb % 2
                    xt, xb = xts[par], xbs[par]
                    xk, bk = ("xt", par), ("xb", par)
                    s.dma("sp", xt, xres_d[T0:T0 + 128, :], writes=[xk])
                    r_ap, rk = rstd(fsb[:, tb, :], xb, 32 * par, D, [("fsb", tb)], [bk])
                    s.op("dve", lambda g: g.scalar_tensor_tensor(out=fsb[:, tb, :], in0=fsb[:, tb, :], scalar=r_ap,
                                                                 in1=gb1, op0=ALU.mult, op1=ALU.mult),
                         reads=[("fsb", tb), rk, "gb1"], writes=[("fsb", tb)])
                    s.op("pool", lambda g: g.tensor_tensor(out=xt, in0=xt, in1=fsb[:, tb, :], op=ALU.add),
                         reads=[xk, ("fsb", tb)], writes=[xk])
                    s.dma("sp", dst[T0:T0 + 128, :], xt, reads=[xk])
                s.barrier()
        s.finish()
    print("instructions:", s.n_inst, "sems:", s.nsem)
    return nc


def make_consts(S):
    kin = np.arange(128)[:, None]
    j = np.arange(S)[None, :]
    delta = j - kin
    msd = np.zeros((128, S), np.float32)
    for window, dil in DILATED_PATTERNS:
        msd += ((delta >= 0) & (delta <= window) & (delta % dil == 0)).astype(np.float32)
    msc = (delta >= 0).astype(np.float32)
    mss = (delta > 0).astype(np.float32)
    nbm = S // MOBA_BLOCK
    nm = np.zeros((128, nbm, 8), np.float32)
    for blk in range(nbm):
        for n in range(8):
            if n >= blk:
                nm[:, blk, n] = -1e30
    jj = np.arange(128)[:, None]
    ss = np.arange(128)[None, :]
    tri = (jj > ss).astype(np.float32)
    return {"c_ident": np.eye(128, dtype=np.float32), "c_tri": tri, "c_msd": msd, "c_msc": msc, "c_mss": mss, "c_nm": nm}


def pool_coef(S, windows):
    t = np.arange(S)
    out = np.zeros((len(windows), 4, 128, S), np.float32)
    for g, w in enumerate(windows):
        wi = {2: 0, 4: 1, 8: 2, 16: 3}[w]
        out[g, wi] = np.broadcast_to(1.0 / np.minimum(w, t + 1).astype(np.float32), (128, S))
    return out


def bcast128(v):
    return np.ascontiguousarray(np.broadcast_to(v[:, None, :], (v.shape[0], 128, v.shape[1]))).astype(np.float32)


_NC_CACHE = {}


def kernel(x, ln_mix_pre, w_in, w_pool, pool_scale, mix_out_norm, w_out, ln_mix_post, ln_ffn_pre,
           w_gate, w_up, w_down, ln_ffn_post):
    x = np.asarray(x, np.float32)
    B, S, D = x.shape
    L = w_in.shape[0]
    DFF = w_gate.shape[2]
    GWF = D // 4
    NR = 2
    GW = GWF // NR
    NH = GW // 128
    TO = S // NR
    PGD = GWF // 4
    PGn = 4 // NR
    n_cores = B * NR
    perm = np.concatenate([np.arange(m * GWF + r * GW, m * GWF + (r + 1) * GW) for r in range(NR) for m in range(4)])
    groups = [[(r * 4 * GW + m * GW, GW) for r in range(NR)] for m in range(4)]
    cfg = dict(S=S, D=D, NH=NH, DFF=DFF, TO=TO, L=L, PGn=PGn, PGD=PGD, NGW=GWF, windows=None, groups=groups,
               NR=NR, RG=[[2 * i, 2 * i + 1] for i in range(B)])
    key = (S, D, NH, DFF, L, NR, B)
    if key not in _NC_CACHE:
        _NC_CACHE[key] = build(cfg)
    nc = _NC_CACHE[key]
    w_in = np.asarray(w_in, np.float32)
    common = {
        "w_out": np.ascontiguousarray(np.asarray(w_out, np.float32)[:, perm, :]),
        "w_gate": np.ascontiguousarray(w_gate, np.float32), "w_up": np.ascontiguousarray(w_up, np.float32),
        "w_down": np.ascontiguousarray(w_down, np.float32),
        "gb_pre": bcast128(np.asarray(ln_mix_pre)), "gb_post": bcast128(np.asarray(ln_mix_post)),
        "gb_fpre": bcast128(np.asarray(ln_ffn_pre)), "gb_fpost": bcast128(np.asarray(ln_ffn_post)),
        "gb_mo": bcast128(np.asarray(mix_out_norm)[:, perm]),
    }
    common.update(make_consts(S))
    per_rank = []
    for r in range(NR):
        cols = np.concatenate([np.arange(sg * GWF + r * GW, sg * GWF + (r + 1) * GW) for sg in range(10)])
        ab = np.zeros((128, 2), np.float32)
        ab[:, r] = 1.0
        per_rank.append({
            "w_in": np.ascontiguousarray(w_in[:, :, cols]),
            "w_pool": np.ascontiguousarray(np.asarray(w_pool, np.float32)[:, r * PGn:(r + 1) * PGn]),
            "psb": bcast128(np.asarray(pool_scale)[:, r * GW:(r + 1) * GW]),
            "c_pcoef": pool_coef(S, POOL_WINDOWS[r * PGn:(r + 1) * PGn]),
            "c_ab": ab,
        })
    in_maps = []
    for c in range(n_cores):
        b, r = divmod(c, NR)
        m = dict(common)
        m.update(per_rank[r])
        m["x_seq"] = np.ascontiguousarray(x[b, r * TO:(r + 1) * TO])
        in_maps.append(m)
    res = run_bass_kernel_spmd(nc, in_maps, core_ids=list(range(n_cores)))
    out = np.empty((B, S, D), np.float32)
    for c in range(n_cores):
        b, r = divmod(c, NR)
        out[b, r * TO:(r + 1) * TO] = res.results[c]["out"]
    return out
```

```python
import contextlib
import numpy as np
import concourse.bass as bass
import concourse.mybir as mybir
from concourse.bass_utils import run_bass_kernel_spmd

F32 = mybir.dt.float32
BF16 = mybir.dt.bfloat16
ALU = mybir.AluOpType
AF = mybir.ActivationFunctionType
AX = mybir.AxisListType

SEM_CAP = 30000
RMS_EPS = 1e-6
MOBA_BLOCK = 256
POOL_WINDOWS = (2, 4, 8, 16)
DILATED_PATTERNS = ((128, 1), (512, 4), (2048, 16))


class Sched:
    ENG = ("pe", "act", "dve", "pool", "sp")

    def __init__(self, nc, stack, n_lanes=8):
        self.nc = nc
        self.stack = stack
        self.engs = {"pe": nc.tensor, "act": nc.scalar, "dve": nc.vector, "pool": nc.gpsimd, "sp": nc.sync}
        self.prog = {}
        self.seen = {e: {} for e in self.ENG}
        self.nsem = 0
        self.pe_sems = set()
        for e in self.ENG:
            self.prog[e] = [self._newsem("p_" + e), 0]
        self.pe_sems.add(self.prog["pe"][0].num)
        self.lanes = {}
        for e in ("sp", "pool"):
            self.lanes[e] = [[self._newsem("l_%s%d" % (e, i)), 0] for i in range(n_lanes)]
        self.lane_rr = {e: 0 for e in self.lanes}
        self.all_sems = {}
        self.last_w = {}
        self.readers = {}
        self.n_inst = {e: 0 for e in self.ENG}

    def _newsem(self, name):
        self.nsem += 1
        return self.stack.enter_context(self.nc.semaphore("%s_%d" % (name, self.nsem)))

    def _emit(self, e, fn):
        self.n_inst[e] += 1
        return fn(self.engs[e])

    def _wait(self, e, tok):
        if tok is None:
            return
        sem, val = tok
        if e == "pe" and sem.num in self.pe_sems:
            return
        if self.seen[e].get(sem.num, 0) >= val:
            return
        self.seen[e][sem.num] = val
        self._emit(e, lambda eng: eng.wait_ge(sem, val))

    def _deps(self, e, reads, writes):
        for k in reads:
            self._wait(e, self.last_w.get(k))
        for k in writes:
            self._wait(e, self.last_w.get(k))
            for t in self.readers.get(k, ()):
                self._wait(e, t)

    def _commit(self, tok, reads, writes):
        self.all_sems[tok[0].num] = tok
        for k in writes:
            self.last_w[k] = tok
            self.readers[k] = []
        for k in reads:
            self.readers.setdefault(k, []).append(tok)

    def _next_tok(self, e):
        p = self.prog[e]
        if p[1] >= SEM_CAP:
            p[0] = self._newsem("p_" + e)
            p[1] = 0
            if e == "pe":
                self.pe_sems.add(p[0].num)
        p[1] += 1
        return (p[0], p[1])

    def op(self, e, fn, reads=(), writes=()):
        self._deps(e, reads, writes)
        tok = self._next_tok(e)
        self._emit(e, lambda eng: fn(eng).then_inc(tok[0], 1))
        self._commit(tok, reads, writes)
        return tok

    def group(self, e, fns, reads=(), writes=()):
        self._deps(e, reads, writes)
        tok = self._next_tok(e)
        for fn in fns[:-1]:
            self._emit(e, fn)
        self._emit(e, lambda eng: fns[-1](eng).then_inc(tok[0], 1))
        self._commit(tok, reads, writes)
        return tok

    def dma(self, e, out, in_, reads=(), writes=()):
        lanes = self.lanes[e]
        i = self.lane_rr[e]
        self.lane_rr[e] = (i + 1) % len(lanes)
        ln = lanes[i]
        if ln[1] >= SEM_CAP // 16:
            ln[0] = self._newsem("l_" + e)
            ln[1] = 0
        if ln[1] > 0:
            self._wait(e, (ln[0], 16 * ln[1]))
        self._deps(e, reads, writes)
        ln[1] += 1
        tok = (ln[0], 16 * ln[1])
        self._emit(e, lambda eng: eng.dma_start(out=out, in_=in_).then_inc(tok[0], 16))
        self._commit(tok, reads, writes)
        return tok

    def barrier(self, engines=None):
        toks = list(self.all_sems.values())
        for e in (engines or self.ENG):
            for tok in toks:
                self._wait(e, tok)

    def finish(self):
        self.barrier(engines=("sp",))


ARENA_BYTES = 200 * 1024
CC_MAX_BYTES = 2 * 1024 * 1024


def build(cfg):
    S, D, NH, DFF, TO, L = cfg["S"], cfg["D"], cfg["NH"], cfg["DFF"], cfg["TO"], cfg["L"]
    PGn, PGD, NGW = cfg["PGn"], cfg["PGD"], cfg["NGW"]
    windows = cfg["windows"]
    groups = cfg["groups"]
    debug = cfg.get("debug", False)
    NR = cfg.get("NR", 1)
    RG = cfg.get("RG", None)
    KC = D // 128
    GW = NH * 128
    INW = 10 * GW
    MWL = 4 * GW
    MW = NR * MWL
    KC2 = MW // 128
    TB = S // 128
    FC = DFF // 128
    PC = PGD // 128
    NBM = S // MOBA_BLOCK
    NQC = S // 512
    scale = 128 ** -0.5
    TG = 512
    NTG = TO // TG

    nc = bass.Bass("TRN2", target_bir_lowering=False)

    def din(name, shape, dt=F32):
        return nc.dram_tensor(name, list(shape), dt, kind="ExternalInput").ap()

    skind = "ExternalOutput" if debug else "Internal"

    def dscr(name, shape, dt):
        return nc.dram_tensor(name, list(shape), dt, kind=skind).ap()

    x_seq = din("x_seq", [TO, D])
    c_ab = din("c_ab", [128, 2])
    w_in = din("w_in", [L, D, INW])
    w_out = din("w_out", [L, MW, D])
    w_gate = din("w_gate", [L, D, DFF])
    w_up = din("w_up", [L, D, DFF])
    w_down = din("w_down", [L, DFF, D])
    w_pool = din("w_pool", [L, PGn, PGD, PGD])
    gb_pre = din("gb_pre", [L, 128, D])
    gb_post = din("gb_post", [L, 128, D])
    gb_fpre = din("gb_fpre", [L, 128, D])
    gb_fpost = din("gb_fpost", [L, 128, D])
    gb_mo = din("gb_mo", [L, 128, MW])
    psb = din("psb", [L, 128, GW])
    c_ident = din("c_ident", [128, 128])
    c_tri = din("c_tri", [128, 128])
    c_msd = din("c_msd", [128, S])
    c_msc = din("c_msc", [128, S])
    c_mss = din("c_mss", [128, S])
    c_pcoef = din("c_pcoef", [PGn, 4, 128, S])
    c_nm = din("c_nm", [128, NBM, 8])
    out = nc.dram_tensor("out", [TO, D], F32, kind="ExternalOutput").ap()

    qkT_d = dscr("qkT_d", [6, NH, 128, S], BF16)
    v_d = dscr("v_d", [3, S, GW], BF16)
    u_d = dscr("u_d", [GW // 128, 128, S], F32)
    assert S * GW * 2 <= CC_MAX_BYTES
    y_d = dscr("y_d", [4, S, GW], BF16)
    yg_d = dscr("yg_d", [4, NR * S, GW], BF16)
    TCH = min(TO, max(128, (CC_MAX_BYTES // (128 * KC * 2)) // 128 * 128))
    NCH = TO // TCH
    hTo_d = dscr("hTo_d", [NCH, 128 * KC, TCH], BF16)
    hTg_d = dscr("hTg_d", [NCH, NR * 128 * KC, TCH], BF16)
    xres_d = dscr("xres_d", [TO, D], F32)

    with contextlib.ExitStack() as st:
        s = Sched(nc, st)

        def SB(name, shape, dt):
            return st.enter_context(nc.sbuf_tensor(name, list(shape), dt))

        ps = [st.enter_context(nc.psum_tensor("ps%d" % i, [128, 512], F32)) for i in range(8)]
        psb16 = [p[:].bitcast(BF16) for p in ps]
        rr = [0]

        def PS():
            i = rr[0]
            rr[0] = (i + 1) % 8
            return i

        evt = [0]

        def EV():
            evt[0] ^= 1
            return "act" if evt[0] else "dve"

        def copy_op(e, out_ap, in_ap, reads, writes):
            if e == "act":
                s.op("act", lambda g: g.copy(out=out_ap, in_=in_ap), reads=reads, writes=writes)
            else:
                s.op(e, lambda g: g.tensor_copy(out=out_ap, in_=in_ap), reads=reads, writes=writes)

        idb = SB("idb", [128, 128], BF16)
        trib = SB("trib", [128, 128], BF16)
        onesb = SB("onesb", [128, 128], BF16)
        epsb = SB("epsb", [128, 1], F32)
        stt = SB("stt", [128, 64], F32)
        arena = SB("arena", [128, ARENA_BYTES // 2], BF16)
        s.dma("pool", idb[:], c_ident, writes=["idb"])
        s.dma("pool", trib[:], c_tri, writes=["trib"])
        s.op("dve", lambda g: g.memset(onesb[:], 1.0), writes=["onesb"])
        s.op("dve", lambda g: g.memset(epsb[:], RMS_EPS), writes=["epsb"])

        class Arena:
            def __init__(self):
                self.off = 0

            def at(self, off, shape, dt):
                n = int(np.prod(shape))
                nb = n * (2 if dt == BF16 else 4)
                assert off % 4 == 0 and off + nb <= ARENA_BYTES, (off, nb)
                if dt == BF16:
                    ap = arena[:, off // 2: off // 2 + n]
                else:
                    ap = arena[:, off // 2: off // 2 + 2 * n].bitcast(F32)
                if len(shape) == 2:
                    ap = ap.rearrange("p (a b) -> p a b", a=shape[0])
                return ap, off + ((nb + 31) // 32) * 32

            def reset(self, off=0):
                self.off = off

            def alloc(self, shape, dt):
                ap, self.off = self.at(self.off, shape, dt)
                return ap

        A = Arena()
        ccsem = [s._newsem("cc"), 0]
        abt = SB("abt", [128, 2], F32)
        s.dma("sp", abt[:], c_ab, writes=["abt"])

        def cc_gather(src, dst, reads, writes):
            s._deps("pool", reads, writes)
            ccsem[1] += 1
            tok = (ccsem[0], ccsem[1])
            s._emit("pool", lambda eng: eng.collective_compute(
                "AllGather", ALU.bypass, replica_groups=RG, ins=[src.opt()], outs=[dst.opt()]).then_inc(ccsem[0]))
            s._commit(tok, reads, writes)

        def rstd(src_ap, junk_ap, col, denom, rkeys, jkeys):
            s.op("act", lambda g: g.activation(out=junk_ap, in_=src_ap, func=AF.Square, accum_out=stt[:, col:col + 1]),
                 reads=rkeys, writes=list(jkeys) + [("stt", col)])
            s.op("act", lambda g: g.activation(out=stt[:, col + 1:col + 2], in_=stt[:, col:col + 1], func=AF.Sqrt,
                                               scale=1.0 / denom, bias=epsb[:]),
                 reads=[("stt", col), "epsb"], writes=[("stt", col + 1)])
            s.op("dve", lambda g: g.reciprocal(out=stt[:, col + 2:col + 3], in_=stt[:, col + 1:col + 2]),
                 reads=[("stt", col + 1)], writes=[("stt", col + 2)])
            return stt[:, col + 2:col + 3], ("stt", col + 2)

        def transposeT(src, skeys, nchunk, dst, dkey, t_off):
            for g0 in range(0, nchunk, 8):
                n = min(8, nchunk - g0)
                b = PS()
                fns = [(lambda e, j=j: e.transpose(out=psb16[b][:, j * 128:(j + 1) * 128],
                                                   in_=src[:, (g0 + j) * 128:(g0 + j + 1) * 128], identity=idb[:]))
                       for j in range(n)]
                s.group("pe", fns, reads=list(skeys) + ["idb"], writes=[("ps", b)])
                copy_op(EV(), dst[:, g0:g0 + n, t_off:t_off + 128],
                        psb16[b][:, 0:n * 128].rearrange("p (k t) -> p k t", k=n), [("ps", b)], [dkey])

        def mm_group(b, ncols, pairs, reads):
            n = len(pairs)
            fns = [(lambda e, i=i: e.matmul(ps[b][:, 0:ncols], lhsT=pairs[i][0], rhs=pairs[i][1],
                                            start=(i == 0), stop=(i == n - 1))) for i in range(n)]
            s.group("pe", fns, reads=reads, writes=[("ps", b)])

        for l in range(L):
            xsrc = x_seq if l == 0 else xres_d
            A.reset()
            gb = A.alloc([D], F32)
            xts = [A.alloc([D], F32) for _ in range(2)]
            xbs = [A.alloc([D], BF16) for _ in range(2)]
            p1keys = ["gb", ("xt", 0), ("xt", 1), ("xb", 0), ("xb", 1)]
            assert A.off <= KC * S * 2
            hTo, _ = A.at(KC * S * 2, [KC, TO], BF16)
            s.dma("sp", gb, gb_pre[l], writes=["gb"])
            tpc = TCH // 128
            for tb in range(TO // 128):
                par = tb % 2
                xt, xb = xts[par], xbs[par]
                s.dma("sp", xt, xsrc[tb * 128:(tb + 1) * 128, :], writes=[("xt", par)])
                r_ap, rk = rstd(xt, xb, 32 * par, D, [("xt", par)], [("xb", par)])
                s.op("dve", lambda g: g.scalar_tensor_tensor(out=xb, in0=xt, scalar=r_ap, in1=gb,
                                                             op0=ALU.mult, op1=ALU.mult),
                     reads=[("xt", par), rk, "gb"], writes=[("xb", par)])
                transposeT(xb, [("xb", par)], KC, hTo, ("hTo", tb), tb * 128)
                if (tb + 1) % tpc == 0:
                    c = tb // tpc
                    s.dma("sp", hTo_d[c].rearrange("(p k) t -> p k t", k=KC), hTo[:, :, c * TCH:(c + 1) * TCH],
                          reads=[("hTo", t_) for t_ in range(c * tpc, (c + 1) * tpc)], writes=[("hTod", c)])
                    cc_gather(hTo_d[c], hTg_d[c], [("hTod", c)], [("hTgd", c)])
            hT, p_mark = A.at(0, [KC, S], BF16)
            hTo_keys = [("hTo", t_) for t_ in range(TO // 128)]
            for c in range(NCH):
                for r in range(NR):
                    t0 = r * TO + c * TCH
                    s.dma("sp", hT[:, :, t0:t0 + TCH],
                          hTg_d[c, r * 128 * KC:(r + 1) * 128 * KC, :].rearrange("(p k) t -> p k t", k=KC),
                          reads=[("hTgd", c)], writes=[("hT", tb_) for tb_ in range(t0 // 128, (t0 + TCH) // 128)] + p1keys)
            A.reset(p_mark)
            CT = 256
            wbufs = [A.alloc([KC, CT], BF16) for _ in range(3)]
            stg = [A.alloc([512], BF16) for _ in range(2)]
            stgf = [A.alloc([512], F32) for _ in range(2)]
            si = 0
            for ct in range(INW // CT):
                wb = wbufs[ct % 3]
                wk = ("w2", ct % 3)
                s.dma("pool", wb, w_in[l, :, ct * CT:(ct + 1) * CT].rearrange("(k p) n -> p k n", p=128),
                      writes=[wk] + (hTo_keys if ct < 3 else []))
                seg = (ct * CT) // GW
                off = ct * CT - seg * GW
                if seg in (2, 5, 8):
                    m = {2: 0, 5: 1, 8: 2}[seg]
                    for tb in range(TB):
                        b = PS()
                        mm_group(b, CT, [(hT[:, k, tb * 128:(tb + 1) * 128], wb[:, k, :]) for k in range(KC)],
                                 [("hT", tb), wk])
                        si ^= 1
                        copy_op(EV(), stg[si][:, 0:CT], ps[b][:, 0:CT], [("ps", b)], [("stg", si)])
                        s.dma("sp", v_d[m, tb * 128:(tb + 1) * 128, off:off + CT], stg[si][:, 0:CT], reads=[("stg", si)])
                else:
                    for sub in range(CT // 128):
                        h = (off + sub * 128) // 128
                        for tc in range(NQC):
                            b = PS()
                            mm_group(b, 512, [(wb[:, k, sub * 128:(sub + 1) * 128], hT[:, k, tc * 512:(tc + 1) * 512])
                                              for k in range(KC)],
                                     [("hT", tc * 4 + i) for i in range(4)] + [wk])
                            si ^= 1
                            if seg == 9:
                                copy_op(EV(), stgf[si], ps[b][:], [("ps", b)], [("stgf", si)])
                                s.dma("sp", u_d[h, :, tc * 512:(tc + 1) * 512], stgf[si], reads=[("stgf", si)])
                            else:
                                idx = {0: 0, 1: 1, 3: 2, 4: 3, 6: 4, 7: 5}[seg]
                                copy_op(EV(), stg[si], ps[b][:], [("ps", b)], [("stg", si)])
                                s.dma("sp", qkT_d[idx, h, :, tc * 512:(tc + 1) * 512], stg[si], reads=[("stg", si)])
            s.barrier()

            A.reset()
            ystage = A.alloc([TB, GW], BF16)
            msd = A.alloc([S], BF16)
            msc = A.alloc([S], BF16)
            mss = A.alloc([S], BF16)
            s.dma("pool", msd, c_msd, writes=["msd"])
            s.dma("pool", msc, c_msc, writes=["msc"])
            s.dma("pool", mss, c_mss, writes=["mss"])
            qT = [A.alloc([S], BF16) for _ in range(2)]
            kT = [A.alloc([S], BF16) for _ in range(2)]
            vx = [A.alloc([TB, 130], BF16) for _ in range(2)]
            for i in range(2):
                s.op("dve", lambda g, i=i: g.memset(vx[i][:, :, 128:129], 1.0), writes=[("vx1", i)])
            Ealls = [A.alloc([TB, 512], BF16) for _ in range(2)]
            E2s = [A.alloc([2, 512], BF16) for _ in range(2)]
            acc = A.alloc([4, 129], F32)
            nm = A.alloc([NBM, 8], F32)
            s.dma("sp", nm, c_nm, writes=["nm"])
            km = A.alloc([NBM], F32)
            kmh = A.alloc([NBM], BF16)
            kml = A.alloc([NBM], BF16)
            g8 = A.alloc([8], F32)
            top8 = A.alloc([8], F32)
            sels = [A.alloc([TB, 8], F32) for _ in range(2)]
            s.op("dve", lambda g: g.memset(g8, -1e30), writes=["g8"])
            t_e = A.alloc([512], F32)
            t_1 = A.alloc([512], F32)
            t_3 = A.alloc([512], F32)
            p3_mark = A.off
            hcount = [0]

            def load_head(m, h):
                i = hcount[0] % 2
                hcount[0] += 1
                s.dma("sp", qT[i], qkT_d[2 * m, h], writes=[("qT", i)])
                s.dma("sp", kT[i], qkT_d[2 * m + 1, h], writes=[("kT", i)])
                s.dma("sp", vx[i][:, :, 0:128],
                      v_d[m, :, h * 128:(h + 1) * 128].rearrange("(tb p) d -> p tb d", p=128), writes=[("vx", i)])
                return i

            def finalize(src_ap, den_ap, rkeys, qb, h):
                s.op("dve", lambda g: g.reciprocal(out=stt[:, 8:9], in_=den_ap), reads=rkeys, writes=[("stt", 8)])
                s.op("dve", lambda g: g.tensor_scalar(out=ystage[:, qb, h * 128:(h + 1) * 128], in0=src_ap,
                                                      scalar1=stt[:, 8:9], scalar2=None, op0=ALU.mult),
                     reads=list(rkeys) + [("stt", 8)], writes=[("ys", qb)])

            def store_mixer(mi):
                s.dma("sp", y_d[mi].rearrange("(tb p) c -> p tb c", p=128), ystage,
                      reads=[("ys", qb) for qb in range(TB)], writes=[("yd", mi)])
                cc_gather(y_d[mi], yg_d[mi], [("yd", mi)], [("ygd", mi)])

            mtog = [0]

            def mask_eng():
                mtog[0] ^= 1
                return "pool" if mtog[0] else "dve"

            A.reset(p3_mark)
            up = A.alloc([16 + S], F32)
            pa = A.alloc([16 + S], F32)
            pb = A.alloc([16 + S], F32)
            coef = [A.alloc([S], F32) for _ in range(4)]
            tmpf = A.alloc([S], F32)
            tmp2 = A.alloc([S], F32)
            dT = A.alloc([PC, S], BF16)
            wp = A.alloc([PC, PGD], BF16)
            pst = A.alloc([GW], F32)
            ysC = A.alloc([TB, GW], BF16)
            s.dma("sp", pst, psb[l], writes=["pst"])
            for t_, k_ in ((up, "up"), (pa, "pa"), (pb, "pb")):
                s.op("dve", lambda g, t_=t_: g.memset(t_[:, 0:16], 0.0), writes=[k_ + "z"])

            def c_chunk(gi, c):
                if c == 0:
                    s.dma("pool", wp, w_pool[l, gi].rearrange("(c p) d -> p c d", p=128), writes=["wp"])
                    for wi in range(4):
                        s.dma("sp", coef[wi], c_pcoef[gi, wi], writes=[("coef", wi)])
                ch = gi * PC + c
                s.dma("sp", up[:, 16:16 + S], u_d[ch], writes=["up"])
                cur, ck = up, "up"
                for wi in range(4):
                    sh = 1 << wi
                    nxt, nk = (pa, "pa") if cur is not pa else (pb, "pb")
                    s.op("dve", lambda g: g.tensor_tensor(
                        out=nxt[:, 16:16 + S], in0=cur[:, 16:16 + S], in1=cur[:, 16 - sh:16 - sh + S], op=ALU.add),
                         reads=[ck, ck + "z"], writes=[nk])
                    cur, ck = nxt, nk
                    if wi == 0:
                        s.op("pool", lambda g: g.tensor_tensor(out=tmpf, in0=cur[:, 16:16 + S], in1=coef[0],
                                                               op=ALU.mult), reads=[ck, ("coef", 0)], writes=["tmpf"])
                    else:
                        s.op("pool", lambda g: g.tensor_tensor(out=tmp2, in0=cur[:, 16:16 + S], in1=coef[wi],
                                                               op=ALU.mult), reads=[ck, ("coef", wi)], writes=["tmp2"])
                        s.op("pool", lambda g: g.tensor_tensor(out=tmpf, in0=tmpf, in1=tmp2, op=ALU.add),
                             reads=["tmpf", "tmp2"], writes=["tmpf"])
                s.op("dve", lambda g: g.tensor_tensor(out=dT[:, c, :], in0=tmpf, in1=up[:, 16:16 + S],
                                                      op=ALU.subtract), reads=["tmpf", "up"], writes=[("dT", c)])

            def c_mm(gi, half):
                for tb in range(half * TB // 2, (half + 1) * TB // 2):
                    b = PS()
                    mm_group(b, PGD, [(dT[:, c, tb * 128:(tb + 1) * 128], wp[:, c, :]) for c in range(PC)],
                             [("dT", c) for c in range(PC)] + ["wp"])
                    s.op("dve", lambda g: g.tensor_tensor(out=ysC[:, tb, gi * PGD:(gi + 1) * PGD], in0=ps[b][:, 0:PGD],
                                                          in1=pst[:, gi * PGD:(gi + 1) * PGD], op=ALU.mult),
                         reads=[("ps", b), "pst"], writes=[("ysC", tb)])

            c_items = []
            for gi in range(PGn):
                for c in range(PC):
                    c_items.append(lambda gi=gi, c=c: c_chunk(gi, c))
                for half in range(2):
                    c_items.append(lambda gi=gi, half=half: c_mm(gi, half))

            def moba_gates(i):
                s.op("dve", lambda g: g.tensor_reduce(out=km, in_=kT[i].rearrange("p (n k) -> p n k", k=MOBA_BLOCK),
                                                      axis=AX.X, op=ALU.add), reads=[("kT", i)], writes=["km"])
                s.op("dve", lambda g: g.tensor_scalar(out=kmh, in0=km, scalar1=1.0 / MOBA_BLOCK, scalar2=None,
                                                      op0=ALU.mult), reads=["km"], writes=["kmh"])
                s.op("dve", lambda g: g.scalar_tensor_tensor(out=kml, in0=km, scalar=1.0 / MOBA_BLOCK, in1=kmh,
                                                             op0=ALU.mult, op1=ALU.subtract),
                     reads=["km", "kmh"], writes=["kml"])
                for tb in range(TB):
                    b = PS()
                    mm_group(b, NBM, [(qT[i][:, tb * 128:(tb + 1) * 128], kmh),
                                      (qT[i][:, tb * 128:(tb + 1) * 128], kml)], [("qT", i), "kmh", "kml"])
                    blk = tb // 2
                    s.op("dve", lambda g: g.tensor_tensor(out=g8[:, 0:NBM], in0=ps[b][:, 0:NBM], in1=nm[:, blk, 0:NBM],
                                                          op=ALU.add), reads=[("ps", b), "nm"], writes=["g8"])
                    s.op("dve", lambda g: g.max(out=top8, in_=g8), reads=["g8"], writes=["top8"])
                    s.op("dve", lambda g: g.tensor_scalar(out=sels[i][:, tb, :], in0=g8, scalar1=top8[:, 2:3], scalar2=None,
                                                          op0=ALU.is_ge), reads=["g8", "top8"], writes=[("sel", i, tb)])

            heads_i = {}

            def a_s1(u, h, qc, n):
                if (qc, n) == (0, 0):
                    heads_i[h] = load_head(0, h)
                    moba_gates(heads_i[h])
                i = heads_i[h]
                hk = [("qT", i), ("kT", i), ("vx", i), ("vx1", i)]
                E2, ek = E2s[u % 2], ("E2", u % 2)
                q0 = max(qc * 512, n * 256)
                W = (qc + 1) * 512 - q0
                c0 = q0 - qc * 512
                own = (n >= 2 * qc)
                for j in range(2):
                    kb = 2 * n + j
                    b = PS()
                    mm_group(b, W, [(kT[i][:, kb * 128:(kb + 1) * 128], qT[i][:, q0:q0 + W])], hk)
                    s.op("act", lambda g: g.activation(out=E2[:, j, c0:512], in_=ps[b][:, 0:W], func=AF.Exp,
                                                       scale=scale), reads=[("ps", b)], writes=[ek + (j,)])
                    if own and j == 0:
                        s.op(mask_eng(), lambda g: g.tensor_tensor(out=E2[:, 0, c0:c0 + 256], in0=E2[:, 0, c0:c0 + 256],
                                                                   in1=msc[:, 0:256], op=ALU.mult),
                             reads=[ek + (0,), "msc"], writes=[ek + (0,)])
                    elif own:
                        s.op("pool", lambda g: g.memset(E2[:, 1, c0:c0 + 128], 0.0), reads=[ek + (1,)], writes=[ek + (1,)])
                        s.op(mask_eng(), lambda g: g.tensor_tensor(out=E2[:, 1, c0 + 128:c0 + 256],
                                                                   in0=E2[:, 1, c0 + 128:c0 + 256],
                                                                   in1=msc[:, 0:128], op=ALU.mult),
                             reads=[ek + (1,), "msc"], writes=[ek + (1,)])

            def a_s2(u, h, qc, n):
                i = heads_i[h]
                hk = [("qT", i), ("kT", i), ("vx", i), ("vx1", i)]
                E2, ek = E2s[u % 2], ("E2", u % 2)
                q0 = max(qc * 512, n * 256)
                c0 = q0 - qc * 512
                sel = sels[i]
                for sub in range(c0 // 128, 4):
                    qb = qc * 4 + sub
                    b = PS()
                    mm_group(b, 129, [(E2[:, 0, sub * 128:(sub + 1) * 128], vx[i][:, 2 * n, 0:129]),
                                      (E2[:, 1, sub * 128:(sub + 1) * 128], vx[i][:, 2 * n + 1, 0:129])],
                             [ek + (0,), ek + (1,)] + hk)
                    ak = ("acc", sub)
                    if n == 0:
                        if qb // 2 == 0:
                            s.op("dve", lambda g: g.tensor_copy(out=acc[:, sub, :], in_=ps[b][:, 0:129]),
                                 reads=[("ps", b)], writes=[ak])
                        else:
                            s.op("dve", lambda g: g.tensor_scalar(out=acc[:, sub, :], in0=ps[b][:, 0:129],
                                                                  scalar1=sel[:, qb, 0:1], scalar2=None, op0=ALU.mult),
                                 reads=[("ps", b), ("sel", i, qb)], writes=[ak])
                    elif n == qb // 2:
                        s.op("dve", lambda g: g.tensor_tensor(out=acc[:, sub, :], in0=ps[b][:, 0:129],
                                                              in1=acc[:, sub, :], op=ALU.add),
                             reads=[("ps", b), ak], writes=[ak])
                    else:
                        s.op("dve", lambda g: g.scalar_tensor_tensor(out=acc[:, sub, :], in0=ps[b][:, 0:129],
                                                                     scalar=sel[:, qb, n:n + 1], in1=acc[:, sub, :],
                                                                     op0=ALU.mult, op1=ALU.add),
                             reads=[("ps", b), ak, ("sel", i, qb)], writes=[ak])
                if n == 2 * qc + 1:
                    for sub in range(4):
                        finalize(acc[:, sub, 0:128], acc[:, sub, 128:129], [("acc", sub)], qc * 4 + sub, h)

            unitsA = [(h, qc, n) for h in range(NH) for qc in range(NQC) for n in range(2 * qc + 2)]
            c_every = max(1, len(unitsA) // (len(c_items) + 1))
            for u, un in enumerate(unitsA):
                a_s1(u, *un)
                if u > 0:
                    a_s2(u - 1, *unitsA[u - 1])
                if c_items and (u + 1) % c_every == 0:
                    c_items.pop(0)()
            a_s2(len(unitsA) - 1, *unitsA[-1])
            store_mixer(0)
            while c_items:
                c_items.pop(0)()
            s.dma("sp", y_d[2].rearrange("(tb p) c -> p tb c", p=128), ysC,
                  reads=[("ysC", tb) for tb in range(TB)], writes=[("yd", 2)])
            cc_gather(y_d[2], yg_d[2], [("yd", 2)], [("ygd", 2)])
            s.barrier()

            A.reset(p3_mark)
            spf = A.alloc([TB, 512], F32)
            spmA = A.alloc([TB, 512], BF16)
            t_e2 = [t_e, A.alloc([512], F32)]
            t_12 = [t_1, A.alloc([512], F32)]
            t_32 = [t_3, A.alloc([512], F32)]
            bcnt = [0, 0]

            def b_geo(qc, kb):
                q0 = max(qc * 512, kb * 128)
                return q0, (qc + 1) * 512 - q0, q0 - qc * 512, kb * 128 >= qc * 512

            def b_s1(i, qc, kb):
                hk = [("qT", i), ("kT", i), ("vx", i)]
                q0, W, c0, diag = b_geo(qc, kb)
                bcnt[0] ^= 1
                te, tek = t_e2[bcnt[0]], ("t_e", bcnt[0])
                bz = PS()
                mm_group(bz, W, [(kT[i][:, kb * 128:(kb + 1) * 128], qT[i][:, q0:q0 + W])], hk)
                s.op("act", lambda g: g.activation(out=te[:, 0:W], in_=ps[bz][:, 0:W], func=AF.Exp, scale=scale),
                     reads=[("ps", bz)], writes=[tek])
                s.op("act", lambda g: g.activation(out=spf[:, kb, c0:512], in_=te[:, 0:W], func=AF.Ln, bias=1.0),
                     reads=[tek], writes=[("spf", kb)])
                if diag:
                    if c0 > 0:
                        s.op("pool", lambda g: g.memset(spmA[:, kb, 0:c0], 0.0), writes=[("spm0", kb)])
                    s.op("dve", lambda g: g.tensor_tensor(out=spmA[:, kb, c0:c0 + 128], in0=spf[:, kb, c0:c0 + 128],
                                                          in1=mss[:, 0:128], op=ALU.mult),
                         reads=[("spf", kb), "mss"], writes=[("spm", kb)])
                    if W > 128:
                        s.op("pool", lambda g: g.tensor_copy(out=spmA[:, kb, c0 + 128:512], in_=spf[:, kb, c0 + 128:512]),
                             reads=[("spf", kb)], writes=[("spm2", kb)])
                else:
                    s.op("pool", lambda g: g.tensor_copy(out=spmA[:, kb, :], in_=spf[:, kb, :]),
                         reads=[("spf", kb)], writes=[("spm", kb), ("spm2", kb), ("spm0", kb)])

            def b_s2(i, qc, kb, nkb, Eall, ek):
                hk = [("qT", i), ("kT", i), ("vx", i)]
                q0, W, c0, diag = b_geo(qc, kb)
                bcnt[1] ^= 1
                t1, t1k = t_12[bcnt[1]], ("t_1", bcnt[1])
                t3, t3k = t_32[bcnt[1]], ("t_3", bcnt[1])
                bc = PS()
                mm_group(bc, W, [(trib[:], spmA[:, kb, c0:512])] +
                         [(onesb[:], spmA[:, k2, c0:512]) for k2 in range(kb + 1, nkb)],
                         ["trib", "onesb"] + [(nm_, k2) for k2 in range(kb, nkb) for nm_ in ("spm", "spm2", "spm0")])
                bz = PS()
                mm_group(bz, W, [(kT[i][:, kb * 128:(kb + 1) * 128], qT[i][:, q0:q0 + W])], hk)
                s.op("dve", lambda g: g.tensor_tensor(out=t1[:, 0:W], in0=ps[bc][:, 0:W], in1=spf[:, kb, c0:512],
                                                      op=ALU.add), reads=[("ps", bc), ("spf", kb)], writes=[t1k])
                s.op("dve", lambda g: g.scalar_tensor_tensor(out=t3[:, 0:W], in0=ps[bz][:, 0:W], scalar=scale,
                                                             in1=t1[:, 0:W], op0=ALU.mult, op1=ALU.subtract),
                     reads=[("ps", bz), t1k], writes=[t3k])
                s.op("act", lambda g: g.activation(out=Eall[:, kb, c0:512], in_=t3[:, 0:W], func=AF.Exp),
                     reads=[t3k], writes=[ek + (kb,)])
                if diag:
                    s.op("pool", lambda g: g.tensor_tensor(out=Eall[:, kb, c0:c0 + 128], in0=Eall[:, kb, c0:c0 + 128],
                                                           in1=mss[:, 0:128], op=ALU.mult),
                         reads=[ek + (kb,), "mss"], writes=[ek + (kb,)])

            def b_pv(i, h, qc, Eall, ek):
                hk = [("qT", i), ("kT", i), ("vx", i)]
                for sub in range(4):
                    qb = qc * 4 + sub
                    b = PS()
                    mm_group(b, 128, [(Eall[:, kb, sub * 128:(sub + 1) * 128], vx[i][:, kb, 0:128]) for kb in range(qb + 1)],
                             [ek + (kb,) for kb in range(qb + 1)] + hk)
                    copy_op(EV(), ystage[:, qb, h * 128:(h + 1) * 128], ps[b][:, 0:128], [("ps", b)], [("ys", qb)])

            pend = None
            ucnt = 0
            for h in range(NH):
                i = load_head(1, h)
                for qc in range(NQC):
                    nkb = (qc + 1) * 4
                    Eall, ek = Ealls[ucnt % 2], ("E", ucnt % 2)
                    ucnt += 1
                    kbs = list(reversed(range(nkb)))
                    LA = 2
                    for kb in kbs[:LA]:
                        b_s1(i, qc, kb)
                    if pend is not None:
                        b_pv(*pend)
                        pend = None
                    for idx, kb in enumerate(kbs):
                        if idx + LA < len(kbs):
                            b_s1(i, qc, kbs[idx + LA])
                        b_s2(i, qc, kb, nkb, Eall, ek)
                    pend = (i, h, qc, Eall, ek)
            b_pv(*pend)
            store_mixer(1)

            def d_s1(u, h, qc):
                if qc == 0:
                    heads_i[("d", h)] = load_head(2, h)
                i = heads_i[("d", h)]
                hk = [("qT", i), ("kT", i), ("vx", i), ("vx1", i)]
                Eall, ek = Ealls[u % 2], ("E", u % 2)
                for kb in range((qc + 1) * 4):
                    q0 = max(qc * 512, kb * 128)
                    W = (qc + 1) * 512 - q0
                    c0 = q0 - qc * 512
                    b = PS()
                    mm_group(b, W, [(kT[i][:, kb * 128:(kb + 1) * 128], qT[i][:, q0:q0 + W])], hk)
                    s.op("act", lambda g: g.activation(out=Eall[:, kb, c0:512], in_=ps[b][:, 0:W], func=AF.Exp, scale=scale),
                         reads=[("ps", b)], writes=[ek + (kb,)])
                    mo = q0 - 128 * kb
                    s.op(mask_eng(), lambda g: g.tensor_tensor(out=Eall[:, kb, c0:512], in0=Eall[:, kb, c0:512],
                                                               in1=msd[:, mo:mo + W], op=ALU.mult),
                         reads=[ek + (kb,), "msd"], writes=[ek + (kb,)])

            def d_s2(u, h, qc):
                i = heads_i[("d", h)]
                hk = [("qT", i), ("kT", i), ("vx", i), ("vx1", i)]
                Eall, ek = Ealls[u % 2], ("E", u % 2)
                for sub in range(4):
                    qb = qc * 4 + sub
                    b = PS()
                    mm_group(b, 129, [(Eall[:, kb, sub * 128:(sub + 1) * 128], vx[i][:, kb, 0:129]) for kb in range(qb + 1)],
                             [ek + (kb,) for kb in range(qb + 1)] + hk)
                    finalize(ps[b][:, 0:128], ps[b][:, 128:129], [("ps", b)], qb, h)

            unitsD = [(h, qc) for h in range(NH) for qc in range(NQC)]
            for u, un in enumerate(unitsD):
                d_s1(u, *un)
                if u > 0:
                    d_s2(u - 1, *unitsD[u - 1])
            d_s2(len(unitsD) - 1, *unitsD[-1])
            store_mixer(3)

            s.barrier()

            dst = out if l == L - 1 else xres_d
            LOW = 0
            MID1 = 32 * 1024
            MID2 = 64 * 1024
            MSB_OFF = ARENA_BYTES - 4 * D * 4
            ACT_OFF = ARENA_BYTES - ((FC * TG * 2 + 31) // 32) * 32
            assert KC2 * TG * 2 <= MID1 and KC * TG * 2 <= MID1 and 4 * D * 4 <= MID2
            for tg in range(NTG):
                yT, _ = A.at(LOW, [KC2, TG], BF16)
                A.reset(MID1)
                gbm = A.alloc([MW], F32)
                yts = [A.alloc([MW], BF16) for _ in range(2)]
                yt1s = [A.alloc([MW], BF16) for _ in range(2)]
                ybs = [A.alloc([MW], BF16) for _ in range(2)]
                assert A.off <= MSB_OFF
                s.dma("sp", gbm, gb_mo[l], writes=["gbm"])
                ygk = [("ygd", m) for m in range(4)]
                for tb in range(4):
                    T0 = tg * TG + tb * 128
                    par = tb % 2
                    so = 32 * par
                    yt, yt1, yb = yts[par], yt1s[par], ybs[par]
                    ytk = [("ytp", par, 0, r) for r in range(NR)]
                    for r in range(NR):
                        s.dma("sp", yt[:, r * MWL:(r + 1) * MWL].rearrange("p (m c) -> p m c", m=4),
                              yg_d[:, r * S + T0:r * S + T0 + 128, :].rearrange("m p c -> p m c"),
                              reads=ygk, writes=[("ytp", par, 0, r)])
                        if NR > 1:
                            s.dma("sp", yt1[:, r * MWL:(r + 1) * MWL].rearrange("p (m c) -> p m c", m=4),
                                  yg_d[:, r * S + TO + T0:r * S + TO + T0 + 128, :].rearrange("m p c -> p m c"),
                                  reads=ygk, writes=[("ytp", par, 1, r)])
                    if NR > 1:
                        s.op("pool", lambda g: g.tensor_scalar(out=yt, in0=yt, scalar1=abt[:, 0:1], scalar2=None, op0=ALU.mult),
                             reads=ytk + ["abt"], writes=ytk)
                        s.op("dve", lambda g: g.scalar_tensor_tensor(out=yt, in0=yt1, scalar=abt[:, 1:2], in1=yt,
                                                                     op0=ALU.mult, op1=ALU.add),
                             reads=[("ytp", par, 1, r) for r in range(NR)] + ["abt"] + ytk, writes=ytk)
                    for gi, ranges in enumerate(groups):
                        for ri, (c0, cl) in enumerate(ranges):
                            col = so + 12 + ri
                            s.op("act", lambda g: g.activation(out=yb[:, c0:c0 + cl], in_=yt[:, c0:c0 + cl],
                                                               func=AF.Square, accum_out=stt[:, col:col + 1]),
                                 reads=ytk, writes=[("yb", par, gi), ("stt", col)])
                        if len(ranges) == 2:
                            s.op("dve", lambda g: g.tensor_tensor(out=stt[:, so + 12:so + 13], in0=stt[:, so + 12:so + 13],
                                                                  in1=stt[:, so + 13:so + 14], op=ALU.add),
                                 reads=[("stt", so + 12), ("stt", so + 13)], writes=[("stt", so + 12)])
                        s.op("act", lambda g: g.activation(out=stt[:, so + 14:so + 15], in_=stt[:, so + 12:so + 13], func=AF.Sqrt,
                                                           scale=1.0 / NGW, bias=epsb[:]),
                             reads=[("stt", so + 12), "epsb"], writes=[("stt", so + 14)])
                        s.op("dve", lambda g: g.reciprocal(out=stt[:, so + 16 + gi:so + 17 + gi], in_=stt[:, so + 14:so + 15]),
                             reads=[("stt", so + 14)], writes=[("stt", so + 16 + gi)])
                        for (c0, cl) in ranges:
                            s.op("dve", lambda g: g.scalar_tensor_tensor(out=yb[:, c0:c0 + cl], in0=yt[:, c0:c0 + cl],
                                                                         scalar=stt[:, so + 16 + gi:so + 17 + gi],
                                                                         in1=gbm[:, c0:c0 + cl], op0=ALU.mult, op1=ALU.mult),
                                 reads=ytk + [("stt", so + 16 + gi), "gbm"], writes=[("yb", par, gi)])
                    transposeT(yb, [("yb", par, gi) for gi in range(len(groups))], KC2, yT, ("yT", tb), tb * 128)
                s.barrier()
                msb, _ = A.at(MSB_OFF, [4, D], F32)
                A.reset(MID1)
                wobuf = [A.alloc([KC2, 512], BF16) for _ in range(2)]
                assert A.off <= MSB_OFF
                for nt in range(D // 512):
                    wb = wobuf[nt % 2]
                    wk = ("wo", nt % 2)
                    s.dma("pool", wb, w_out[l, :, nt * 512:(nt + 1) * 512].rearrange("(k p) n -> p k n", p=128), writes=[wk])
                    for tb in range(4):
                        b = PS()
                        mm_group(b, 512, [(yT[:, k, tb * 128:(tb + 1) * 128], wb[:, k, :]) for k in range(KC2)],
                                 [("yT", tb), wk])
                        copy_op(EV(), msb[:, tb, nt * 512:(nt + 1) * 512], ps[b][:], [("ps", b)], [("msb", tb)])
                s.barrier()
                h2T, _ = A.at(LOW, [KC, TG], BF16)
                A.reset(MID1)
                gb1 = A.alloc([D], F32)
                gb2 = A.alloc([D], F32)
                xts = [A.alloc([D], F32) for _ in range(2)]
                xbs = [A.alloc([D], BF16) for _ in range(2)]
                assert A.off <= MSB_OFF
                s.dma("sp", gb1, gb_post[l], writes=["gb1"])
                s.dma("sp", gb2, gb_fpre[l], writes=["gb2"])
                for tb in range(4):
                    T0 = tg * TG + tb * 128
                    par = tb % 2
                    xt, xb = xts[par], xbs[par]
                    xk, bk = ("xt", par), ("xb", par)
                    s.dma("sp", xt, xsrc[T0:T0 + 128, :], writes=[xk])
                    r_ap, rk = rstd(msb[:, tb, :], xb, 32 * par, D, [("msb", tb)], [bk])
                    s.op("dve", lambda g: g.scalar_tensor_tensor(out=msb[:, tb, :], in0=msb[:, tb, :], scalar=r_ap,
                                                                 in1=gb1, op0=ALU.mult, op1=ALU.mult),
                         reads=[("msb", tb), rk, "gb1"], writes=[("msb", tb)])
                    s.op("pool", lambda g: g.tensor_tensor(out=xt, in0=xt, in1=msb[:, tb, :], op=ALU.add),
                         reads=[xk, ("msb", tb)], writes=[xk])
                    s.dma("sp", xres_d[T0:T0 + 128, :], xt, reads=[xk])
                    r_ap, rk = rstd(xt, xb, 32 * par + 4, D, [xk], [bk])
                    s.op("dve", lambda g: g.scalar_tensor_tensor(out=xb, in0=xt, scalar=r_ap, in1=gb2,
                                                                 op0=ALU.mult, op1=ALU.mult),
                         reads=[xk, rk, "gb2"], writes=[bk])
                    transposeT(xb, [bk], KC, h2T, ("h2T", tb), tb * 128)
                s.barrier()
                actT, _ = A.at(ACT_OFF, [FC, TG], BF16)
                A.reset(MID1)
                CT2 = 256
                wgu = [A.alloc([KC, CT2], BF16) for _ in range(3)]
                sg = A.alloc([2, TG], F32)
                assert A.off <= ACT_OFF
                wi = 0
                h2keys = [("h2T", tb) for tb in range(4)]
                for j in range(DFF // CT2):
                    wg, wgk = wgu[wi % 3], ("wgu", wi % 3)
                    wi += 1
                    s.dma("pool", wg, w_gate[l, :, j * CT2:(j + 1) * CT2].rearrange("(k p) n -> p k n", p=128), writes=[wgk])
                    wu, wuk = wgu[wi % 3], ("wgu", wi % 3)
                    wi += 1
                    s.dma("pool", wu, w_up[l, :, j * CT2:(j + 1) * CT2].rearrange("(k p) n -> p k n", p=128), writes=[wuk])
                    for c in range(2):
                        b = PS()
                        mm_group(b, TG, [(wg[:, k, c * 128:(c + 1) * 128], h2T[:, k, :]) for k in range(KC)], h2keys + [wgk])
                        s.op("act", lambda g: g.activation(out=sg[:, c, :], in_=ps[b][:], func=AF.Silu),
                             reads=[("ps", b)], writes=[("sg", c)])
                    for c in range(2):
                        b = PS()
                        mm_group(b, TG, [(wu[:, k, c * 128:(c + 1) * 128], h2T[:, k, :]) for k in range(KC)], h2keys + [wuk])
                        s.op("dve", lambda g: g.tensor_tensor(out=actT[:, 2 * j + c, :], in0=ps[b][:], in1=sg[:, c, :],
                                                              op=ALU.mult),
                             reads=[("ps", b), ("sg", c)], writes=[("actT", 2 * j + c)])
                s.barrier()
                fsb, _ = A.at(LOW, [4, D], F32)
                A.reset(MID2)
                FG = 8
                wdb = [A.alloc([FG, 512], BF16) for _ in range(3)]
                assert A.off <= ACT_OFF
                nfg = (FC + FG - 1) // FG
                wdi = 0
                for nt in range(D // 512):
                    banks = [PS() for _ in range(4)]
                    for fg in range(nfg):
                        f0 = fg * FG
                        nf = min(FG, FC - f0)
                        wd, wdk = wdb[wdi % 3], ("wd", wdi % 3)
                        wdi += 1
                        s.dma("pool", wd[:, 0:nf, :],
                              w_down[l, f0 * 128:(f0 + nf) * 128, nt * 512:(nt + 1) * 512].rearrange("(c p) n -> p c n", p=128),
                              writes=[wdk])
                        for tb in range(4):
                            fns = [(lambda e, fc=fc, tb=tb, wd=wd: e.matmul(ps[banks[tb]][:], lhsT=actT[:, fc, tb * 128:(tb + 1) * 128],
                                                                            rhs=wd[:, fc - f0, :], start=(fc == 0), stop=(fc == FC - 1)))
                                   for fc in range(f0, f0 + nf)]
                            s.group("pe", fns, reads=[("actT", fc) for fc in range(f0, f0 + nf)] + [wdk],
                                    writes=[("ps", banks[tb])])
                    for tb in range(4):
                        copy_op(EV(), fsb[:, tb, nt * 512:(nt + 1) * 512], ps[banks[tb]][:], [("ps", banks[tb])], [("fsb", tb)])
                s.barrier()
                A.reset(MID2)
                gb1 = A.alloc([D], F32)
                xts = [A.alloc([D], F32) for _ in range(2)]
                xbs = [A.alloc([D], BF16) for _ in range(2)]
                s.dma("sp", gb1, gb_fpost[l], writes=["gb1"])
                for tb in range(4):
                    T0 = tg * TG + tb * 128
                    par = tb % 2
                    xt, xb = xts[par], xbs[par]
                    xk, bk = ("xt", par), ("xb", par)
                    s.dma("sp", xt, xres_d[T0:T0 + 128, :], writes=[xk])
                    r_ap, rk = rstd(fsb[:, tb, :], xb, 32 * par, D, [("fsb", tb)], [bk])
                    s.op("dve", lambda g: g.scalar_tensor_tensor(out=fsb[:, tb, :], in0=fsb[:, tb, :], scalar=r_ap,
                                                                 in1=gb1, op0=ALU.mult, op1=ALU.mult),
                         reads=[("fsb", tb), rk, "gb1"], writes=[("fsb", tb)])
                    s.op("pool", lambda g: g.tensor_tensor(out=xt, in0=xt, in1=fsb[:, tb, :], op=ALU.add),
                         reads=[xk, ("fsb", tb)], writes=[xk])
                    s.dma("sp", dst[T0:T0 + 128, :], xt, reads=[xk])
                s.barrier()
        s.finish()
    print("instructions:", s.n_inst, "sems:", s.nsem)
    return nc


def make_consts(S):
    kin = np.arange(128)[:, None]
    j = np.arange(S)[None, :]
    delta = j - kin
    msd = np.zeros((128, S), np.float32)
    for window, dil in DILATED_PATTERNS:
        msd += ((delta >= 0) & (delta <= window) & (delta % dil == 0)).astype(np.float32)
    msc = (delta >= 0).astype(np.float32)
    mss = (delta > 0).astype(np.float32)
    nbm = S // MOBA_BLOCK
    nm = np.zeros((128, nbm, 8), np.float32)
    for blk in range(nbm):
        for n in range(8):
            if n >= blk:
                nm[:, blk, n] = -1e30
    jj = np.arange(128)[:, None]
    ss = np.arange(128)[None, :]
    tri = (jj > ss).astype(np.float32)
    return {"c_ident": np.eye(128, dtype=np.float32), "c_tri": tri, "c_msd": msd, "c_msc": msc, "c_mss": mss, "c_nm": nm}


def pool_coef(S, windows):
    t = np.arange(S)
    out = np.zeros((len(windows), 4, 128, S), np.float32)
    for g, w in enumerate(windows):
        wi = {2: 0, 4: 1, 8: 2, 16: 3}[w]
        out[g, wi] = np.broadcast_to(1.0 / np.minimum(w, t + 1).astype(np.float32), (128, S))
    return out


def bcast128(v):
    return np.ascontiguousarray(np.broadcast_to(v[:, None, :], (v.shape[0], 128, v.shape[1]))).astype(np.float32)


_NC_CACHE = {}


def kernel(x, ln_mix_pre, w_in, w_pool, pool_scale, mix_out_norm, w_out, ln_mix_post, ln_ffn_pre,
           w_gate, w_up, w_down, ln_ffn_post):
    x = np.asarray(x, np.float32)
    B, S, D = x.shape
    L = w_in.shape[0]
    DFF = w_gate.shape[2]
    GWF = D // 4
    NR = 2
    GW = GWF // NR
    NH = GW // 128
    TO = S // NR
    PGD = GWF // 4
    PGn = 4 // NR
    n_cores = B * NR
    perm = np.concatenate([np.arange(m * GWF + r * GW, m * GWF + (r + 1) * GW) for r in range(NR) for m in range(4)])
    groups = [[(r * 4 * GW + m * GW, GW) for r in range(NR)] for m in range(4)]
    cfg = dict(S=S, D=D, NH=NH, DFF=DFF, TO=TO, L=L, PGn=PGn, PGD=PGD, NGW=GWF, windows=None, groups=groups,
               NR=NR, RG=[[2 * i, 2 * i + 1] for i in range(B)])
    key = (S, D, NH, DFF, L, NR, B)
    if key not in _NC_CACHE:
        _NC_CACHE[key] = build(cfg)
    nc = _NC_CACHE[key]
    w_in = np.asarray(w_in, np.float32)
    common = {
        "w_out": np.ascontiguousarray(np.asarray(w_out, np.float32)[:, perm, :]),
        "w_gate": np.ascontiguousarray(w_gate, np.float32), "w_up": np.ascontiguousarray(w_up, np.float32),
        "w_down": np.ascontiguousarray(w_down, np.float32),
        "gb_pre": bcast128(np.asarray(ln_mix_pre)), "gb_post": bcast128(np.asarray(ln_mix_post)),
        "gb_fpre": bcast128(np.asarray(ln_ffn_pre)), "gb_fpost": bcast128(np.asarray(ln_ffn_post)),
        "gb_mo": bcast128(np.asarray(mix_out_norm)[:, perm]),
    }
    common.update(make_consts(S))
    per_rank = []
    for r in range(NR):
        cols = np.concatenate([np.arange(sg * GWF + r * GW, sg * GWF + (r + 1) * GW) for sg in range(10)])
        ab = np.zeros((128, 2), np.float32)
        ab[:, r] = 1.0
        per_rank.append({
            "w_in": np.ascontiguousarray(w_in[:, :, cols]),
            "w_pool": np.ascontiguousarray(np.asarray(w_pool, np.float32)[:, r * PGn:(r + 1) * PGn]),
            "psb": bcast128(np.asarray(pool_scale)[:, r * GW:(r + 1) * GW]),
            "c_pcoef": pool_coef(S, POOL_WINDOWS[r * PGn:(r + 1) * PGn]),
            "c_ab": ab,
        })
    in_maps = []
    for c in range(n_cores):
        b, r = divmod(c, NR)
        m = dict(common)
        m.update(per_rank[r])
        m["x_seq"] = np.ascontiguousarray(x[b, r * TO:(r + 1) * TO])
        in_maps.append(m)
    res = run_bass_kernel_spmd(nc, in_maps, core_ids=list(range(n_cores)))
    out = np.empty((B, S, D), np.float32)
    for c in range(n_cores):
        b, r = divmod(c, NR)
        out[b, r * TO:(r + 1) * TO] = res.results[c]["out"]
    return out
```
